# Optimizing a Trainium2 kernel written in Bass

```python
import math
import numpy as np
import jax
import jax.numpy as jnp
from jax import lax

D_MODEL = 2048
BATCH = 8
SEQ = 4096
DEPTH = 2

CTX_LEN = 256
GRID_W = 64
HEAD_DIM = 128
Q_BLOCK = 128
ROPE_THETA = 10000.0
EPS = 1e-6
NEG_INF = -1e30

A_HEADS = 8
A_KV_HEADS = 2
A_WINDOW = 128
B_HEADS = 4
C_HEADS = 8
C_KV_HEADS = 2
D_HEADS = 8
NA_ROWS = 8
NA_COLS = 16

EVEN_SPLITS = (A_HEADS * HEAD_DIM, A_KV_HEADS * HEAD_DIM, A_KV_HEADS * HEAD_DIM,
               B_HEADS * 2 * HEAD_DIM, B_HEADS * 2 * HEAD_DIM, B_HEADS * 2 * HEAD_DIM)
ODD_SPLITS = (C_HEADS * HEAD_DIM, C_KV_HEADS * HEAD_DIM, C_KV_HEADS * HEAD_DIM,
              D_HEADS * HEAD_DIM, D_HEADS * HEAD_DIM, D_HEADS * HEAD_DIM)
IN_WIDTH = 4608
MIX_WIDTH = 2048

N_EXPERTS = 16
EC_CAPACITY_FACTOR = 2
D_EXPERT = 1536

kernel_name = 'hybrid_window_diff_axial_natten_ecmoe_dit'


def rmsnorm(x, g):
    xf = x.astype(jnp.float32)
    y = xf * lax.rsqrt(jnp.mean(xf * xf, axis=-1, keepdims=True) + EPS)
    return y.astype(x.dtype) * g


def axial_rope_tables(n, dtype):
    t = jnp.arange(n, dtype=jnp.int32)
    row = (t // GRID_W).astype(jnp.float32)
    col = (t % GRID_W).astype(jnp.float32)
    n_freq = HEAD_DIM // 4
    inv = ROPE_THETA ** (-jnp.arange(n_freq, dtype=jnp.float32) / n_freq)
    ar = row[:, None] * inv
    ac = col[:, None] * inv
    ang = jnp.concatenate([ar, ar, ac, ac], axis=-1)
    return jnp.cos(ang).astype(dtype), jnp.sin(ang).astype(dtype)


def apply_rope(x, cos, sin):
    nf = HEAD_DIM // 4
    xr = x.reshape(x.shape[:-1] + (2, 2, nf))
    rot = jnp.stack([-xr[..., 1, :], xr[..., 0, :]], axis=-2).reshape(x.shape)
    shp = (x.shape[1],) + (1,) * (x.ndim - 3) + (HEAD_DIM,)
    return x * cos.reshape(shp) + rot * sin.reshape(shp)


def split_cols(p, sizes):
    offsets = np.cumsum(np.array(sizes))[:-1].tolist()
    return jnp.split(p, offsets, axis=-1)


def sweep_query_blocks(fn, *qs):
    b, s = qs[0].shape[:2]
    nb = s // Q_BLOCK
    blocks = tuple(jnp.moveaxis(q.reshape((b, nb, Q_BLOCK) + q.shape[2:]), 1, 0) for q in qs)
    out = lax.map(lambda a: fn(*a[0], a[1]), (blocks, jnp.arange(nb)))
    return jnp.moveaxis(out, 0, 1).reshape((b, s) + out.shape[3:])


def attend(q, k, v, scale, sink=None, mask=None):
    s = jnp.einsum('bqgrd,bkgd->bgrqk', q, k).astype(jnp.float32) * scale
    if mask is not None:
        s = jnp.where(mask, s, NEG_INF)
    if sink is not None:
        sk = jnp.broadcast_to(sink.astype(jnp.float32)[None, :, :, None, None], s.shape[:-1] + (1,))
        p = jax.nn.softmax(jnp.concatenate([s, sk], axis=-1), axis=-1)[..., :-1]
    else:
        p = jax.nn.softmax(s, axis=-1)
    return jnp.einsum('bgrqk,bkgd->bqgrd', p.astype(v.dtype), v)


def diff_attend(q1, q2, k1, k2, v, lam, scale):
    p1 = jax.nn.softmax(jnp.einsum('bqhd,bkhd->bhqk', q1, k1).astype(jnp.float32) * scale, axis=-1)
    p2 = jax.nn.softmax(jnp.einsum('bqhd,bkhd->bhqk', q2, k2).astype(jnp.float32) * scale, axis=-1)
    p = p1 - lam * p2
    return jnp.einsum('bhqk,bkhe->bqhe', p.astype(v.dtype), v)


def window_attention(q, k, v, k_ctx, v_ctx, sink, scale):
    s = q.shape[1]
    band = Q_BLOCK + 2 * A_WINDOW
    pad = ((0, 0), (A_WINDOW, A_WINDOW), (0, 0), (0, 0))
    k_pad = jnp.pad(k, pad)
    v_pad = jnp.pad(v, pad)
    ctx_mask = jnp.ones((Q_BLOCK, k_ctx.shape[1]), dtype=bool)

    def block(qb, i):
        start = i * Q_BLOCK
        kb = lax.dynamic_slice_in_dim(k_pad, start, band, axis=1)
        vb = lax.dynamic_slice_in_dim(v_pad, start, band, axis=1)
        qpos = start + jnp.arange(Q_BLOCK)
        kpos = start - A_WINDOW + jnp.arange(band)
        in_band = ((jnp.abs(qpos[:, None] - kpos[None, :]) <= A_WINDOW)
                   & (kpos >= 0)[None, :] & (kpos < s)[None, :])
        mask = jnp.concatenate([ctx_mask, in_band], axis=1)
        return attend(qb, jnp.concatenate([k_ctx, kb], axis=1), jnp.concatenate([v_ctx, vb], axis=1),
                      scale, sink, mask)

    return sweep_query_blocks(block, q)


def neighbourhood_attention(q, k, v, k_ctx, v_ctx, rpb, scale):
    b, s, h, d = q.shape
    rows = s // GRID_W
    kh = min(NA_ROWS, rows)
    n_ctx = k_ctx.shape[1]
    qg = q.reshape(b, rows, GRID_W, h, d)
    kg = k.reshape(b, rows, GRID_W, h, d)
    vg = v.reshape(b, rows, GRID_W, h, d)
    col = jnp.arange(GRID_W)
    cs = jnp.clip(col - NA_COLS // 2, 0, GRID_W - NA_COLS)
    col_mask = (col[None, :] >= cs[:, None]) & (col[None, :] < cs[:, None] + NA_COLS)
    col_idx = jnp.clip(col[None, :] - col[:, None], -(NA_COLS - 1), NA_COLS - 1) + NA_COLS - 1
    rpb_c = rpb[:, :, col_idx]

    def row_block(args):
        q_row, r = args
        rs = jnp.clip(r - kh // 2, 0, rows - kh)
        kb = lax.dynamic_slice_in_dim(kg, rs, kh, axis=1)
        vb = lax.dynamic_slice_in_dim(vg, rs, kh, axis=1)
        sc = jnp.einsum('bqhd,bawhd->bhqaw', q_row, kb).astype(jnp.float32) * scale
        roff = rs + jnp.arange(kh) - r + NA_ROWS - 1
        bias = jnp.transpose(rpb_c[:, roff], (0, 2, 1, 3)).astype(jnp.float32)
        sc = jnp.where(col_mask[:, None, :], sc + bias[None], NEG_INF)
        sc = sc.reshape(b, h, GRID_W, kh * GRID_W)
        sx = jnp.einsum('bqhd,bkhd->bhqk', q_row, k_ctx).astype(jnp.float32) * scale
        p = jax.nn.softmax(jnp.concatenate([sx, sc], axis=-1), axis=-1).astype(v.dtype)
        px = p[..., :n_ctx]
        pb = p[..., n_ctx:].reshape(b, h, GRID_W, kh, GRID_W)
        return (jnp.einsum('bhqk,bkhd->bqhd', px, v_ctx)
                + jnp.einsum('bhqaw,bawhd->bqhd', pb, vb))

    out = lax.map(row_block, (jnp.moveaxis(qg, 1, 0), jnp.arange(rows)))
    return jnp.moveaxis(out, 0, 1).reshape(b, s, h, d)


def mixer_window_diff(p_lat, p_ctx, cos, sin, sink, lam_q1, lam_k1, lam_q2, lam_k2, subln_w, lam_init, need_ctx):
    b, s, _ = p_lat.shape
    scale = HEAD_DIM ** -0.5
    ra = A_HEADS // A_KV_HEADS

    def heads(p):
        n = p.shape[1]
        qa, ka, va, qb, kb, vb = split_cols(p, EVEN_SPLITS)
        return (qa.reshape(b, n, A_KV_HEADS, ra, HEAD_DIM),
                ka.reshape(b, n, A_KV_HEADS, HEAD_DIM),
                va.reshape(b, n, A_KV_HEADS, HEAD_DIM),
                qb.reshape(b, n, B_HEADS, 2, HEAD_DIM),
                kb.reshape(b, n, B_HEADS, 2, HEAD_DIM),
                vb.reshape(b, n, B_HEADS, 2 * HEAD_DIM))

    qa, ka, va, qb, kb, vb = heads(p_lat)
    qa_x, ka_x, va_x, qb_x, kb_x, vb_x = heads(p_ctx)
    qa, ka, qb, kb = (apply_rope(t, cos, sin) for t in (qa, ka, qb, kb))
    sink_gr = sink.reshape(A_KV_HEADS, ra)
    o_a = window_attention(qa, ka, va, ka_x, va_x, sink_gr, scale)
    lam = (jnp.exp(jnp.sum(lam_q1.astype(jnp.float32) * lam_k1.astype(jnp.float32)))
           - jnp.exp(jnp.sum(lam_q2.astype(jnp.float32) * lam_k2.astype(jnp.float32))) + lam_init)
    k1_all = jnp.concatenate([kb_x[..., 0, :], kb[..., 0, :]], axis=1)
    k2_all = jnp.concatenate([kb_x[..., 1, :], kb[..., 1, :]], axis=1)
    v_all = jnp.concatenate([vb_x, vb], axis=1)
    o_b = sweep_query_blocks(lambda q1, q2, i: diff_attend(q1, q2, k1_all, k2_all, v_all, lam, scale),
                             qb[..., 0, :], qb[..., 1, :])
    o_b = rmsnorm(o_b, subln_w) * (1.0 - lam_init)
    out_lat = jnp.concatenate([o_a.reshape(b, s, -1), o_b.reshape(b, s, -1)], axis=-1)
    if not need_ctx:
        return out_lat, None
    n = p_ctx.shape[1]
    oa_x = attend(qa_x, ka_x, va_x, scale, sink_gr)
    ob_x = diff_attend(qb_x[..., 0, :], qb_x[..., 1, :], kb_x[..., 0, :], kb_x[..., 1, :], vb_x, lam, scale)
    ob_x = rmsnorm(ob_x, subln_w) * (1.0 - lam_init)
    out_ctx = jnp.concatenate([oa_x.reshape(b, n, -1), ob_x.reshape(b, n, -1)], axis=-1)
    return out_lat, out_ctx


def mixer_axial_neighbourhood(p_lat, p_ctx, cos, sin, q_norm, k_norm, rpb, need_ctx):
    b, s, _ = p_lat.shape
    scale = HEAD_DIM ** -0.5
    rc = C_HEADS // C_KV_HEADS

    def heads(p):
        n = p.shape[1]
        cq, ck, cv, dq, dk, dv = split_cols(p, ODD_SPLITS)
        return (rmsnorm(cq.reshape(b, n, C_KV_HEADS, rc, HEAD_DIM), q_norm),
                rmsnorm(ck.reshape(b, n, C_KV_HEADS, HEAD_DIM), k_norm),
                cv.reshape(b, n, C_KV_HEADS, HEAD_DIM),
                dq.reshape(b, n, D_HEADS, HEAD_DIM),
                dk.reshape(b, n, D_HEADS, HEAD_DIM),
                dv.reshape(b, n, D_HEADS, HEAD_DIM))

    cq, ck, cv, dq, dk, dv = heads(p_lat)
    cq_x, ck_x, cv_x, dq_x, dk_x, dv_x = heads(p_ctx)
    cq = apply_rope(cq, cos, sin)
    ck = apply_rope(ck, cos, sin)
    k_all = jnp.concatenate([ck_x, ck], axis=1)
    v_all = jnp.concatenate([cv_x, cv], axis=1)
    o_c = sweep_query_blocks(lambda q, i: attend(q, k_all, v_all, scale), cq)
    o_d = neighbourhood_attention(dq, dk, dv, dk_x, dv_x, rpb, scale)
    out_lat = jnp.concatenate([o_c.reshape(b, s, -1), o_d.reshape(b, s, -1)], axis=-1)
    if not need_ctx:
        return out_lat, None
    n = p_ctx.shape[1]
    oc_x = attend(cq_x, ck_x, cv_x, scale)
    od_x = attend(dq_x[:, :, :, None, :], dk_x, dv_x, scale)
    out_ctx = jnp.concatenate([oc_x.reshape(b, n, -1), od_x.reshape(b, n, -1)], axis=-1)
    return out_lat, out_ctx


def expert_choice_ffn(h, w_router, w_gate, w_up, w_down):
    b, n, _ = h.shape
    cap = max(1, EC_CAPACITY_FACTOR * n // N_EXPERTS)
    aff = jax.nn.softmax((h @ w_router).astype(jnp.float32), axis=-1)
    gate, idx = lax.top_k(jnp.swapaxes(aff, 1, 2), cap)
    bidx = jnp.arange(b)[:, None, None]
    xe = h[bidx, idx]
    hid = (jax.nn.silu(jnp.einsum('becd,edf->becf', xe, w_gate))
           * jnp.einsum('becd,edf->becf', xe, w_up))
    ye = jnp.einsum('becf,efd->becd', hid, w_down) * gate[..., None].astype(h.dtype)
    return jnp.zeros_like(h).at[bidx, idx].add(ye)


def lambda_init_for(layer):
    return 0.8 - 0.6 * math.exp(-0.3 * layer)


def setup_inputs(seed: int = 0) -> dict:
    key = jax.random.key(seed)
    ks = jax.random.split(key, 24)
    n_even = (DEPTH + 1) // 2
    n_odd = DEPTH // 2
    d = D_MODEL

    def nrm(k, shape, scale):
        return jax.random.normal(k, shape, jnp.float32) * scale

    return {
        'x': nrm(ks[0], (BATCH, SEQ, d), 1.0),
        'c': nrm(ks[1], (BATCH, d), 1.0),
        'ctx': nrm(ks[2], (BATCH, CTX_LEN, d), 1.0),
        'c_ctx': nrm(ks[3], (d,), 1.0),
        'w_ada': nrm(ks[4], (DEPTH, d, 6 * d), 0.5 * d ** -0.5),
        'b_ada': nrm(ks[5], (DEPTH, 6 * d), 0.01),
        'g_mix': 1.0 + nrm(ks[6], (DEPTH, d), 0.05),
        'g_ffn': 1.0 + nrm(ks[7], (DEPTH, d), 0.05),
        'w_in': nrm(ks[8], (DEPTH, d, IN_WIDTH), d ** -0.5),
        'w_out': nrm(ks[9], (DEPTH, MIX_WIDTH, d), MIX_WIDTH ** -0.5),
        'a_sink': nrm(ks[10], (n_even, A_HEADS), 0.5),
        'b_lam_q1': nrm(ks[11], (n_even, HEAD_DIM), 0.1),
        'b_lam_k1': nrm(ks[12], (n_even, HEAD_DIM), 0.1),
        'b_lam_q2': nrm(ks[13], (n_even, HEAD_DIM), 0.1),
        'b_lam_k2': nrm(ks[14], (n_even, HEAD_DIM), 0.1),
        'b_subln': 1.0 + nrm(ks[15], (n_even, 2 * HEAD_DIM), 0.05),
        'c_q_norm': 1.0 + nrm(ks[16], (n_odd, HEAD_DIM), 0.05),
        'c_k_norm': 1.0 + nrm(ks[17], (n_odd, HEAD_DIM), 0.05),
        'd_rpb': nrm(ks[18], (n_odd, D_HEADS, 2 * NA_ROWS - 1, 2 * NA_COLS - 1), 0.1),
        'w_router': nrm(ks[19], (DEPTH, d, N_EXPERTS), d ** -0.5),
        'w_gate': nrm(ks[20], (DEPTH, N_EXPERTS, d, D_EXPERT), d ** -0.5),
        'w_up': nrm(ks[21], (DEPTH, N_EXPERTS, d, D_EXPERT), d ** -0.5),
        'w_down': nrm(ks[22], (DEPTH, N_EXPERTS, D_EXPERT, d), D_EXPERT ** -0.5),
        'g_final': 1.0 + nrm(ks[23], (d,), 0.05),
    }


def reference(x, c, ctx, c_ctx, w_ada, b_ada, g_mix, g_ffn, w_in, w_out, a_sink,
              b_lam_q1, b_lam_k1, b_lam_q2, b_lam_k2, b_subln, c_q_norm, c_k_norm, d_rpb,
              w_router, w_gate, w_up, w_down, g_final):
    s = x.shape[1]
    cos, sin = axial_rope_tables(s, x.dtype)
    cond = jax.nn.silu(c)
    cond_x = jax.nn.silu(c_ctx)
    xc = ctx
    for l in range(DEPTH):
        need_ctx = l < DEPTH - 1
        e = l // 2
        mod = (cond @ w_ada[l] + b_ada[l])[:, None, :]
        mod_x = cond_x @ w_ada[l] + b_ada[l]
        sh1, sc1, gt1, sh2, sc2, gt2 = jnp.split(mod, 6, axis=-1)
        sh1x, sc1x, gt1x, sh2x, sc2x, gt2x = jnp.split(mod_x, 6, axis=-1)
        p = (rmsnorm(x, g_mix[l]) * (1 + sc1) + sh1) @ w_in[l]
        px = (rmsnorm(xc, g_mix[l]) * (1 + sc1x) + sh1x) @ w_in[l]
        if l % 2 == 0:
            o, ox = mixer_window_diff(p, px, cos, sin, a_sink[e], b_lam_q1[e], b_lam_k1[e],
                                      b_lam_q2[e], b_lam_k2[e], b_subln[e], lambda_init_for(l), need_ctx)
        else:
            o, ox = mixer_axial_neighbourhood(p, px, cos, sin, c_q_norm[e], c_k_norm[e], d_rpb[e], need_ctx)
        x = x + gt1 * (o @ w_out[l])
        h = rmsnorm(x, g_ffn[l]) * (1 + sc2) + sh2
        x = x + gt2 * expert_choice_ffn(h, w_router[l], w_gate[l], w_up[l], w_down[l])
        if need_ctx:
            xc = xc + gt1x * (ox @ w_out[l])
            hx = rmsnorm(xc, g_ffn[l]) * (1 + sc2x) + sh2x
            xc = xc + gt2x * expert_choice_ffn(hx, w_router[l], w_gate[l], w_up[l], w_down[l])
    return rmsnorm(x, g_final)
```

```python
import numpy as np
import ml_dtypes
from contextlib import ExitStack
import concourse.bass as bass
import concourse.mybir as mybir
from concourse.bass_utils import run_bass_kernel_spmd

F32 = mybir.dt.float32
BF16 = mybir.dt.bfloat16
I32 = mybir.dt.int32
AF = mybir.ActivationFunctionType
ALU = mybir.AluOpType

D = 2048
S = 4096
CTX = 256
NT = S + CTX
NTT = NT // 128
INW = 4608
NE = 16
DE = 1536
CAP = 512
CAPX = 32
HD = 128
EPS = 1e-6
SCALE = HD ** -0.5
GRID_W = 64
NBF16 = ml_dtypes.bfloat16


def lambda_init_for(layer):
    import math
    return 0.8 - 0.6 * math.exp(-0.3 * layer)


class Res:
    __slots__ = ("w", "r")

    def __init__(self):
        self.w = None
        self.r = {}


class Trk:
    def __init__(self, nc, es):
        self.nc = nc
        self.eng = {}
        for name, e in (("pe", nc.tensor), ("act", nc.scalar), ("dve", nc.vector),
                        ("pool", nc.gpsimd), ("sp", nc.sync)):
            sem = es.enter_context(nc.semaphore("s_" + name)) if name != "sp" else None
            self.eng[name] = dict(e=e, sem=sem, n=0, waited={}, name=name)
        self.dp = {}
        for q, n in (("sp", 28), ("pool", 28), ("act", 6)):
            self.dp[q] = dict(sems=[es.enter_context(nc.semaphore("d_%s_%d" % (q, i))) for i in range(n)],
                              cnt=[0] * n, nxt=0)

    def _wait(self, en, tok):
        if tok is None:
            return
        key, sem, val = tok
        E = self.eng[en]
        if en == "pe" and key == "pe":
            return
        if E["waited"].get(key, 0) >= val:
            return
        E["e"].wait_ge(sem, val)
        E["waited"][key] = val

    def _deps(self, en, reads, writes):
        for r in reads:
            self._wait(en, r.w)
        for w in writes:
            self._wait(en, w.w)
            for t in list(w.r.values()):
                self._wait(en, t)

    def _upd(self, tok, reads, writes):
        for r in reads:
            old = r.r.get(tok[0])
            if old is None or old[2] < tok[2]:
                r.r[tok[0]] = tok
        for w in writes:
            w.w = tok
            w.r = {}

    def op(self, en, fn, reads=(), writes=(), inc=True):
        E = self.eng[en]
        self._deps(en, reads, writes)
        ins = fn()
        if inc:
            E["n"] += 1
            ins.then_inc(E["sem"], 1)
            tok = (en, E["sem"], E["n"])
        else:
            tok = (en, E["sem"], E["n"] + 1)
        self._upd(tok, reads, writes)
        return tok

    def dma(self, q, out, in_, reads=(), writes=(), indirect=None, extra_waits=(), **kw):
        P = self.dp[q]
        i = P["nxt"]
        P["nxt"] = (i + 1) % len(P["sems"])
        key = "d_%s_%d" % (q, i)
        sem = P["sems"][i]
        if P["cnt"][i] > 0:
            self._wait(q, (key, sem, P["cnt"][i]))
        self._deps(q, reads, writes)
        for t in extra_waits:
            self._wait(q, t)
        E = self.eng[q]
        if indirect is None:
            ins = E["e"].dma_start(out=out, in_=in_, **kw)
        else:
            ins = E["e"].indirect_dma_start(out=out, in_=in_, **indirect)
        P["cnt"][i] += 16
        ins.then_inc(sem, 16)
        tok = (key, sem, P["cnt"][i])
        self._upd(tok, reads, writes)
        return tok

    def all_tokens(self):
        toks = []
        for name, E in self.eng.items():
            if E["sem"] is not None and E["n"] > 0:
                toks.append((name, E["sem"], E["n"]))
        for q, P in self.dp.items():
            for i, s in enumerate(P["sems"]):
                if P["cnt"][i] > 0:
                    toks.append(("d_%s_%d" % (q, i), s, P["cnt"][i]))
        return toks

    def barrier(self):
        toks = self.all_tokens()
        for en in ("pe", "act", "dve", "pool", "sp"):
            for t in toks:
                self._wait(en, t)


class Ctx:
    pass


def rope_tables():
    t = np.arange(S)
    row = (t // GRID_W).astype(np.float32)
    col = (t % GRID_W).astype(np.float32)
    nf = HD // 4
    inv = (10000.0 ** (-np.arange(nf, dtype=np.float32) / nf)).astype(np.float32)
    ar = row[:, None] * inv
    ac = col[:, None] * inv
    ang = np.concatenate([ar, ar, ac, ac], axis=-1).astype(np.float32)
    cos = np.cos(ang).astype(np.float32)
    sin = np.sin(ang).astype(np.float32)
    sgn = np.ones(HD, np.float32)
    sgn[0:32] = -1.0
    sgn[64:96] = -1.0
    tab = np.zeros((NT, 2, HD), np.float32)
    tab[:CTX, 0, :] = 1.0
    tab[CTX:, 0, :] = cos
    tab[CTX:, 1, :] = sin * sgn
    return np.ascontiguousarray(tab.reshape(NTT, 128, 2 * HD).transpose(1, 0, 2))


def d_tile_sets():
    out = []
    for i in range(32):
        if i == 0:
            js = [0, 1, 2, 3]
            ids = [5 + k for k in range(4)]
        elif i == 1:
            js = [0, 1, 2, 3]
            ids = [9 + k for k in range(4)]
        elif i == 30:
            js = [28, 29, 30, 31]
            ids = [13 + k for k in range(4)]
        elif i == 31:
            js = [28, 29, 30, 31]
            ids = [17 + k for k in range(4)]
        else:
            js = [i - 2, i - 1, i, i + 1, i + 2]
            ids = [0, 1, 2, 3, 4]
        out.append((js, ids))
    return out


def d_tables(rpb):
    tabs = np.full((8, 21, 128, 128), -1e30, np.float32)
    sets = d_tile_sets()
    done = set()
    kl = np.arange(128)
    ql = np.arange(128)
    for i in (2, 0, 1, 30, 31):
        js, ids = sets[i]
        for j, tid in zip(js, ids):
            if tid in done:
                continue
            done.add(tid)
            kr = 2 * j + kl // 64
            kc = kl % 64
            qr = 2 * i + ql // 64
            qc = ql % 64
            rs = np.clip(qr - 4, 0, 56)
            cs = np.clip(qc - 8, 0, 48)
            vis = ((kr[:, None] >= rs[None, :]) & (kr[:, None] < rs[None, :] + 8)
                   & (kc[:, None] >= cs[None, :]) & (kc[:, None] < cs[None, :] + 16))
            ridx = np.clip(kr[:, None] - qr[None, :] + 7, 0, 14)
            cidx = np.clip(kc[:, None] - qc[None, :], -15, 15) + 15
            for h in range(8):
                vals = rpb[h][ridx, cidx]
                tabs[h, tid] = np.where(vis, vals, np.float32(-1e30))
    return np.ascontiguousarray(tabs.transpose(2, 0, 1, 3))


def const_pack():
    c = {}
    c["ident_bf"] = np.eye(128, dtype=np.float32).astype(NBF16)
    c["ident_f"] = np.eye(128, dtype=np.float32)
    c["ropetab"] = rope_tables()
    k = np.arange(128)[:, None]
    q = np.arange(128)[None, :]
    mu = (k >= q).astype(np.float32)
    ml = (k <= q).astype(np.float32)
    bm = np.zeros((128, 2, 4, 128), np.float32)
    bm[:, 0] = mu[:, None, :]
    bm[:, 1] = ml[:, None, :]
    c["bandmask"] = bm.astype(NBF16)
    c["tri"] = np.concatenate([np.ones((128, 128), np.float32),
                               (k < q).astype(np.float32)], axis=1).astype(NBF16)
    c["iota"] = np.tile(np.arange(512, dtype=np.float32)[None, :], (128, 1))
    rows = (np.arange(NTT)[None, :] * 128 + np.arange(128)[:, None])
    tk = np.zeros((128, NTT, 2), np.float32)
    tk[:, :, 0] = rows // 64
    tk[:, :, 1] = rows % 64
    c["tokab"] = tk.astype(NBF16)
    return c


EVEN_TYPES = ["R"] * 8 + ["R"] * 2 + ["V"] * 2 + ["R"] * 8 + ["R"] * 8 + ["V"] * 8
ODD_TYPES = ["NQ"] * 8 + ["NK"] * 2 + ["V"] * 2 + ["P"] * 8 + ["P"] * 8 + ["V"] * 8


def block_maps(types):
    ft, vv = {}, {}
    for b, t in enumerate(types):
        if t == "V":
            vv[b] = len(vv)
        else:
            ft[b] = len(ft)
    return ft, vv


def build_program(dbg=None):
    dbg = dbg or {}
    kinds = dbg.get("kinds", {})
    nc = bass.Bass("TRN2", target_bir_lowering=False)
    K = Ctx()
    K.nc = nc
    K.dbg = dbg
    _cnt = [0]

    def _sbt(name, shape, dt):
        _cnt[0] += 1
        return nc.sbuf_tensor("%s_%d" % (name, _cnt[0]), shape, dt)

    def _pst(name, shape, dt):
        _cnt[0] += 1
        return nc.psum_tensor("%s_%d" % (name, _cnt[0]), shape, dt)
    K.sbt = _sbt
    K.pst = _pst

    def dram(name, shape, dt, kind):
        shape = dbg.get("shapes", {}).get(name, shape)
        return nc.dram_tensor(name, list(shape), dt, kind=kinds.get(name, kind)).ap()

    d = {}
    K.d = d
    d["x"] = dram("x", [S, D], F32, "ExternalInput")
    d["ctx"] = dram("ctx", [CTX, D], F32, "ExternalInput")
    d["cvec"] = dram("cvec", [2, D], F32, "ExternalInput")
    d["w_ada"] = dram("w_ada", [2, D, 6 * D], F32, "ExternalInput")
    d["b_ada"] = dram("b_ada", [2, 6 * D], F32, "ExternalInput")
    d["gvec"] = dram("gvec", [5, D], F32, "ExternalInput")
    d["w_in"] = dram("w_in", [2, D, INW], F32, "ExternalInput")
    d["w_out"] = dram("w_out", [2, D, D], F32, "ExternalInput")
    d["w_router"] = dram("w_router", [2, D, NE], F32, "ExternalInput")
    d["w_gate"] = dram("w_gate", [2, NE, D, DE], F32, "ExternalInput")
    d["w_up"] = dram("w_up", [2, NE, D, DE], F32, "ExternalInput")
    d["w_down"] = dram("w_down", [2, NE, DE, D], F32, "ExternalInput")
    d["small"] = dram("small", [1, 1024], F32, "ExternalInput")
    d["small2"] = dram("small2", [1, 256], F32, "ExternalInput")
    d["rpbtab"] = dram("rpbtab", [128, 8, 21, 128], F32, "ExternalInput")
    d["ident_bf"] = dram("ident_bf", [128, 128], BF16, "ExternalInput")
    d["ident_f"] = dram("ident_f", [128, 128], F32, "ExternalInput")
    d["ropetab"] = dram("ropetab", [128, NTT, 2 * HD], F32, "ExternalInput")
    d["bandmask"] = dram("bandmask", [128, 2, 4, 128], BF16, "ExternalInput")
    d["tri"] = dram("tri", [128, 256], BF16, "ExternalInput")
    d["iota"] = dram("iota", [128, 512], F32, "ExternalInput")
    d["tokab"] = dram("tokab", [128, NTT, 2], BF16, "ExternalInput")
    d["out"] = dram("out", [S, D], F32, "ExternalOutput")
    d["modv"] = dram("modv", [2, 2, 6 * D], F32, "Internal")
    d["hT"] = dram("hT", [NTT, 128, 16 * 128], BF16, "Internal")
    d["FT"] = dram("FT", [26, 128, NT], BF16, "Internal")
    d["VV"] = dram("VV", [NT, 10 * 128], BF16, "Internal")
    d["OO"] = dram("OO", [NT, D], BF16, "Internal")
    d["H2"] = dram("H2", [NT, D], BF16, "Internal")
    for l in range(2):
        for nb in range(4):
            d["XS%d_%d" % (l, nb)] = dram("XS%d_%d" % (l, nb), [NT, 512], F32, "Internal")
    K.rs = {}

    def R(name):
        if name not in K.rs:
            K.rs[name] = Res()
        return K.rs[name]
    K.R = R

    with ExitStack() as es:
        T = Trk(nc, es)
        K.T = T
        K.ident_bf = es.enter_context(K.sbt("ident_bf_s", [128, 128], BF16))
        K.ident_f = es.enter_context(K.sbt("ident_f_s", [128, 128], F32))
        K.aff = es.enter_context(K.sbt("aff_tm", [128, NTT, NE], F32))
        K.idx = es.enter_context(K.sbt("idx_i", [128, NE, 4], I32))
        K.gate = es.enter_context(K.sbt("gate_f", [128, NE, 4], F32))
        K.idxx = es.enter_context(K.sbt("idxx_i", [128, NE], I32))
        K.gatex = es.enter_context(K.sbt("gatex_f", [128, NE], F32))
        T.dma("sp", K.ident_bf[:], d["ident_bf"], writes=[R("ident")])
        T.dma("sp", K.ident_f[:], d["ident_f"], writes=[R("ident")])

        phases = dbg.get("phases")

        def want(p):
            return phases is None or p in phases

        for l in dbg.get("layers", [0, 1]):
            if want("mod"):
                with nc.named_scope("mod%d" % l):
                    phase_mod(K, l)
                T.barrier()
            if want("norm1"):
                with nc.named_scope("norm1%d" % l):
                    phase_norm(K, l, 1)
                T.barrier()
            if want("proj"):
                with nc.named_scope("proj%d" % l):
                    phase_proj(K, l)
                T.barrier()
            if want("attn"):
                with nc.named_scope("attn%d" % l):
                    phase_attn(K, l)
                T.barrier()
            if want("outproj"):
                with nc.named_scope("outproj%d" % l):
                    phase_outproj(K, l)
                T.barrier()
            if want("norm2"):
                with nc.named_scope("norm2%d" % l):
                    phase_norm(K, l, 2)
                T.barrier()
            if want("route"):
                with nc.named_scope("route%d" % l):
                    phase_route(K, l)
                T.barrier()
            if want("moe"):
                with nc.named_scope("moe%d" % l):
                    phase_moe(K, l)
                T.barrier()
        if want("final"):
            phase_final(K)
        T.barrier()
        if dbg.get("dump"):
            for nm_, tl, shp, dt_ in (("dbg_aff", K.aff, [128, NTT * NE], F32), ("dbg_idx", K.idx, [128, NE * 4], I32),
                                     ("dbg_gate", K.gate, [128, NE * 4], F32), ("dbg_idxx", K.idxx, [128, NE], I32),
                                     ("dbg_gatex", K.gatex, [128, NE], F32)):
                o = nc.dram_tensor(nm_, shp, dt_, kind="ExternalOutput").ap()
                src = tl[:]
                if len(src.shape) == 3:
                    src = src.rearrange("p a b -> p (a b)")
                T.dma("sp", o, src)
            T.barrier()
    return nc


def xs_src(K, l, tt, nb):
    d = K.d
    if l == 0:
        if tt < 2:
            return d["ctx"][tt * 128:(tt + 1) * 128, nb * 512:(nb + 1) * 512]
        return d["x"][(tt - 2) * 128:(tt - 1) * 128, nb * 512:(nb + 1) * 512]
    return d["XS%d_%d" % (l - 1, nb)][tt * 128:(tt + 1) * 128, :]


def load_x_tile(K, l, tt, dst, res, stream_res=None):
    T = K.T
    for nb in range(4):
        rd = [K.R("XS%d_%d" % (l - 1, nb))] if l > 0 else []
        T.dma("sp", dst[:, nb * 512:(nb + 1) * 512], xs_src(K, l, tt, nb), reads=rd, writes=[res])


def phase_mod(K, l):
    nc, T, d, R = K.nc, K.T, K.d, K.R
    with ExitStack() as es:
        sb = lambda n, s, dt: es.enter_context(K.sbt(n, s, dt))
        cT = sb("cT", [128, 2, 16], F32)
        cS = sb("cS", [128, 2, 16], F32)
        condT = sb("condT", [128, 16, 2], BF16)
        wch = [sb("wch%d" % i, [128, 16, 512], BF16) for i in range(2)]
        modrow = sb("modrow", [2, 6 * D], F32)
        brow = sb("brow", [2, 6 * D], F32)
        ps = [es.enter_context(K.pst("mps%d" % i, [128, 512], F32)) for i in range(2)]
        r_c, r_cond, r_b, r_mod = Res(), Res(), Res(), Res()
        r_w = [Res(), Res()]
        r_ps = [Res(), Res()]
        with nc.allow_non_contiguous_dma(reason="tiny cond vector transpose load"):
            T.dma("sp", cT[:], d["cvec"].rearrange("w (kc p) -> p w kc", p=128), writes=[r_c])
        T.dma("sp", brow[:], d["b_ada"][l:l + 1, :].partition_broadcast(2), writes=[r_b])
        T.op("act", lambda: nc.scalar.activation(out=cS[:], in_=cT[:], func=AF.Silu), reads=[r_c], writes=[r_cond])
        T.op("dve", lambda: nc.vector.tensor_copy(out=condT[:].rearrange("p k w -> p w k"), in_=cS[:]),
             reads=[r_cond], writes=[r_cond])
        wv = d["w_ada"][l].rearrange("(kc p) n -> p kc n", p=128)
        NB = 24

        def loadw(nb):
            T.dma("pool", wch[nb % 2][:], wv[:, :, nb * 512:(nb + 1) * 512], writes=[r_w[nb % 2]])
        loadw(0)
        for nb in range(NB):
            if nb + 1 < NB:
                loadw(nb + 1)
            b = nb % 2
            for kc in range(16):
                T.op("pe", lambda kc=kc: nc.tensor.matmul(ps[b][0:2, :], lhsT=condT[:, kc, :], rhs=wch[b][:, kc, :],
                                                         start=(kc == 0), stop=(kc == 15)),
                     reads=[r_cond, r_w[b]], writes=[r_ps[b]], inc=(kc == 15))
            T.op("dve", lambda: nc.vector.tensor_tensor(out=modrow[:, nb * 512:(nb + 1) * 512], in0=ps[b][0:2, :],
                                                        in1=brow[:, nb * 512:(nb + 1) * 512], op=ALU.add),
                 reads=[r_ps[b], r_b], writes=[r_mod])
        for c0 in (D, 4 * D):
            T.op("dve", lambda c0=c0: nc.vector.tensor_scalar(out=modrow[:, c0:c0 + D], in0=modrow[:, c0:c0 + D],
                                                              scalar1=1.0, scalar2=None, op0=ALU.add),
                 reads=[r_mod], writes=[r_mod])
        T.dma("sp", d["modv"][l], modrow[:], reads=[r_mod], writes=[R("modv")])


def load_bc(K, dst_ap, src_row_ap, res, reads=()):
    K.T.dma("sp", dst_ap, src_row_ap.partition_broadcast(128), reads=list(reads), writes=[res])


def phase_norm(K, l, which):
    nc, T, d, R = K.nc, K.T, K.d, K.R
    first_tt = 0
    if which == 2:
        first_tt = 0 if l == 0 else 2
    with ExitStack() as es:
        sb = lambda n, s, dt: es.enter_context(K.sbt(n, s, dt))
        A = [sb("nA%d" % i, [128, D], F32) for i in range(2)]
        SH = [sb("nS%d" % i, [128, D], F32) for i in range(2)]
        gb = sb("ngb", [128, D], F32)
        xt = [sb("nxt%d" % i, [128, D], F32) for i in range(2)]
        junk_ = [sb("njunk%d" % i, [128, D], BF16) for i in range(2)]
        hf_ = [sb("nhf%d" % i, [128, D], F32) for i in range(2)]
        hb = [sb("nhb%d" % i, [128, D], BF16) for i in range(2)]
        hT = [sb("nhT%d" % i, [128, 16, 128], BF16) for i in range(2)]
        st_ = [sb("nst%d" % i, [128, 4], F32) for i in range(2)]
        wr = sb("nwr", [128, 16, NE], BF16)
        ex_ = [sb("nex%d" % i, [128, NE], F32) for i in range(2)]
        tp = [es.enter_context(K.pst("ntp%d" % i, [128, 1024], BF16)) for i in range(2)]
        lp = es.enter_context(K.pst("nlp", [128, 512], F32))
        r_A, r_x, r_hb, r_hT = Res(), [Res(), Res()], [Res(), Res()], [Res(), Res()]
        r_tp, r_lp, r_wr = [Res(), Res()], Res(), Res()
        r_hf_, r_st_, r_junk_, r_ex_ = [Res(), Res()], [Res(), Res()], [Res(), Res()], [Res(), Res()]
        sh_c, sc_c = (0, D) if which == 1 else (3 * D, 4 * D)
        grow = l if which == 1 else 2 + l
        load_bc(K, gb[:], d["gvec"][grow:grow + 1, :], r_A)
        for w in range(2):
            load_bc(K, A[w][:], d["modv"][l, w:w + 1, sc_c:sc_c + D], r_A, reads=[R("modv")])
            load_bc(K, SH[w][:], d["modv"][l, w:w + 1, sh_c:sh_c + D], r_A, reads=[R("modv")])
        for w in range(2):
            T.op("dve", lambda w=w: nc.vector.tensor_tensor(out=A[w][:], in0=A[w][:], in1=gb[:], op=ALU.mult),
                 reads=[r_A], writes=[r_A])
        if which == 2:
            T.dma("pool", wr[:], d["w_router"][l].rearrange("(kc p) e -> p kc e", p=128), writes=[r_wr])
        tiles = list(range(first_tt, NTT))
        def load(tt, buf):
            if which == 1:
                load_x_tile(K, l, tt, xt[buf], r_x[buf])
            else:
                for nb in range(4):
                    T.dma("sp", xt[buf][:, nb * 512:(nb + 1) * 512],
                          d["XS%d_%d" % (l, nb)][tt * 128:(tt + 1) * 128, :],
                          reads=[R("XS%d_%d" % (l, nb))], writes=[r_x[buf]])
        load(tiles[0], 0)
        for n, tt in enumerate(tiles):
            b = n % 2
            if n + 1 < len(tiles):
                load(tiles[n + 1], (n + 1) % 2)
            w = 1 if tt < 2 else 0
            junk, hf, st, ex = junk_[b], hf_[b], st_[b], ex_[b]
            r_junk, r_hf, r_st, r_ex = r_junk_[b], r_hf_[b], r_st_[b], r_ex_[b]
            T.op("act", lambda: nc.scalar.activation(out=junk[:], in_=xt[b][:], func=AF.Square, accum_out=st[:, 0:1]),
                 reads=[r_x[b]], writes=[r_junk, r_st])
            T.op("act", lambda: nc.scalar.activation(out=st[:, 1:2], in_=st[:, 0:1], func=AF.Sqrt, scale=1.0 / D, bias=EPS),
                 reads=[r_st], writes=[r_st])
            T.op("dve", lambda: nc.vector.reciprocal(out=st[:, 2:3], in_=st[:, 1:2]), reads=[r_st], writes=[r_st])
            T.op("dve", lambda: nc.vector.scalar_tensor_tensor(out=hf[:], in0=xt[b][:], scalar=st[:, 2:3], in1=A[w][:],
                                                               op0=ALU.mult, op1=ALU.mult),
                 reads=[r_x[b], r_st, r_A], writes=[r_hf])
            T.op("dve", lambda: nc.vector.tensor_tensor(out=hb[b][:], in0=hf[:], in1=SH[w][:], op=ALU.add),
                 reads=[r_hf, r_A], writes=[r_hb[b]])
            if which == 2:
                T.dma("sp", d["H2"][tt * 128:(tt + 1) * 128, :], hb[b][:], reads=[r_hb[b]], writes=[R("H2")])
            for g in range(2):
                for k8 in range(8):
                    kc = g * 8 + k8
                    T.op("pe", lambda kc=kc, k8=k8, g=g: nc.tensor.transpose(out=tp[g][:, k8 * 128:(k8 + 1) * 128],
                                                                           in_=hb[b][:, kc * 128:(kc + 1) * 128],
                                                                           identity=K.ident_bf[:]),
                         reads=[r_hb[b], R("ident")], writes=[r_tp[g]], inc=(k8 == 7))
                eng = "act" if g == 0 else "dve"
                if eng == "act":
                    T.op("act", lambda g=g: nc.scalar.copy(out=hT[b][:, g * 8:(g + 1) * 8, :].rearrange("p a b -> p (a b)"),
                                                           in_=tp[g][:]),
                         reads=[r_tp[g]], writes=[r_hT[b]])
                else:
                    T.op("dve", lambda g=g: nc.vector.tensor_copy(out=hT[b][:, g * 8:(g + 1) * 8, :].rearrange("p a b -> p (a b)"),
                                                                  in_=tp[g][:]),
                         reads=[r_tp[g]], writes=[r_hT[b]])
            if which == 1:
                T.dma("sp", d["hT"][tt], hT[b][:].rearrange("p a b -> p (a b)"), reads=[r_hT[b]], writes=[R("hT")])
            else:
                for kc in range(16):
                    T.op("pe", lambda kc=kc: nc.tensor.matmul(lp[:, 0:NE], lhsT=hT[b][:, kc, :], rhs=wr[:, kc, :],
                                                             start=(kc == 0), stop=(kc == 15)),
                         reads=[r_hT[b], r_wr], writes=[r_lp], inc=(kc == 15))
                T.op("act", lambda: nc.scalar.activation(out=ex[:], in_=lp[:, 0:NE], func=AF.Exp, accum_out=st[:, 3:4]),
                     reads=[r_lp], writes=[r_ex, r_st])
                T.op("dve", lambda: nc.vector.reciprocal(out=st[:, 3:4], in_=st[:, 3:4]), reads=[r_st], writes=[r_st])
                T.op("dve", lambda tt=tt: nc.vector.tensor_scalar(out=K.aff[:, tt, :], in0=ex[:], scalar1=st[:, 3:4],
                                                                  scalar2=None, op0=ALU.mult),
                     reads=[r_ex, r_st], writes=[K.R("aff")])


def phase_proj(K, l):
    nc, T, d, R = K.nc, K.T, K.d, K.R
    types = EVEN_TYPES if l % 2 == 0 else ODD_TYPES
    ftmap, vvmap = block_maps(types)
    with ExitStack() as es:
        sb = lambda n, s, dt: es.enter_context(K.sbt(n, s, dt))
        wch = [sb("pw%d" % i, [128, 16, 512], BF16) for i in range(2)]
        hT = [sb("phT%d" % i, [128, 16, 128], BF16) for i in range(3)]
        rope = sb("prope", [128, NTT, 2 * HD], F32)
        FTst = sb("pFT", [128, 4, NT], BF16)
        Vst = [sb("pV%d" % i, [128, 512], BF16) for i in range(2)]
        xs = [sb("pxs%d" % i, [128, 512], F32) for i in range(2)]
        t1 = sb("pt1", [128, 512], F32)
        t2 = sb("pt2", [128, 512], F32)
        xb = [sb("pxb%d" % i, [128, 512], BF16) for i in range(2)]
        nrm = sb("pnrm", [128, 2, 128], F32)
        sq = sb("psq", [128, 512], F32)
        st = sb("pst", [128, 8], F32)
        ps = [es.enter_context(K.pst("pps%d" % i, [128, 512], F32)) for i in range(2)]
        tp = [es.enter_context(K.pst("ptp%d" % i, [128, 512], BF16)) for i in range(2)]
        r_w, r_hT = [Res(), Res()], [Res(), Res(), Res()]
        r_rope, r_FT, r_V, r_xs, r_t, r_xb = Res(), Res(), [Res(), Res()], [Res(), Res()], Res(), [Res(), Res()]
        r_ps, r_tp, r_nrm, r_st = [Res(), Res()], [Res(), Res()], Res(), Res()
        T.dma("sp", rope[:], d["ropetab"], writes=[r_rope])
        if l % 2 == 1:
            load_bc(K, nrm[:].rearrange("p a b -> p (a b)"), d["small2"][0:1, :], r_nrm)
        wv = d["w_in"][l].rearrange("(kc p) n -> p kc n", p=128)

        def loadw(cb):
            T.dma("pool", wch[cb % 2][:], wv[:, :, cb * 512:(cb + 1) * 512], writes=[r_w[cb % 2]])

        def loadh(n):
            tt = n % NTT
            T.dma("sp", hT[n % 3][:].rearrange("p a b -> p (a b)"), d["hT"][tt], reads=[R("hT")], writes=[r_hT[n % 3]])
        loadw(0)
        loadh(0)
        loadh(1)
        n = 0
        deferred = []

        def mk_B(tb, xbuf, b0, nbk, c0, c1, tt):
            def fn_():
                for k in range(nbk):
                    T.op("pe", lambda k=k: nc.tensor.transpose(out=tp[tb][:, (b0 + k) * 128:(b0 + k + 1) * 128],
                                                               in_=xb[xbuf][:, (b0 + k) * 128:(b0 + k + 1) * 128],
                                                               identity=K.ident_bf[:]),
                         reads=[r_xb[xbuf], R("ident")], writes=[r_tp[tb]], inc=(k == nbk - 1))
                T.op("act", lambda: nc.scalar.copy(out=FTst[:, b0:b0 + nbk, tt * 128:(tt + 1) * 128],
                                                   in_=tp[tb][:, c0:c1].rearrange("p (h e) -> p h e", e=128)),
                     reads=[r_tp[tb]], writes=[r_FT])
            return fn_

        def mk_flush(cb, btypes):
            def fn_():
                for bi, t in enumerate(btypes):
                    if t == "V":
                        continue
                    T.dma("sp", d["FT"][ftmap[cb * 4 + bi]], FTst[:, bi, :], reads=[r_FT], writes=[R("FT")])
            return fn_
        for cb in range(9):
            if cb + 1 < 9:
                loadw(cb + 1)
            btypes = types[cb * 4:(cb + 1) * 4]
            for tt in range(NTT):
                if n + 2 < 9 * NTT:
                    loadh(n + 2)
                hb_ = n % 3
                pb = n % 2
                for kc in range(16):
                    T.op("pe", lambda kc=kc: nc.tensor.matmul(ps[pb][:], lhsT=hT[hb_][:, kc, :], rhs=wch[cb % 2][:, kc, :],
                                                             start=(kc == 0), stop=(kc == 15)),
                         reads=[r_hT[hb_], r_w[cb % 2]], writes=[r_ps[pb]], inc=(kc == 15))
                for fn_ in deferred:
                    fn_()
                deferred = []
                groups = []
                for bi, t in enumerate(btypes):
                    if groups and groups[-1][0] == t:
                        groups[-1][2] += 1
                    else:
                        groups.append([t, bi, 1])
                for (t, b0, nbk) in groups:
                    c0, c1 = b0 * 128, (b0 + nbk) * 128
                    if t == "V":
                        vb = n % 2
                        T.op("act", lambda: nc.scalar.copy(out=Vst[vb][:, c0:c1], in_=ps[pb][:, c0:c1]),
                             reads=[r_ps[pb]], writes=[r_V[vb]])
                        v0 = vvmap[cb * 4 + b0]
                        T.dma("sp", d["VV"][tt * 128:(tt + 1) * 128, v0 * 128:(v0 + nbk) * 128], Vst[vb][:, c0:c1],
                              reads=[r_V[vb]], writes=[R("VV")])
                        continue
                    xbuf = n % 2
                    if t == "P":
                        T.op("act", lambda: nc.scalar.copy(out=xb[xbuf][:, c0:c1], in_=ps[pb][:, c0:c1]),
                             reads=[r_ps[pb]], writes=[r_xb[xbuf]])
                    else:
                        T.op("act", lambda: nc.scalar.copy(out=xs[xbuf][:, c0:c1], in_=ps[pb][:, c0:c1]),
                             reads=[r_ps[pb]], writes=[r_xs[xbuf]])
                        xv = xs[xbuf][:, c0:c1].rearrange("p (h e) -> p h e", e=128)
                        if t in ("NQ", "NK"):
                            wsel = 0 if t == "NQ" else 1
                            T.op("act", lambda: nc.scalar.activation(out=sq[:, c0:c1], in_=xs[xbuf][:, c0:c1], func=AF.Square),
                                 reads=[r_xs[xbuf]], writes=[r_t])
                            T.op("dve", lambda: nc.vector.tensor_reduce(out=st[:, 0:nbk], in_=sq[:, c0:c1].rearrange("p (h e) -> p h e", e=128),
                                                                        axis=mybir.AxisListType.X, op=ALU.add),
                                 reads=[r_t], writes=[r_st])
                            T.op("act", lambda: nc.scalar.activation(out=st[:, 0:nbk], in_=st[:, 0:nbk], func=AF.Sqrt,
                                                                     scale=1.0 / HD, bias=EPS),
                                 reads=[r_st], writes=[r_st])
                            T.op("dve", lambda: nc.vector.reciprocal(out=st[:, 4:4 + nbk], in_=st[:, 0:nbk]),
                                 reads=[r_st], writes=[r_st])
                            T.op("dve", lambda: nc.vector.tensor_tensor(out=xv, in0=xv,
                                                                        in1=st[:, 4:4 + nbk].unsqueeze(2).to_broadcast([128, nbk, 128]),
                                                                        op=ALU.mult),
                                 reads=[r_xs[xbuf], r_st], writes=[r_xs[xbuf]])
                            T.op("dve", lambda: nc.vector.tensor_tensor(out=xv, in0=xv,
                                                                        in1=nrm[:, wsel, :].unsqueeze(1).to_broadcast([128, nbk, 128]),
                                                                        op=ALU.mult),
                                 reads=[r_xs[xbuf], r_nrm], writes=[r_xs[xbuf]])
                        cosb = rope[:, tt, 0:HD].unsqueeze(1).to_broadcast([128, nbk, HD])
                        t1v = t1[:, c0:c1].rearrange("p (h e) -> p h e", e=128)
                        T.op("dve", lambda: nc.vector.tensor_tensor(out=t1v, in0=xv, in1=cosb, op=ALU.mult),
                             reads=[r_xs[xbuf], r_rope], writes=[r_t])
                        x5 = xs[xbuf][:, c0:c1].rearrange("p (h a j f) -> p (h a) j f", a=2, j=2, f=32)
                        t5 = t2[:, c0:c1].rearrange("p (h a j f) -> p (h a) j f", a=2, j=2, f=32)
                        s5 = rope[:, tt, HD:2 * HD].rearrange("p (a j f) -> p a j f", a=2, j=2, f=32)
                        for j in range(2):
                            for a in range(2):
                                T.op("dve", lambda j=j, a=a: nc.vector.tensor_tensor(
                                    out=t2[:, c0:c1].rearrange("p (h a j f) -> p h a j f", a=2, j=2, f=32)[:, :, a, j, :],
                                    in0=xs[xbuf][:, c0:c1].rearrange("p (h a j f) -> p h a j f", a=2, j=2, f=32)[:, :, a, 1 - j, :],
                                    in1=s5[:, a, j, :].unsqueeze(1).to_broadcast([128, nbk, 32]), op=ALU.mult),
                                    reads=[r_xs[xbuf], r_rope], writes=[r_t])
                        T.op("dve", lambda: nc.vector.tensor_tensor(out=xb[xbuf][:, c0:c1], in0=t1[:, c0:c1], in1=t2[:, c0:c1], op=ALU.add),
                             reads=[r_t], writes=[r_xb[xbuf]])
                    deferred.append(mk_B(n % 2, xbuf, b0, nbk, c0, c1, tt))
                n += 1
            deferred.append(mk_flush(cb, btypes))
        for fn_ in deferred:
            fn_()


def attn_group(K, A, qap, ncols, keys, dvp, finish):
    nc, T = K.nc, K.T
    nsub = ncols // 128
    n = len(keys)
    S_ps, P_sb, acc = A["S"], A["P"], A["acc"]
    r_S, r_P, r_acc = A["r_S"], A["r_P"], A["r_acc"]
    rq, rk, rv, rt = A["rq"], A["rk"], A["rv"], A["rt"]

    def qk(k):
        sbuf = A["cnt"] % 3
        A["scur"][k] = sbuf
        kT, _, _ = keys[k]
        T.op("pe", lambda: nc.tensor.matmul(S_ps[sbuf][:, 0:ncols], lhsT=kT, rhs=qap, start=True, stop=True),
             reads=[rq, rk], writes=[r_S[sbuf]])
        A["cnt"] += 1
    A["scur"] = {}
    qk(0)
    if n > 1:
        qk(1)
    for k in range(n):
        if k + 2 < n:
            qk(k + 2)
        sbuf = A["scur"][k]
        pb = A["pcnt"] % 3
        A["pcnt"] += 1
        _, v, tbl = keys[k]
        T.op("act", lambda: nc.scalar.activation(out=P_sb[pb][:, 0:ncols], in_=S_ps[sbuf][:, 0:ncols], func=AF.Exp, scale=SCALE),
             reads=[r_S[sbuf]], writes=[r_P[pb]])
        if tbl is not None:
            T.op("dve", lambda: nc.vector.tensor_tensor(out=P_sb[pb][:, 0:ncols], in0=P_sb[pb][:, 0:ncols], in1=tbl, op=ALU.mult),
                 reads=[rt], writes=[r_P[pb]])
        for s in range(nsub):
            T.op("pe", lambda s=s: nc.tensor.matmul(acc[s][:, 0:dvp], lhsT=P_sb[pb][:, s * 128:(s + 1) * 128], rhs=v,
                                                   start=(k == 0), stop=(k == n - 1)),
                 reads=[r_P[pb], rv], writes=[r_acc[s]], inc=(k == n - 1 or s == nsub - 1))
    for s in range(nsub):
        finish(s, acc[s], r_acc[s])


def attn_bufs(K, es, nacc=4):
    nc = K.nc
    A = {}
    A["S"] = [es.enter_context(K.pst("aS%d" % i, [128, 512], F32)) for i in range(3)]
    A["acc"] = [es.enter_context(K.pst("aacc%d" % i, [128, 512], F32)) for i in range(nacc)]
    A["P"] = [es.enter_context(K.sbt("aP%d" % i, [128, 512], BF16)) for i in range(3)]
    A["r_S"] = [Res(), Res(), Res()]
    A["r_P"] = [Res(), Res(), Res()]
    A["r_acc"] = [Res() for _ in range(nacc)]
    A["cnt"] = 0
    A["pcnt"] = 0
    A["rt"] = Res()
    return A


def phase_attn(K, l):
    nc, T, d, R = K.nc, K.T, K.d, K.R
    types = EVEN_TYPES if l % 2 == 0 else ODD_TYPES
    ftmap, vvmap = block_maps(types)
    FT, VV, OO = d["FT"], d["VV"], d["OO"]
    lat_tiles = list(range(2, NTT))

    def ftv(blk):
        return FT[ftmap[blk]].rearrange("p (t c) -> p t c", c=128)

    with ExitStack() as es:
        sb = lambda n, s, dt: es.enter_context(K.sbt(n, s, dt))
        A = attn_bufs(K, es)
        QA = sb("aQA", [128, NTT, 4, 128], BF16)
        KT = sb("aKT", [128, NT], BF16)
        VA = sb("aVA", [128, NTT, 129], BF16)
        Ost = [sb("aO%d" % i, [128, 512], BF16) for i in range(2)]
        rc = [sb("arc%d" % i, [128, 4], F32) for i in range(2)]
        esk = sb("aesk", [128, 8], F32)
        bm = sb("abm", [128, 2, 4, 128], BF16)
        r_es, r_O, r_rc = Res(), [Res(), Res()], [Res(), Res()]
        A["rq"], A["rk"], A["rv"] = Res(), Res(), Res()
        if l % 2 == 0:
            load_bc(K, esk[:], d["small"][0:1, 0:8], r_es)
            T.op("act", lambda: nc.scalar.activation(out=esk[:], in_=esk[:], func=AF.Exp), reads=[r_es], writes=[r_es])
            T.dma("sp", bm[:], d["bandmask"], writes=[A["rt"]])
        on = 0
        for g in range(2):
            for hh in range(4):
                T.dma("sp", QA[:, :, hh, :], ftv(g * 4 + hh), reads=[R("FT")], writes=[A["rq"]])
            T.dma("sp", KT[:], FT[ftmap[8 + g]], reads=[R("FT")], writes=[A["rk"]])
            T.op("dve", lambda: nc.vector.memset(VA[:, :, 128:129], 1.0), writes=[A["rv"]])
            T.dma("sp", VA[:, :, 0:128], VV[:, vvmap[10 + g] * 128:(vvmap[10 + g] + 1) * 128].rearrange("(t p) c -> p t c", p=128),
                  reads=[R("VV")], writes=[A["rv"]])
            qtiles = list(range(NTT)) if l % 2 == 0 else lat_tiles
            for i in qtiles:
                if l % 2 == 0:
                    if i < 2:
                        kl = [(0, None), (1, None)]
                    else:
                        kl = [(0, None), (1, None)]
                        if i - 1 >= 2:
                            kl.append((i - 1, 0))
                        kl.append((i, None))
                        if i + 1 < NTT:
                            kl.append((i + 1, 1))
                else:
                    kl = [(j, None) for j in range(NTT)]
                keys = [(KT[:, j * 128:(j + 1) * 128], VA[:, j, :],
                         None if m is None else bm[:, m, :, :].rearrange("p h q -> p (h q)")) for (j, m) in kl]
                ob = on % 2
                on += 1

                def finish(s, acc, r_acc, ob=ob, g=g):
                    if l % 2 == 0:
                        T.op("dve", lambda: nc.vector.tensor_scalar(out=rc[ob][:, s:s + 1], in0=acc[:, 128:129],
                                                                    scalar1=esk[:, g * 4 + s:g * 4 + s + 1], scalar2=None, op0=ALU.add),
                             reads=[r_acc, r_es], writes=[r_rc[ob]])
                        T.op("dve", lambda: nc.vector.reciprocal(out=rc[ob][:, s:s + 1], in_=rc[ob][:, s:s + 1]),
                             reads=[r_rc[ob]], writes=[r_rc[ob]])
                    else:
                        T.op("dve", lambda: nc.vector.reciprocal(out=rc[ob][:, s:s + 1], in_=acc[:, 128:129]),
                             reads=[r_acc], writes=[r_rc[ob]])
                    T.op("dve", lambda: nc.vector.tensor_scalar(out=Ost[ob][:, s * 128:(s + 1) * 128], in0=acc[:, 0:128],
                                                                scalar1=rc[ob][:, s:s + 1], scalar2=None, op0=ALU.mult),
                         reads=[r_acc, r_rc[ob]], writes=[r_O[ob]])
                attn_group(K, A, QA[:, i, :, :].rearrange("p h c -> p (h c)"), 512, keys, 129, finish)
                T.dma("sp", OO[i * 128:(i + 1) * 128, g * 512:(g + 1) * 512], Ost[ob][:], reads=[r_O[ob]], writes=[R("OO")])
    T.barrier()
    if l % 2 == 0:
        attn_B(K, l, ftmap, vvmap)
    else:
        attn_D(K, l, ftmap, vvmap)


def attn_B(K, l, ftmap, vvmap):
    nc, T, d, R = K.nc, K.T, K.d, K.R
    FT, VV, OO = d["FT"], d["VV"], d["OO"]
    lam_init = lambda_init_for(l)
    with ExitStack() as es:
        sb = lambda n, s, dt: es.enter_context(K.sbt(n, s, dt))
        A = attn_bufs(K, es)
        QT = [sb("bQ%d" % m, [128, NT], BF16) for m in range(2)]
        KT = [sb("bK%d" % m, [128, NT], BF16) for m in range(2)]
        VB = sb("bV", [128, NTT, 257], BF16)
        o1 = sb("bo1", [128, 4, 256], F32)
        o2 = sb("bo2", [128, 4, 256], F32)
        Ost = [sb("bO%d" % i, [128, 4, 256], BF16) for i in range(2)]
        rc = sb("brc", [128, 8], F32)
        lamv = sb("blam", [128, 4, 128], F32)
        lamt = sb("blamt", [128, 2, 128], F32)
        lams = sb("blams", [128, 4], F32)
        subw = sb("bsub", [128, 256], F32)
        sqj = sb("bsq", [128, 256], F32)
        r_q, r_k, r_v = [Res(), Res()], [Res(), Res()], Res()
        r_o, r_O, r_rc, r_lam, r_sub = Res(), [Res(), Res()], Res(), Res(), Res()
        load_bc(K, lamv[:].rearrange("p a b -> p (a b)"), d["small"][0:1, 8:520], r_lam)
        load_bc(K, subw[:], d["small"][0:1, 520:776], r_sub)
        T.op("dve", lambda: nc.vector.tensor_tensor(out=lamt[:], in0=lamv[:, 0:4:2, :], in1=lamv[:, 1:4:2, :], op=ALU.mult),
             reads=[r_lam], writes=[r_lam])
        T.op("dve", lambda: nc.vector.tensor_reduce(out=lams[:, 0:2], in_=lamt[:], axis=mybir.AxisListType.X, op=ALU.add),
             reads=[r_lam], writes=[r_lam])
        T.op("act", lambda: nc.scalar.activation(out=lams[:, 0:2], in_=lams[:, 0:2], func=AF.Exp), reads=[r_lam], writes=[r_lam])
        T.op("dve", lambda: nc.vector.tensor_tensor(out=lams[:, 2:3], in0=lams[:, 1:2], in1=lams[:, 0:1], op=ALU.subtract),
             reads=[r_lam], writes=[r_lam])
        T.op("dve", lambda: nc.vector.tensor_scalar(out=lams[:, 2:3], in0=lams[:, 2:3], scalar1=-lam_init, scalar2=None, op0=ALU.add),
             reads=[r_lam], writes=[r_lam])
        T.op("dve", lambda: nc.vector.tensor_scalar(out=subw[:], in0=subw[:], scalar1=1.0 - lam_init, scalar2=None, op0=ALU.mult),
             reads=[r_sub], writes=[r_sub])
        on = 0
        for hb in range(4):
            for m in range(2):
                T.dma("sp", QT[m][:], FT[ftmap[12 + hb * 2 + m]], reads=[R("FT")], writes=[r_q[m]])
                T.dma("sp", KT[m][:], FT[ftmap[20 + hb * 2 + m]], reads=[R("FT")], writes=[r_k[m]])
            T.op("dve", lambda: nc.vector.memset(VB[:, :, 256:257], 1.0), writes=[r_v])
            v0 = vvmap[28 + hb * 2]
            T.dma("sp", VB[:, :, 0:256], VV[:, v0 * 128:(v0 + 2) * 128].rearrange("(t p) c -> p t c", p=128),
                  reads=[R("VV")], writes=[r_v])
            chunks = [(0, 2, [0, 1])] + [(2 + 4 * c, 4, list(range(NTT))) for c in range(8)]
            for (t0, ntile, kts) in chunks:
                ob = on % 2
                on += 1
                for m in range(2):
                    A["rq"], A["rk"], A["rv"] = r_q[m], r_k[m], r_v
                    keys = [(KT[m][:, j * 128:(j + 1) * 128], VB[:, j, :], None) for j in kts]
                    dst = o1 if m == 0 else o2

                    def finish(s, acc, r_acc, m=m, dst=dst):
                        T.op("dve", lambda: nc.vector.reciprocal(out=rc[:, m * 4 + s:m * 4 + s + 1], in_=acc[:, 256:257]),
                             reads=[r_acc], writes=[r_rc])
                        T.op("dve", lambda: nc.vector.tensor_scalar(out=dst[:, s, :], in0=acc[:, 0:256],
                                                                    scalar1=rc[:, m * 4 + s:m * 4 + s + 1], scalar2=None, op0=ALU.mult),
                             reads=[r_acc, r_rc], writes=[r_o])
                    attn_group(K, A, QT[m][:, t0 * 128:(t0 + ntile) * 128], ntile * 128, keys, 257, finish)
                for s in range(ntile):
                    T.op("dve", lambda s=s: nc.vector.scalar_tensor_tensor(out=o1[:, s, :], in0=o2[:, s, :], scalar=lams[:, 2:3],
                                                                           in1=o1[:, s, :], op0=ALU.mult, op1=ALU.add),
                         reads=[r_o, r_lam], writes=[r_o])
                    T.op("act", lambda s=s: nc.scalar.activation(out=sqj[:], in_=o1[:, s, :], func=AF.Square, accum_out=rc[:, s:s + 1]),
                         reads=[r_o], writes=[r_rc])
                    T.op("act", lambda s=s: nc.scalar.activation(out=rc[:, s:s + 1], in_=rc[:, s:s + 1], func=AF.Sqrt,
                                                                 scale=1.0 / 256, bias=EPS),
                         reads=[r_rc], writes=[r_rc])
                    T.op("dve", lambda s=s: nc.vector.reciprocal(out=rc[:, s:s + 1], in_=rc[:, s:s + 1]), reads=[r_rc], writes=[r_rc])
                    T.op("dve", lambda s=s: nc.vector.scalar_tensor_tensor(out=Ost[ob][:, s, :], in0=o1[:, s, :], scalar=rc[:, s:s + 1],
                                                                           in1=subw[:], op0=ALU.mult, op1=ALU.mult),
                         reads=[r_o, r_rc, r_sub], writes=[r_O[ob]])
                T.dma("sp", OO[t0 * 128:(t0 + ntile) * 128, 1024 + hb * 256:1024 + (hb + 1) * 256].rearrange("(t p) c -> p t c", p=128),
                      Ost[ob][:, 0:ntile, :], reads=[r_O[ob]], writes=[R("OO")])


def attn_D(K, l, ftmap, vvmap):
    nc, T, d, R = K.nc, K.T, K.d, K.R
    FT, VV, OO = d["FT"], d["VV"], d["OO"]
    sets = d_tile_sets()
    with ExitStack() as es:
        sb = lambda n, s, dt: es.enter_context(K.sbt(n, s, dt))
        A = attn_bufs(K, es, nacc=1)
        QT = sb("dQ", [128, NT], BF16)
        KT = sb("dK", [128, NT], BF16)
        VD = sb("dV", [128, NTT, 129], BF16)
        raw = sb("draw", [128, 21, 128], F32)
        tab = sb("dtab", [128, 21, 128], BF16)
        Ost = [sb("dO%d" % i, [128, 128], BF16) for i in range(2)]
        rc = sb("drc", [128, 2], F32)
        r_raw, r_O, r_rc = Res(), [Res(), Res()], Res()
        A["rq"], A["rk"], A["rv"] = Res(), Res(), Res()
        on = 0
        for h in range(8):
            T.dma("sp", raw[:], d["rpbtab"][:, h, :, :], writes=[r_raw])
            T.op("act", lambda: nc.scalar.activation(out=tab[:], in_=raw[:], func=AF.Exp), reads=[r_raw], writes=[A["rt"]])
            T.dma("sp", QT[:], FT[ftmap[12 + h]], reads=[R("FT")], writes=[A["rq"]])
            T.dma("sp", KT[:], FT[ftmap[20 + h]], reads=[R("FT")], writes=[A["rk"]])
            T.op("dve", lambda: nc.vector.memset(VD[:, :, 128:129], 1.0), writes=[A["rv"]])
            v0 = vvmap[28 + h]
            T.dma("sp", VD[:, :, 0:128], VV[:, v0 * 128:(v0 + 1) * 128].rearrange("(t p) c -> p t c", p=128),
                  reads=[R("VV")], writes=[A["rv"]])
            for i in range(32):
                js, ids = sets[i]
                keys = [(KT[:, j * 128:(j + 1) * 128], VD[:, j, :], None) for j in (0, 1)]
                keys += [(KT[:, (2 + j) * 128:(3 + j) * 128], VD[:, 2 + j, :], tab[:, tid, :]) for j, tid in zip(js, ids)]
                ob = on % 2
                on += 1

                def finish(s, acc, r_acc, ob=ob):
                    T.op("dve", lambda: nc.vector.reciprocal(out=rc[:, 0:1], in_=acc[:, 128:129]), reads=[r_acc], writes=[r_rc])
                    T.op("dve", lambda: nc.vector.tensor_scalar(out=Ost[ob][:], in0=acc[:, 0:128], scalar1=rc[:, 0:1],
                                                                scalar2=None, op0=ALU.mult),
                         reads=[r_acc, r_rc], writes=[r_O[ob]])
                attn_group(K, A, QT[:, (2 + i) * 128:(3 + i) * 128], 128, keys, 129, finish)
                T.dma("sp", OO[(2 + i) * 128:(3 + i) * 128, 1024 + h * 128:1024 + (h + 1) * 128], Ost[ob][:],
                      reads=[r_O[ob]], writes=[R("OO")])


def phase_outproj(K, l):
    nc, T, d, R = K.nc, K.T, K.d, K.R
    first_tt = 0 if l == 0 else 2
    with ExitStack() as es:
        sb = lambda n, s, dt: es.enter_context(K.sbt(n, s, dt))
        W = sb("oW", [128, 16, D], BF16)
        gt = [sb("ogt%d" % i, [128, D], F32) for i in range(2)]
        Ot = [sb("oO%d" % i, [128, D], BF16) for i in range(2)]
        OT = [sb("oOT%d" % i, [128, 16, 128], BF16) for i in range(2)]
        xt = [sb("oxt%d" % i, [128, D], F32) for i in range(2)]
        xn = [sb("oxn%d" % i, [128, D], F32) for i in range(2)]
        tp = [es.enter_context(K.pst("otp%d" % i, [128, 1024], BF16)) for i in range(2)]
        ps = [es.enter_context(K.pst("ops%d" % i, [128, 512], F32)) for i in range(2)]
        r_W, r_gt, r_O, r_OT, r_x, r_xn = Res(), Res(), [Res(), Res()], [Res(), Res()], [Res(), Res()], [Res(), Res()]
        r_tp, r_ps = [Res(), Res()], [Res(), Res()]
        wv = d["w_out"][l].rearrange("(kc p) n -> p kc n", p=128)
        for nb in range(4):
            T.dma("pool", W[:, :, nb * 512:(nb + 1) * 512], wv[:, :, nb * 512:(nb + 1) * 512], writes=[r_W])
        for w in range(2):
            load_bc(K, gt[w][:], d["modv"][l, w:w + 1, 2 * D:3 * D], r_gt, reads=[R("modv")])
        tiles = list(range(first_tt, NTT))

        def load(tt, b):
            T.dma("sp", Ot[b][:], d["OO"][tt * 128:(tt + 1) * 128, :], reads=[R("OO")], writes=[r_O[b]])
            load_x_tile(K, l, tt, xt[b], r_x[b])
        load(tiles[0], 0)
        pn = [0]

        def do_tr(n):
            b = n % 2
            for g in range(2):
                for k8 in range(8):
                    kc = g * 8 + k8
                    T.op("pe", lambda kc=kc, k8=k8, g=g: nc.tensor.transpose(out=tp[g][:, k8 * 128:(k8 + 1) * 128],
                                                                           in_=Ot[b][:, kc * 128:(kc + 1) * 128],
                                                                           identity=K.ident_bf[:]),
                         reads=[r_O[b], R("ident")], writes=[r_tp[g]], inc=(k8 == 7))
                T.op("act", lambda g=g: nc.scalar.copy(out=OT[b][:, g * 8:(g + 1) * 8, :].rearrange("p a b -> p (a b)"), in_=tp[g][:]),
                     reads=[r_tp[g]], writes=[r_OT[b]])

        def do_mm(n, tt):
            b = n % 2
            w = 1 if tt < 2 else 0
            for nb in range(4):
                pb = pn[0] % 2
                pn[0] += 1
                for kc in range(16):
                    T.op("pe", lambda kc=kc: nc.tensor.matmul(ps[pb][:], lhsT=OT[b][:, kc, :], rhs=W[:, kc, nb * 512:(nb + 1) * 512],
                                                             start=(kc == 0), stop=(kc == 15)),
                         reads=[r_OT[b], r_W], writes=[r_ps[pb]], inc=(kc == 15))
                T.op("dve", lambda: nc.vector.tensor_tensor(out=xn[b][:, nb * 512:(nb + 1) * 512], in0=ps[pb][:],
                                                            in1=gt[w][:, nb * 512:(nb + 1) * 512], op=ALU.mult),
                     reads=[r_ps[pb], r_gt], writes=[r_xn[b]])
                T.op("dve", lambda: nc.vector.tensor_tensor(out=xn[b][:, nb * 512:(nb + 1) * 512], in0=xn[b][:, nb * 512:(nb + 1) * 512],
                                                            in1=xt[b][:, nb * 512:(nb + 1) * 512], op=ALU.add),
                     reads=[r_xn[b], r_x[b]], writes=[r_xn[b]])
                T.dma("sp", d["XS%d_%d" % (l, nb)][tt * 128:(tt + 1) * 128, :], xn[b][:, nb * 512:(nb + 1) * 512],
                      reads=[r_xn[b]], writes=[R("XS%d_%d" % (l, nb))])
        do_tr(0)
        for n, tt in enumerate(tiles):
            if n + 1 < len(tiles):
                load(tiles[n + 1], (n + 1) % 2)
                do_tr(n + 1)
            do_mm(n, tt)


def phase_route(K, l):
    nc, T, d, R = K.nc, K.T, K.d, K.R
    sets = [(2, NTT, CAP)] + ([(0, 2, CAPX)] if l == 0 else [])
    with ExitStack() as es:
        sb = lambda n, s, dt: es.enter_context(K.sbt(n, s, dt))
        affT = sb("raffT", [NE, NT], F32)
        maskT = sb("rmaskT", [NE, NT], BF16)
        junk = sb("rjunk", [NE, S], BF16)
        bs = sb("rbs", [NE, 4], F32)
        mask = sb("rmask", [128, NTT, NE], BF16)
        slot = sb("rslot", [128, NTT, NE], F32)
        tmp = sb("rtmp", [128, NTT, NE], F32)
        Rt = sb("rR", [128, NTT, NE, 5], BF16)
        g1 = sb("rg1", [128, NTT, NE], BF16)
        r1 = sb("rr1", [128, NTT, NE], F32)
        tri = sb("rtri", [128, 256], BF16)
        iota = sb("riota", [128, 512], F32)
        tokab = sb("rtok", [128, NTT, 2], BF16)
        OH = [sb("rOH%d" % i, [128, 512], BF16) for i in range(3)]
        accs = sb("raccs", [128, 4, 5], F32)
        acc5 = sb("racc5", [8, 512], F32)
        r_a5 = Res()
        idf = sb("ridf", [128, 4], F32)
        tp = es.enter_context(K.pst("rtp", [128, 512], F32))
        tpb = es.enter_context(K.pst("rtpb", [128, 1024], BF16))
        pp = [es.enter_context(K.pst("rpp%d" % i, [128, 512], F32)) for i in range(2)]
        ap_ = [es.enter_context(K.pst("rap%d" % i, [128, 512], F32)) for i in range(2)]
        r_affT, r_maskT, r_bs, r_mask, r_slot, r_R, r_c, r_tp, r_tpb = Res(), Res(), Res(), Res(), Res(), Res(), Res(), Res(), Res()
        r_pp, r_ap, r_OH, r_acc, r_j = [Res(), Res()], [Res(), Res()], [Res(), Res(), Res()], Res(), Res()
        T.dma("sp", tri[:], d["tri"], writes=[r_c])
        T.dma("sp", iota[:], d["iota"], writes=[r_c])
        T.dma("sp", tokab[:], d["tokab"], writes=[r_c])
        r_aff = K.R("aff")
        tiles_all = list(range(0 if l == 0 else 2, NTT))
        for tt in tiles_all:
            T.op("pe", lambda tt=tt: nc.tensor.transpose(out=tp[0:NE, 0:128], in_=K.aff[:, tt, :], identity=K.ident_f[:]),
                 reads=[r_aff, R("ident")], writes=[r_tp])
            T.op("dve", lambda tt=tt: nc.vector.tensor_copy(out=affT[:, tt * 128:(tt + 1) * 128], in_=tp[0:NE, 0:128]),
                 reads=[r_tp], writes=[r_affT])
        for (ta, tb_, cap) in sets:
            c0, c1 = ta * 128, tb_ * 128
            T.op("dve", lambda: nc.vector.memset(bs[:, 0:1], 0.0), writes=[r_bs])
            for it in range(30):
                wk = 2.0 ** -(it + 1)
                T.op("dve", lambda wk=wk: nc.vector.tensor_scalar(out=bs[:, 1:2], in0=bs[:, 0:1], scalar1=wk, scalar2=None, op0=ALU.add),
                     reads=[r_bs], writes=[r_bs])
                T.op("dve", lambda: nc.vector.tensor_scalar(out=junk[:, 0:c1 - c0], in0=affT[:, c0:c1], scalar1=bs[:, 1:2], scalar2=0.0,
                                                            op0=ALU.is_ge, op1=ALU.add, accum_out=bs[:, 2:3]),
                     reads=[r_bs, r_affT], writes=[r_bs, r_j])
                T.op("dve", lambda: nc.vector.tensor_scalar(out=bs[:, 3:4], in0=bs[:, 2:3], scalar1=float(cap) - 0.5, scalar2=None,
                                                            op0=ALU.is_ge),
                     reads=[r_bs], writes=[r_bs])
                T.op("dve", lambda wk=wk: nc.vector.scalar_tensor_tensor(out=bs[:, 0:1], in0=bs[:, 3:4], scalar=wk, in1=bs[:, 0:1],
                                                                         op0=ALU.mult, op1=ALU.add),
                     reads=[r_bs], writes=[r_bs])
            T.op("dve", lambda: nc.vector.tensor_scalar(out=maskT[:, c0:c1], in0=affT[:, c0:c1], scalar1=bs[:, 0:1], scalar2=None,
                                                        op0=ALU.is_ge),
                 reads=[r_bs, r_affT], writes=[r_maskT])
        for tt in tiles_all:
            T.op("pe", lambda tt=tt: nc.tensor.transpose(out=tpb[:, 0:NE], in_=maskT[:, tt * 128:(tt + 1) * 128],
                                                         identity=K.ident_bf[0:NE, 0:NE]),
                 reads=[r_maskT, R("ident")], writes=[r_tpb])
            T.op("dve", lambda tt=tt: nc.vector.tensor_copy(out=mask[:, tt, :], in_=tpb[:, 0:NE]), reads=[r_tpb], writes=[r_mask])
        pn = 0
        for (ta, tb_, cap) in sets:
            for j in range(ta, tb_):
                pb = pn % 2
                pn += 1
                prev = list(range(ta, j))
                for ii, i in enumerate(prev):
                    T.op("pe", lambda i=i, ii=ii: nc.tensor.matmul(pp[pb][:, 0:NE], lhsT=tri[:, 0:128], rhs=mask[:, i, :],
                                                                  start=(ii == 0), stop=False),
                         reads=[r_mask, r_c], writes=[r_pp[pb]], inc=False)
                T.op("pe", lambda j=j: nc.tensor.matmul(pp[pb][:, 0:NE], lhsT=tri[:, 128:256], rhs=mask[:, j, :],
                                                       start=(len(prev) == 0), stop=True),
                     reads=[r_mask, r_c], writes=[r_pp[pb]])
                T.op("dve", lambda j=j: nc.vector.tensor_scalar(out=tmp[:, j, :], in0=mask[:, j, :], scalar1=-1.0, scalar2=10000.0,
                                                                op0=ALU.add, op1=ALU.mult),
                     reads=[r_mask], writes=[r_slot])
                T.op("dve", lambda j=j: nc.vector.tensor_tensor(out=slot[:, j, :], in0=tmp[:, j, :], in1=pp[pb][:, 0:NE], op=ALU.add),
                     reads=[r_pp[pb], r_slot], writes=[r_slot])
        T.op("dve", lambda: nc.vector.tensor_copy(out=Rt[:, :, :, 0:2], in_=tokab[:].unsqueeze(2).to_broadcast([128, NTT, NE, 2])),
             reads=[r_c], writes=[r_R])
        T.op("dve", lambda: nc.vector.tensor_copy(out=g1[:], in_=K.aff[:]), reads=[r_aff], writes=[r_R])
        T.op("dve", lambda: nc.vector.tensor_copy(out=Rt[:, :, :, 2], in_=g1[:]), reads=[r_R], writes=[r_R])
        T.op("dve", lambda: nc.vector.tensor_tensor(out=r1[:], in0=K.aff[:], in1=g1[:], op=ALU.subtract), reads=[r_aff, r_R], writes=[r_R])
        T.op("dve", lambda: nc.vector.tensor_copy(out=g1[:], in_=r1[:]), reads=[r_R], writes=[r_R])
        T.op("dve", lambda: nc.vector.tensor_copy(out=Rt[:, :, :, 3], in_=g1[:]), reads=[r_R], writes=[r_R])
        T.op("dve", lambda: nc.vector.tensor_tensor(out=r1[:], in0=r1[:], in1=g1[:], op=ALU.subtract), reads=[r_R], writes=[r_R])
        T.op("dve", lambda: nc.vector.tensor_copy(out=Rt[:, :, :, 4], in_=r1[:]), reads=[r_R], writes=[r_R])
        on = 0
        an = 0
        for (ta, tb_, cap) in sets:
            nsc = (cap + 127) // 128
            wid = min(cap, 512)
            for e in range(NE):
                ab = an % 2
                an += 1
                for j in range(ta, tb_):
                    ob = on % 3
                    on += 1
                    T.op("dve", lambda j=j, e=e: nc.vector.tensor_scalar(out=OH[ob][:, 0:wid], in0=iota[:, 0:wid],
                                                                          scalar1=slot[:, j, e:e + 1], scalar2=None, op0=ALU.is_equal),
                         reads=[r_slot, r_c], writes=[r_OH[ob]])
                    T.op("pe", lambda j=j, e=e: nc.tensor.matmul(ap_[ab][0:5, 0:wid], lhsT=Rt[:, j, e, :], rhs=OH[ob][:, 0:wid],
                                                                 start=(j == ta), stop=(j == tb_ - 1)),
                         reads=[r_OH[ob], r_R], writes=[r_ap[ab]], inc=True)
                T.op("dve", lambda: nc.vector.tensor_copy(out=acc5[0:5, 0:wid], in_=ap_[ab][0:5, 0:wid]), reads=[r_ap[ab]], writes=[r_a5])
                for sc in range(nsc):
                    m = min(128, cap - sc * 128)
                    T.op("pe", lambda sc=sc, m=m: nc.tensor.transpose(out=tp[0:m, sc * 8:sc * 8 + 5], in_=acc5[0:5, sc * 128:sc * 128 + m],
                                                                      identity=K.ident_f[0:5, 0:5]),
                         reads=[r_a5, R("ident")], writes=[r_tp])
                m = min(128, cap)
                av = tp[0:m, 0:32].rearrange("p (s c) -> p s c", c=8)
                T.op("dve", lambda: nc.vector.tensor_copy(out=accs[0:m, 0:nsc, :], in_=av[:, 0:nsc, 0:5]), reads=[r_tp], writes=[r_acc])
                T.op("dve", lambda: nc.vector.scalar_tensor_tensor(out=idf[0:m, 0:nsc], in0=accs[0:m, 0:nsc, 0], scalar=64.0,
                                                                   in1=accs[0:m, 0:nsc, 1], op0=ALU.mult, op1=ALU.add),
                     reads=[r_acc], writes=[r_acc])
                if cap == CAP:
                    idst, gdst = K.idx[:, e, :], K.gate[:, e, :]
                else:
                    idst, gdst = K.idxx[0:m, e:e + 1], K.gatex[0:m, e:e + 1]
                T.op("dve", lambda: nc.vector.tensor_copy(out=idst, in_=idf[0:m, 0:nsc]), reads=[r_acc], writes=[K.R("idx")])
                T.op("dve", lambda: nc.vector.tensor_tensor(out=gdst, in0=accs[0:m, 0:nsc, 2], in1=accs[0:m, 0:nsc, 3], op=ALU.add),
                     reads=[r_acc], writes=[K.R("idx")])
                T.op("dve", lambda: nc.vector.tensor_tensor(out=gdst, in0=gdst, in1=accs[0:m, 0:nsc, 4], op=ALU.add),
                     reads=[r_acc, K.R("idx")], writes=[K.R("idx")])


def phase_moe(K, l):
    nc, T, d, R = K.nc, K.T, K.d, K.R
    has_ctx = (l == 0)
    NS = CAP + (CAPX if has_ctx else 0)
    experts = K.dbg.get("experts", list(range(NE)))
    with ExitStack() as es:
        sb = lambda n, s, dt: es.enter_context(K.sbt(n, s, dt))
        NR = 6
        ring = [sb("mring%d" % i, [128, 16, 512], BF16) for i in range(NR)]
        xe = sb("mxe", [128, 4, D], BF16)
        xex = sb("mxex", [128, D], BF16)
        xeT = sb("mxeT", [128, 16, CAP + CAPX], BF16)
        hidT = sb("mhidT", [128, 12, CAP + CAPX], BF16)
        gt = [sb("mgt%d" % i, [128, D], F32) for i in range(2)]
        s1 = [sb("ms1%d" % i, [128, 512], F32) for i in range(2)]
        s1x = sb("ms1x", [128, 32], F32)
        yst = [sb("myst%d" % i, [128, 512], F32) for i in range(4)]
        GU = [es.enter_context(K.pst("mGU%d" % i, [128, 512], F32)) for i in range(4)]
        GX = es.enter_context(K.pst("mGX", [128, 512], F32))
        tp = [es.enter_context(K.pst("mtp%d" % i, [128, 1024], BF16)) for i in range(2)]
        r_ring = [Res() for _ in range(NR)]
        r_xe, r_xeT, r_hid, r_gt = Res(), Res(), Res(), Res()
        r_s1, r_s1x, r_y = [Res(), Res()], Res(), [Res() for _ in range(4)]
        r_GU, r_GX, r_tp = [Res() for _ in range(4)], Res(), [Res(), Res()]
        r_idx = K.R("idx")
        for w in range(2):
            load_bc(K, gt[w][:], d["modv"][l, w:w + 1, 5 * D:6 * D], r_gt, reads=[R("modv")])
        pieces = []
        for e in experts:
            for fb in range(3):
                pieces.append(("g", e, fb))
                pieces.append(("u", e, fb))
            for nb in range(4):
                pieces.append(("d", e, nb))
        pstate = dict(next=0)

        def issue_piece():
            i = pstate["next"]
            if i >= len(pieces):
                return
            kind, e, j = pieces[i]
            slot = i % NR
            if kind == "d":
                src = d["w_down"][l, e].rearrange("(fc p) n -> p fc n", p=128)[:, :, j * 512:(j + 1) * 512]
                T.dma("pool", ring[slot][:, 0:12, :], src, writes=[r_ring[slot]])
            else:
                wn = "w_gate" if kind == "g" else "w_up"
                src = d[wn][l, e].rearrange("(kc p) n -> p kc n", p=128)[:, :, j * 512:(j + 1) * 512]
                T.dma("pool", ring[slot][:], src, writes=[r_ring[slot]])
            pstate["next"] = i + 1

        def gather(e):
            for sc in range(4):
                T.dma("pool", xe[:, sc, :], d["H2"], reads=[R("H2"), r_idx], writes=[r_xe],
                      indirect=dict(out_offset=None, in_offset=bass.IndirectOffsetOnAxis(ap=K.idx[:, e, sc:sc + 1], axis=0)))
            if has_ctx:
                T.dma("pool", xex[0:CAPX, :], d["H2"], reads=[R("H2"), r_idx], writes=[r_xe],
                      indirect=dict(out_offset=None, in_offset=bass.IndirectOffsetOnAxis(ap=K.idxx[0:CAPX, e:e + 1], axis=0)))

        def transposes(e):
            tn = 0
            for sc in range(4):
                for g in range(2):
                    tb = tn % 2
                    tn += 1
                    for k8 in range(8):
                        kc = g * 8 + k8
                        T.op("pe", lambda kc=kc, k8=k8: nc.tensor.transpose(out=tp[tb][:, k8 * 128:(k8 + 1) * 128],
                                                                          in_=xe[:, sc, kc * 128:(kc + 1) * 128], identity=K.ident_bf[:]),
                             reads=[r_xe, R("ident")], writes=[r_tp[tb]], inc=(k8 == 7))
                    eng = "act" if g == 0 else "dve"
                    src = tp[tb][:].rearrange("p (a b) -> p a b", b=128)
                    dst = xeT[:, g * 8:(g + 1) * 8, sc * 128:(sc + 1) * 128]
                    if eng == "act":
                        T.op("act", lambda: nc.scalar.copy(out=dst, in_=src), reads=[r_tp[tb]], writes=[r_xeT])
                    else:
                        T.op("dve", lambda: nc.vector.tensor_copy(out=dst, in_=src), reads=[r_tp[tb]], writes=[r_xeT])
            if has_ctx:
                tb = tn % 2
                for kc in range(16):
                    T.op("pe", lambda kc=kc: nc.tensor.transpose(out=tp[tb][:, kc * 32:(kc + 1) * 32],
                                                                 in_=xex[0:CAPX, kc * 128:(kc + 1) * 128],
                                                                 identity=K.ident_bf[0:CAPX, 0:CAPX]),
                         reads=[r_xe, R("ident")], writes=[r_tp[tb]], inc=(kc == 15))
                T.op("dve", lambda: nc.vector.tensor_copy(out=xeT[:, :, CAP:CAP + CAPX],
                                                          in_=tp[tb][:, 0:512].rearrange("p (a b) -> p a b", b=32)),
                     reads=[r_tp[tb]], writes=[r_xeT])

        def ensure(upto):
            while pstate["next"] <= min(upto, len(pieces) - 1):
                issue_piece()
        pi = 0
        gn = 0
        yn = 0
        prev_sc = [[] for _ in range(4)]
        gather(experts[0])
        transposes(experts[0])
        for ei, e in enumerate(experts):
            if ei + 1 < len(experts):
                gather(experts[ei + 1])
            for fb in range(3):
                ensure(pi + NR - 1)
                sg, su = pi % NR, (pi + 1) % NR
                pi += 2
                for f4 in range(4):
                    fc = fb * 4 + f4
                    gb = gn % 2
                    gn += 1
                    G, U = GU[gb], GU[2 + gb]
                    for (ps_, slot_, rr) in ((G, sg, r_GU[gb]), (U, su, r_GU[2 + gb])):
                        for kc in range(16):
                            T.op("pe", lambda kc=kc, ps_=ps_, slot_=slot_: nc.tensor.matmul(
                                ps_[:, 0:CAP], lhsT=ring[slot_][:, kc, f4 * 128:(f4 + 1) * 128], rhs=xeT[:, kc, 0:CAP],
                                start=(kc == 0), stop=(kc == 15)),
                                reads=[r_ring[slot_], r_xeT], writes=[rr], inc=(kc == 15))
                    if has_ctx:
                        for ci, slot_ in enumerate((sg, su)):
                            for kc in range(16):
                                T.op("pe", lambda kc=kc, ci=ci, slot_=slot_: nc.tensor.matmul(
                                    GX[:, ci * 32:(ci + 1) * 32], lhsT=ring[slot_][:, kc, f4 * 128:(f4 + 1) * 128],
                                    rhs=xeT[:, kc, CAP:CAP + CAPX], start=(kc == 0), stop=(kc == 15)),
                                    reads=[r_ring[slot_], r_xeT], writes=[r_GX], inc=(kc == 15))
                    sbuf_ = gn % 2
                    T.op("act", lambda: nc.scalar.activation(out=s1[sbuf_][:], in_=G[:, 0:CAP], func=AF.Silu),
                         reads=[r_GU[gb]], writes=[r_s1[sbuf_]])
                    T.op("dve", lambda: nc.vector.tensor_tensor(out=hidT[:, fc, 0:CAP], in0=s1[sbuf_][:], in1=U[:, 0:CAP], op=ALU.mult),
                         reads=[r_s1[sbuf_], r_GU[2 + gb]], writes=[r_hid])
                    if has_ctx:
                        T.op("act", lambda: nc.scalar.activation(out=s1x[:], in_=GX[:, 0:32], func=AF.Silu),
                             reads=[r_GX], writes=[r_s1x])
                        T.op("dve", lambda: nc.vector.tensor_tensor(out=hidT[:, fc, CAP:CAP + CAPX], in0=s1x[:], in1=GX[:, 32:64], op=ALU.mult),
                             reads=[r_s1x, r_GX], writes=[r_hid])
            if ei + 1 < len(experts):
                transposes(experts[ei + 1])
            new_sc = [[] for _ in range(4)]
            for nb in range(4):
                ensure(pi + NR - 1)
                sd = pi % NR
                pi += 1
                for st_ in range(5 if has_ctx else 4):
                    m = 128 if st_ < 4 else CAPX
                    yb = yn % 4
                    yn += 1
                    Y = GU[yb]
                    for fc in range(12):
                        T.op("pe", lambda fc=fc: nc.tensor.matmul(Y[0:m, :], lhsT=hidT[:, fc, st_ * 128:st_ * 128 + m],
                                                                 rhs=ring[sd][:, fc, :], start=(fc == 0), stop=(fc == 11)),
                             reads=[r_hid, r_ring[sd]], writes=[r_GU[yb]], inc=(fc == 11))
                    if st_ < 4:
                        gsc, gtt, iap = K.gate[:, e, st_:st_ + 1], gt[0], K.idx[:, e, st_:st_ + 1]
                    else:
                        gsc, gtt, iap = K.gatex[0:m, e:e + 1], gt[1], K.idxx[0:m, e:e + 1]
                    T.op("dve", lambda: nc.vector.scalar_tensor_tensor(out=yst[yb][0:m, :], in0=Y[0:m, :], scalar=gsc,
                                                                       in1=gtt[0:m, nb * 512:(nb + 1) * 512], op0=ALU.mult, op1=ALU.mult),
                         reads=[r_GU[yb], r_idx, r_gt], writes=[r_y[yb]])
                    xsn = "XS%d_%d" % (l, nb)
                    tok = T.dma("pool", d[xsn], yst[yb][0:m, :], reads=[r_y[yb], r_idx, R(xsn)], writes=[],
                                extra_waits=prev_sc[nb],
                                indirect=dict(out_offset=bass.IndirectOffsetOnAxis(ap=iap, axis=0), in_offset=None,
                                              compute_op=ALU.add))
                    new_sc[nb].append(tok)
            prev_sc = new_sc
        for nb in range(4):
            rr = R("XS%d_%d" % (l, nb))
            for tok in prev_sc[nb]:
                rr.r[tok[0]] = tok


def phase_final(K):
    nc, T, d, R = K.nc, K.T, K.d, K.R
    l = 1
    with ExitStack() as es:
        sb = lambda n, s, dt: es.enter_context(K.sbt(n, s, dt))
        gb = sb("fgb", [128, D], F32)
        xt = [sb("fxt%d" % i, [128, D], F32) for i in range(2)]
        yo = [sb("fyo%d" % i, [128, D], F32) for i in range(2)]
        junk = sb("fjunk", [128, D], BF16)
        st = sb("fst", [128, 4], F32)
        r_g, r_x, r_y, r_st, r_j = Res(), [Res(), Res()], [Res(), Res()], Res(), Res()
        load_bc(K, gb[:], d["gvec"][4:5, :], r_g)
        tiles = list(range(2, NTT))

        def load(tt, b):
            for nb in range(4):
                T.dma("sp", xt[b][:, nb * 512:(nb + 1) * 512], d["XS%d_%d" % (l, nb)][tt * 128:(tt + 1) * 128, :],
                      reads=[R("XS%d_%d" % (l, nb))], writes=[r_x[b]])
        load(tiles[0], 0)
        for n, tt in enumerate(tiles):
            b = n % 2
            if n + 1 < len(tiles):
                load(tiles[n + 1], (n + 1) % 2)
            T.op("act", lambda: nc.scalar.activation(out=junk[:], in_=xt[b][:], func=AF.Square, accum_out=st[:, 0:1]),
                 reads=[r_x[b]], writes=[r_j, r_st])
            T.op("act", lambda: nc.scalar.activation(out=st[:, 1:2], in_=st[:, 0:1], func=AF.Sqrt, scale=1.0 / D, bias=EPS),
                 reads=[r_st], writes=[r_st])
            T.op("dve", lambda: nc.vector.reciprocal(out=st[:, 2:3], in_=st[:, 1:2]), reads=[r_st], writes=[r_st])
            T.op("dve", lambda: nc.vector.scalar_tensor_tensor(out=yo[b][:], in0=xt[b][:], scalar=st[:, 2:3], in1=gb[:],
                                                               op0=ALU.mult, op1=ALU.mult),
                 reads=[r_x[b], r_st, r_g], writes=[r_y[b]])
            T.dma("sp", d["out"][(tt - 2) * 128:(tt - 1) * 128, :], yo[b][:], reads=[r_y[b]], writes=[R("out")])


def make_in_maps(inputs, cores):
    c = const_pack()
    f = lambda a: np.ascontiguousarray(np.asarray(a, dtype=np.float32))
    small = np.zeros((1, 1024), np.float32)
    small[0, 0:8] = f(inputs["a_sink"])[0]
    small[0, 8:136] = f(inputs["b_lam_q1"])[0]
    small[0, 136:264] = f(inputs["b_lam_k1"])[0]
    small[0, 264:392] = f(inputs["b_lam_q2"])[0]
    small[0, 392:520] = f(inputs["b_lam_k2"])[0]
    small[0, 520:776] = f(inputs["b_subln"])[0]
    small2 = np.concatenate([f(inputs["c_q_norm"])[0], f(inputs["c_k_norm"])[0]])[None, :]
    gvec = np.stack([f(inputs["g_mix"])[0], f(inputs["g_mix"])[1], f(inputs["g_ffn"])[0], f(inputs["g_ffn"])[1],
                     f(inputs["g_final"])], axis=0)
    rpbtab = d_tables(f(inputs["d_rpb"])[0])
    shared = dict(
        w_ada=f(inputs["w_ada"]), b_ada=f(inputs["b_ada"]), gvec=np.ascontiguousarray(gvec), w_in=f(inputs["w_in"]),
        w_out=f(inputs["w_out"]), w_router=f(inputs["w_router"]), w_gate=f(inputs["w_gate"]), w_up=f(inputs["w_up"]),
        w_down=f(inputs["w_down"]), small=small, small2=np.ascontiguousarray(small2), rpbtab=rpbtab, **c)
    maps = []
    x = inputs["x"]
    ctx = inputs["ctx"]
    cc = f(inputs["c"])
    c_ctx = f(inputs["c_ctx"])
    for b in cores:
        m = dict(shared)
        m["x"] = f(x[b])
        m["ctx"] = f(ctx[b])
        m["cvec"] = np.ascontiguousarray(np.stack([cc[b], c_ctx], axis=0))
        maps.append(m)
    return maps


def kernel(**inputs):
    nc = build_program()
    maps = make_in_maps(inputs, list(range(8)))
    res = run_bass_kernel_spmd(nc, maps, core_ids=list(range(8)))
    out = np.stack([np.asarray(r["out"], dtype=np.float32) for r in res.results], axis=0)
    return out
```

```python
import numpy as np
import ml_dtypes
from contextlib import ExitStack
import concourse.bass as bass
import concourse.mybir as mybir
from concourse.bass_utils import run_bass_kernel_spmd

F32 = mybir.dt.float32
BF16 = mybir.dt.bfloat16
I32 = mybir.dt.int32
AF = mybir.ActivationFunctionType
ALU = mybir.AluOpType

D = 2048
S = 4096
CTX = 256
NT = S + CTX
NTT = NT // 128
INW = 4608
NE = 16
DE = 1536
CAP = 512
CAPX = 32
HD = 128
EPS = 1e-6
SCALE = HD ** -0.5
GRID_W = 64
NBF16 = ml_dtypes.bfloat16


def lambda_init_for(layer):
    import math
    return 0.8 - 0.6 * math.exp(-0.3 * layer)


class Res:
    __slots__ = ("w", "r")

    def __init__(self):
        self.w = None
        self.r = {}


class Trk:
    def __init__(self, nc, es):
        self.nc = nc
        self.eng = {}
        for name, e in (("pe", nc.tensor), ("act", nc.scalar), ("dve", nc.vector),
                        ("pool", nc.gpsimd), ("sp", nc.sync)):
            sem = es.enter_context(nc.semaphore("s_" + name)) if name != "sp" else None
            self.eng[name] = dict(e=e, sem=sem, n=0, waited={}, name=name)
        self.dp = {}
        for q, n in (("sp", 28), ("pool", 28), ("act", 6)):
            self.dp[q] = dict(sems=[es.enter_context(nc.semaphore("d_%s_%d" % (q, i))) for i in range(n)],
                              cnt=[0] * n, nxt=0)

    def _wait(self, en, tok):
        if tok is None:
            return
        key, sem, val = tok
        E = self.eng[en]
        if en == "pe" and key == "pe":
            return
        if E["waited"].get(key, 0) >= val:
            return
        E["e"].wait_ge(sem, val)
        E["waited"][key] = val

    def _deps(self, en, reads, writes):
        for r in reads:
            self._wait(en, r.w)
        for w in writes:
            self._wait(en, w.w)
            for t in list(w.r.values()):
                self._wait(en, t)

    def _upd(self, tok, reads, writes):
        for r in reads:
            old = r.r.get(tok[0])
            if old is None or old[2] < tok[2]:
                r.r[tok[0]] = tok
        for w in writes:
            w.w = tok
            w.r = {}

    def op(self, en, fn, reads=(), writes=(), inc=True):
        E = self.eng[en]
        self._deps(en, reads, writes)
        ins = fn()
        if inc:
            E["n"] += 1
            ins.then_inc(E["sem"], 1)
            tok = (en, E["sem"], E["n"])
        else:
            tok = (en, E["sem"], E["n"] + 1)
        self._upd(tok, reads, writes)
        return tok

    def dma(self, q, out, in_, reads=(), writes=(), indirect=None, extra_waits=(), **kw):
        P = self.dp[q]
        i = P["nxt"]
        P["nxt"] = (i + 1) % len(P["sems"])
        key = "d_%s_%d" % (q, i)
        sem = P["sems"][i]
        if P["cnt"][i] > 0:
            self._wait(q, (key, sem, P["cnt"][i]))
        self._deps(q, reads, writes)
        for t in extra_waits:
            self._wait(q, t)
        E = self.eng[q]
        if indirect is None:
            ins = E["e"].dma_start(out=out, in_=in_, **kw)
        else:
            ins = E["e"].indirect_dma_start(out=out, in_=in_, **indirect)
        P["cnt"][i] += 16
        ins.then_inc(sem, 16)
        tok = (key, sem, P["cnt"][i])
        self._upd(tok, reads, writes)
        return tok

    def all_tokens(self):
        toks = []
        for name, E in self.eng.items():
            if E["sem"] is not None and E["n"] > 0:
                toks.append((name, E["sem"], E["n"]))
        for q, P in self.dp.items():
            for i, s in enumerate(P["sems"]):
                if P["cnt"][i] > 0:
                    toks.append(("d_%s_%d" % (q, i), s, P["cnt"][i]))
        return toks

    def barrier(self):
        toks = self.all_tokens()
        for en in ("pe", "act", "dve", "pool", "sp"):
            for t in toks:
                self._wait(en, t)


class Ctx:
    pass


def rope_tables():
    t = np.arange(S)
    row = (t // GRID_W).astype(np.float32)
    col = (t % GRID_W).astype(np.float32)
    nf = HD // 4
    inv = (10000.0 ** (-np.arange(nf, dtype=np.float32) / nf)).astype(np.float32)
    ar = row[:, None] * inv
    ac = col[:, None] * inv
    ang = np.concatenate([ar, ar, ac, ac], axis=-1).astype(np.float32)
    cos = np.cos(ang).astype(np.float32)
    sin = np.sin(ang).astype(np.float32)
    sgn = np.ones(HD, np.float32)
    sgn[0:32] = -1.0
    sgn[64:96] = -1.0
    tab = np.zeros((NT, 2, HD), np.float32)
    tab[:CTX, 0, :] = 1.0
    tab[CTX:, 0, :] = cos
    tab[CTX:, 1, :] = sin * sgn
    return np.ascontiguousarray(tab.reshape(NTT, 128, 2 * HD).transpose(1, 0, 2))


def d_tile_sets():
    out = []
    for i in range(32):
        if i == 0:
            js = [0, 1, 2, 3]
            ids = [5 + k for k in range(4)]
        elif i == 1:
            js = [0, 1, 2, 3]
            ids = [9 + k for k in range(4)]
        elif i == 30:
            js = [28, 29, 30, 31]
            ids = [13 + k for k in range(4)]
        elif i == 31:
            js = [28, 29, 30, 31]
            ids = [17 + k for k in range(4)]
        else:
            js = [i - 2, i - 1, i, i + 1, i + 2]
            ids = [0, 1, 2, 3, 4]
        out.append((js, ids))
    return out


def d_tables(rpb):
    tabs = np.full((8, 21, 128, 128), -1e30, np.float32)
    sets = d_tile_sets()
    done = set()
    kl = np.arange(128)
    ql = np.arange(128)
    for i in (2, 0, 1, 30, 31):
        js, ids = sets[i]
        for j, tid in zip(js, ids):
            if tid in done:
                continue
            done.add(tid)
            kr = 2 * j + kl // 64
            kc = kl % 64
            qr = 2 * i + ql // 64
            qc = ql % 64
            rs = np.clip(qr - 4, 0, 56)
            cs = np.clip(qc - 8, 0, 48)
            vis = ((kr[:, None] >= rs[None, :]) & (kr[:, None] < rs[None, :] + 8)
                   & (kc[:, None] >= cs[None, :]) & (kc[:, None] < cs[None, :] + 16))
            ridx = np.clip(kr[:, None] - qr[None, :] + 7, 0, 14)
            cidx = np.clip(kc[:, None] - qc[None, :], -15, 15) + 15
            for h in range(8):
                vals = rpb[h][ridx, cidx]
                tabs[h, tid] = np.where(vis, vals, np.float32(-1e30))
    return np.ascontiguousarray(tabs.transpose(2, 0, 1, 3))


def const_pack():
    c = {}
    c["ident_bf"] = np.eye(128, dtype=np.float32).astype(NBF16)
    c["ident_f"] = np.eye(128, dtype=np.float32)
    c["ropetab"] = rope_tables()
    k = np.arange(128)[:, None]
    q = np.arange(128)[None, :]
    mu = (k >= q).astype(np.float32)
    ml = (k <= q).astype(np.float32)
    bm = np.zeros((128, 2, 4, 128), np.float32)
    bm[:, 0] = mu[:, None, :]
    bm[:, 1] = ml[:, None, :]
    c["bandmask"] = bm.astype(NBF16)
    c["tri"] = np.concatenate([np.ones((128, 128), np.float32),
                               (k < q).astype(np.float32)], axis=1).astype(NBF16)
    c["iota"] = np.tile(np.arange(512, dtype=np.float32)[None, :], (128, 1))
    rows = (np.arange(NTT)[None, :] * 128 + np.arange(128)[:, None])
    tk = np.zeros((128, NTT, 2), np.float32)
    tk[:, :, 0] = rows // 64
    tk[:, :, 1] = rows % 64
    c["tokab"] = tk.astype(NBF16)
    return c


EVEN_TYPES = ["R"] * 8 + ["R"] * 2 + ["V"] * 2 + ["R"] * 8 + ["R"] * 8 + ["V"] * 8
ODD_TYPES = ["NQ"] * 8 + ["NK"] * 2 + ["V"] * 2 + ["P"] * 8 + ["P"] * 8 + ["V"] * 8


def block_maps(types):
    ft, vv = {}, {}
    for b, t in enumerate(types):
        if t == "V":
            vv[b] = len(vv)
        else:
            ft[b] = len(ft)
    return ft, vv


def build_program(dbg=None):
    dbg = dbg or {}
    kinds = dbg.get("kinds", {})
    nc = bass.Bass("TRN2", target_bir_lowering=False)
    K = Ctx()
    K.nc = nc
    K.dbg = dbg
    _cnt = [0]

    def _sbt(name, shape, dt):
        _cnt[0] += 1
        return nc.sbuf_tensor("%s_%d" % (name, _cnt[0]), shape, dt)

    def _pst(name, shape, dt):
        _cnt[0] += 1
        return nc.psum_tensor("%s_%d" % (name, _cnt[0]), shape, dt)
    K.sbt = _sbt
    K.pst = _pst

    def dram(name, shape, dt, kind):
        shape = dbg.get("shapes", {}).get(name, shape)
        return nc.dram_tensor(name, list(shape), dt, kind=kinds.get(name, kind)).ap()

    d = {}
    K.d = d
    d["x"] = dram("x", [S, D], F32, "ExternalInput")
    d["ctx"] = dram("ctx", [CTX, D], F32, "ExternalInput")
    d["cvec"] = dram("cvec", [2, D], F32, "ExternalInput")
    d["w_ada"] = dram("w_ada", [2, D, 6 * D], F32, "ExternalInput")
    d["b_ada"] = dram("b_ada", [2, 6 * D], F32, "ExternalInput")
    d["gvec"] = dram("gvec", [5, D], F32, "ExternalInput")
    d["w_in"] = dram("w_in", [2, D, INW], F32, "ExternalInput")
    d["w_out"] = dram("w_out", [2, D, D], F32, "ExternalInput")
    d["w_router"] = dram("w_router", [2, D, NE], F32, "ExternalInput")
    d["w_gate"] = dram("w_gate", [2, NE, D, DE], F32, "ExternalInput")
    d["w_up"] = dram("w_up", [2, NE, D, DE], F32, "ExternalInput")
    d["w_down"] = dram("w_down", [2, NE, DE, D], F32, "ExternalInput")
    d["small"] = dram("small", [1, 1024], F32, "ExternalInput")
    d["small2"] = dram("small2", [1, 256], F32, "ExternalInput")
    d["rpbtab"] = dram("rpbtab", [128, 8, 21, 128], F32, "ExternalInput")
    d["ident_bf"] = dram("ident_bf", [128, 128], BF16, "ExternalInput")
    d["ident_f"] = dram("ident_f", [128, 128], F32, "ExternalInput")
    d["ropetab"] = dram("ropetab", [128, NTT, 2 * HD], F32, "ExternalInput")
    d["bandmask"] = dram("bandmask", [128, 2, 4, 128], BF16, "ExternalInput")
    d["tri"] = dram("tri", [128, 256], BF16, "ExternalInput")
    d["iota"] = dram("iota", [128, 512], F32, "ExternalInput")
    d["tokab"] = dram("tokab", [128, NTT, 2], BF16, "ExternalInput")
    d["out"] = dram("out", [S, D], F32, "ExternalOutput")
    d["modv"] = dram("modv", [2, 2, 6 * D], F32, "Internal")
    d["hT"] = dram("hT", [NTT, 128, 16 * 128], BF16, "Internal")
    d["FT"] = dram("FT", [26, 128, NT], BF16, "Internal")
    d["VV"] = dram("VV", [NT, 10 * 128], BF16, "Internal")
    d["OO"] = dram("OO", [NT, D], BF16, "Internal")
    d["H2"] = dram("H2", [NT, D], BF16, "Internal")
    for l in range(2):
        for nb in range(4):
            d["XS%d_%d" % (l, nb)] = dram("XS%d_%d" % (l, nb), [NT, 512], F32, "Internal")
    K.rs = {}

    def R(name):
        if name not in K.rs:
            K.rs[name] = Res()
        return K.rs[name]
    K.R = R

    with ExitStack() as es:
        T = Trk(nc, es)
        K.T = T
        K.ident_bf = es.enter_context(K.sbt("ident_bf_s", [128, 128], BF16))
        K.ident_f = es.enter_context(K.sbt("ident_f_s", [128, 128], F32))
        K.aff = es.enter_context(K.sbt("aff_tm", [128, NTT, NE], F32))
        K.idx = es.enter_context(K.sbt("idx_i", [128, NE, 4], I32))
        K.gate = es.enter_context(K.sbt("gate_f", [128, NE, 4], F32))
        K.idxx = es.enter_context(K.sbt("idxx_i", [128, NE], I32))
        K.gatex = es.enter_context(K.sbt("gatex_f", [128, NE], F32))
        T.dma("sp", K.ident_bf[:], d["ident_bf"], writes=[R("ident")])
        T.dma("sp", K.ident_f[:], d["ident_f"], writes=[R("ident")])

        phases = dbg.get("phases")

        def want(p):
            return phases is None or p in phases

        for l in dbg.get("layers", [0, 1]):
            if want("mod"):
                with nc.named_scope("mod%d" % l):
                    phase_mod(K, l)
                T.barrier()
            if want("norm1"):
                with nc.named_scope("norm1%d" % l):
                    phase_norm(K, l, 1)
                T.barrier()
            if want("proj"):
                with nc.named_scope("proj%d" % l):
                    phase_proj(K, l)
                T.barrier()
            if want("attn"):
                with nc.named_scope("attn%d" % l):
                    phase_attn(K, l)
                T.barrier()
            if want("outproj"):
                with nc.named_scope("outproj%d" % l):
                    phase_outproj(K, l)
                T.barrier()
            if want("norm2"):
                with nc.named_scope("norm2%d" % l):
                    phase_norm(K, l, 2)
                T.barrier()
            if want("route"):
                with nc.named_scope("route%d" % l):
                    phase_route(K, l)
                T.barrier()
            if want("moe"):
                with nc.named_scope("moe%d" % l):
                    phase_moe(K, l)
                T.barrier()
        if want("final"):
            phase_final(K)
        T.barrier()
        if dbg.get("dump"):
            for nm_, tl, shp, dt_ in (("dbg_aff", K.aff, [128, NTT * NE], F32), ("dbg_idx", K.idx, [128, NE * 4], I32),
                                     ("dbg_gate", K.gate, [128, NE * 4], F32), ("dbg_idxx", K.idxx, [128, NE], I32),
                                     ("dbg_gatex", K.gatex, [128, NE], F32)):
                o = nc.dram_tensor(nm_, shp, dt_, kind="ExternalOutput").ap()
                src = tl[:]
                if len(src.shape) == 3:
                    src = src.rearrange("p a b -> p (a b)")
                T.dma("sp", o, src)
            T.barrier()
    return nc


def xs_src(K, l, tt, nb):
    d = K.d
    if l == 0:
        if tt < 2:
            return d["ctx"][tt * 128:(tt + 1) * 128, nb * 512:(nb + 1) * 512]
        return d["x"][(tt - 2) * 128:(tt - 1) * 128, nb * 512:(nb + 1) * 512]
    return d["XS%d_%d" % (l - 1, nb)][tt * 128:(tt + 1) * 128, :]


def load_x_tile(K, l, tt, dst, res, stream_res=None):
    T = K.T
    for nb in range(4):
        rd = [K.R("XS%d_%d" % (l - 1, nb))] if l > 0 else []
        T.dma("sp", dst[:, nb * 512:(nb + 1) * 512], xs_src(K, l, tt, nb), reads=rd, writes=[res])


def phase_mod(K, l):
    nc, T, d, R = K.nc, K.T, K.d, K.R
    with ExitStack() as es:
        sb = lambda n, s, dt: es.enter_context(K.sbt(n, s, dt))
        cT = sb("cT", [128, 2, 16], F32)
        cS = sb("cS", [128, 2, 16], F32)
        condT = sb("condT", [128, 16, 2], BF16)
        wch = [sb("wch%d" % i, [128, 16, 512], BF16) for i in range(2)]
        modrow = sb("modrow", [2, 6 * D], F32)
        brow = sb("brow", [2, 6 * D], F32)
        ps = [es.enter_context(K.pst("mps%d" % i, [128, 512], F32)) for i in range(2)]
        r_c, r_cond, r_b, r_mod = Res(), Res(), Res(), Res()
        r_w = [Res(), Res()]
        r_ps = [Res(), Res()]
        with nc.allow_non_contiguous_dma(reason="tiny cond vector transpose load"):
            T.dma("sp", cT[:], d["cvec"].rearrange("w (kc p) -> p w kc", p=128), writes=[r_c])
        T.dma("sp", brow[:], d["b_ada"][l:l + 1, :].partition_broadcast(2), writes=[r_b])
        T.op("act", lambda: nc.scalar.activation(out=cS[:], in_=cT[:], func=AF.Silu), reads=[r_c], writes=[r_cond])
        T.op("dve", lambda: nc.vector.tensor_copy(out=condT[:].rearrange("p k w -> p w k"), in_=cS[:]),
             reads=[r_cond], writes=[r_cond])
        wv = d["w_ada"][l].rearrange("(kc p) n -> p kc n", p=128)
        NB = 24

        def loadw(nb):
            T.dma("pool", wch[nb % 2][:], wv[:, :, nb * 512:(nb + 1) * 512], writes=[r_w[nb % 2]])
        loadw(0)
        for nb in range(NB):
            if nb + 1 < NB:
                loadw(nb + 1)
            b = nb % 2
            for kc in range(16):
                T.op("pe", lambda kc=kc: nc.tensor.matmul(ps[b][0:2, :], lhsT=condT[:, kc, :], rhs=wch[b][:, kc, :],
                                                         start=(kc == 0), stop=(kc == 15)),
                     reads=[r_cond, r_w[b]], writes=[r_ps[b]], inc=(kc == 15))
            T.op("dve", lambda: nc.vector.tensor_tensor(out=modrow[:, nb * 512:(nb + 1) * 512], in0=ps[b][0:2, :],
                                                        in1=brow[:, nb * 512:(nb + 1) * 512], op=ALU.add),
                 reads=[r_ps[b], r_b], writes=[r_mod])
        for c0 in (D, 4 * D):
            T.op("dve", lambda c0=c0: nc.vector.tensor_scalar(out=modrow[:, c0:c0 + D], in0=modrow[:, c0:c0 + D],
                                                              scalar1=1.0, scalar2=None, op0=ALU.add),
                 reads=[r_mod], writes=[r_mod])
        T.dma("sp", d["modv"][l], modrow[:], reads=[r_mod], writes=[R("modv")])


def load_bc(K, dst_ap, src_row_ap, res, reads=()):
    K.T.dma("sp", dst_ap, src_row_ap.partition_broadcast(128), reads=list(reads), writes=[res])


def phase_norm(K, l, which):
    nc, T, d, R = K.nc, K.T, K.d, K.R
    first_tt = 0
    if which == 2:
        first_tt = 0 if l == 0 else 2
    with ExitStack() as es:
        sb = lambda n, s, dt: es.enter_context(K.sbt(n, s, dt))
        A = [sb("nA%d" % i, [128, D], F32) for i in range(2)]
        SH = [sb("nS%d" % i, [128, D], F32) for i in range(2)]
        gb = sb("ngb", [128, D], F32)
        xt = [sb("nxt%d" % i, [128, D], F32) for i in range(4)]
        junk_ = [sb("njunk%d" % i, [128, D], BF16) for i in range(2)]
        hf_ = [sb("nhf%d" % i, [128, D], F32) for i in range(2)]
        hb = [sb("nhb%d" % i, [128, D], BF16) for i in range(2)]
        hT = [sb("nhT%d" % i, [128, 16, 128], BF16) for i in range(2)]
        st_ = [sb("nst%d" % i, [128, 4], F32) for i in range(2)]
        wr = sb("nwr", [128, 16, NE], BF16)
        ex_ = [sb("nex%d" % i, [128, NE], F32) for i in range(2)]
        tp = [es.enter_context(K.pst("ntp%d" % i, [128, 1024], BF16)) for i in range(4)]
        lp_ = [es.enter_context(K.pst("nlp%d" % i, [128, 512], F32)) for i in range(2)]
        r_A, r_x, r_hb, r_hT = Res(), [Res(), Res(), Res(), Res()], [Res(), Res()], [Res(), Res()]
        r_tp, r_lp_, r_wr = [Res(), Res(), Res(), Res()], [Res(), Res()], Res()
        r_hf_, r_st_, r_junk_, r_ex_ = [Res(), Res()], [Res(), Res()], [Res(), Res()], [Res(), Res()]
        sh_c, sc_c = (0, D) if which == 1 else (3 * D, 4 * D)
        grow = l if which == 1 else 2 + l
        load_bc(K, gb[:], d["gvec"][grow:grow + 1, :], r_A)
        for w in range(2):
            load_bc(K, A[w][:], d["modv"][l, w:w + 1, sc_c:sc_c + D], r_A, reads=[R("modv")])
            load_bc(K, SH[w][:], d["modv"][l, w:w + 1, sh_c:sh_c + D], r_A, reads=[R("modv")])
        for w in range(2):
            T.op("dve", lambda w=w: nc.vector.tensor_tensor(out=A[w][:], in0=A[w][:], in1=gb[:], op=ALU.mult),
                 reads=[r_A], writes=[r_A])
        if which == 2:
            T.dma("pool", wr[:], d["w_router"][l].rearrange("(kc p) e -> p kc e", p=128), writes=[r_wr])
        tiles = list(range(first_tt, NTT))
        def load(tt, buf):
            if which == 1:
                load_x_tile(K, l, tt, xt[buf], r_x[buf])
            else:
                for nb in range(4):
                    T.dma("sp", xt[buf][:, nb * 512:(nb + 1) * 512],
                          d["XS%d_%d" % (l, nb)][tt * 128:(tt + 1) * 128, :],
                          reads=[R("XS%d_%d" % (l, nb))], writes=[r_x[buf]])
        def tile_ops(n, tt):
            b = n % 2
            w = 1 if tt < 2 else 0
            junk, hf, st, ex = junk_[b], hf_[b], st_[b], ex_[b]
            r_junk, r_hf, r_st, r_ex = r_junk_[b], r_hf_[b], r_st_[b], r_ex_[b]
            r_lpb = r_lp_[b]
            lpb = lp_[b]
            ops = []
            xb4 = n % 4
            ops.append(lambda: T.op("act", lambda: nc.scalar.activation(out=junk[:], in_=xt[xb4][:], func=AF.Square, accum_out=st[:, 0:1]),
                                    reads=[r_x[xb4]], writes=[r_junk, r_st]))
            ops.append(lambda: T.op("act", lambda: nc.scalar.activation(out=st[:, 1:2], in_=st[:, 0:1], func=AF.Sqrt, scale=1.0 / D, bias=EPS),
                                    reads=[r_st], writes=[r_st]))
            ops.append(lambda: T.op("dve", lambda: nc.vector.reciprocal(out=st[:, 2:3], in_=st[:, 1:2]), reads=[r_st], writes=[r_st]))
            ops.append(lambda: T.op("dve", lambda: nc.vector.scalar_tensor_tensor(out=hf[:], in0=xt[xb4][:], scalar=st[:, 2:3], in1=A[w][:],
                                                                                  op0=ALU.mult, op1=ALU.mult),
                                    reads=[r_x[xb4], r_st, r_A], writes=[r_hf]))
            ops.append(lambda: T.op("dve", lambda: nc.vector.tensor_tensor(out=hb[b][:], in0=hf[:], in1=SH[w][:], op=ALU.add),
                                    reads=[r_hf, r_A], writes=[r_hb[b]]))
            if which == 2:
                ops.append(lambda: T.dma("sp", d["H2"][tt * 128:(tt + 1) * 128, :], hb[b][:], reads=[r_hb[b]], writes=[R("H2")]))

            def trg(g):
                tpg = tp[b * 2 + g]
                r_tpg = r_tp[b * 2 + g]
                for k8 in range(8):
                    kc = g * 8 + k8
                    T.op("pe", lambda kc=kc, k8=k8: nc.tensor.transpose(out=tpg[:, k8 * 128:(k8 + 1) * 128],
                                                                      in_=hb[b][:, kc * 128:(kc + 1) * 128], identity=K.ident_bf[:]),
                         reads=[r_hb[b], R("ident")], writes=[r_tpg], inc=(k8 == 7))
                T.op("act", lambda: nc.scalar.copy(out=hT[b][:, g * 8:(g + 1) * 8, :].rearrange("p a b -> p (a b)"), in_=tpg[:]),
                     reads=[r_tpg], writes=[r_hT[b]])
            ops.append(lambda: trg(0))
            ops.append(lambda: trg(1))
            if which == 1:
                ops.append(lambda: T.dma("sp", d["hT"][tt], hT[b][:].rearrange("p a b -> p (a b)"), reads=[r_hT[b]], writes=[R("hT")]))
            else:
                def router():
                    for kc in range(16):
                        T.op("pe", lambda kc=kc: nc.tensor.matmul(lpb[:, 0:NE], lhsT=hT[b][:, kc, :], rhs=wr[:, kc, :],
                                                                 start=(kc == 0), stop=(kc == 15)),
                             reads=[r_hT[b], r_wr], writes=[r_lpb], inc=(kc == 15))
                ops.append(router)
                ops.append(lambda: T.op("act", lambda: nc.scalar.activation(out=ex[:], in_=lpb[:, 0:NE], func=AF.Exp, accum_out=st[:, 3:4]),
                                        reads=[r_lpb], writes=[r_ex, r_st]))
                ops.append(lambda: T.op("dve", lambda: nc.vector.reciprocal(out=st[:, 3:4], in_=st[:, 3:4]), reads=[r_st], writes=[r_st]))
                ops.append(lambda: T.op("dve", lambda: nc.vector.tensor_scalar(out=K.aff[:, tt, :], in0=ex[:], scalar1=st[:, 3:4],
                                                                               scalar2=None, op0=ALU.mult),
                                        reads=[r_ex, r_st], writes=[K.R("aff")]))
            return ops

        for q0 in range(0, min(2, len(tiles))):
            load(tiles[q0], q0 % 4)
        for p0 in range(0, len(tiles), 2):
            for q0 in range(p0 + 2, min(p0 + 4, len(tiles))):
                load(tiles[q0], q0 % 4)
            oa = tile_ops(p0, tiles[p0])
            ob = tile_ops(p0 + 1, tiles[p0 + 1]) if p0 + 1 < len(tiles) else []
            for i in range(max(len(oa), len(ob))):
                if i < len(oa):
                    oa[i]()
                if i < len(ob):
                    ob[i]()


def phase_proj(K, l):
    nc, T, d, R = K.nc, K.T, K.d, K.R
    types = EVEN_TYPES if l % 2 == 0 else ODD_TYPES
    ftmap, vvmap = block_maps(types)
    with ExitStack() as es:
        sb = lambda n, s, dt: es.enter_context(K.sbt(n, s, dt))
        wch = [sb("pw%d" % i, [128, 16, 512], BF16) for i in range(2)]
        hT = [sb("phT%d" % i, [128, 16, 128], BF16) for i in range(3)]
        rope = sb("prope", [128, NTT, 2 * HD], F32)
        FTst = sb("pFT", [128, 4, NT], BF16)
        Vst = [sb("pV%d" % i, [128, 512], BF16) for i in range(2)]
        xs = [sb("pxs%d" % i, [128, 512], F32) for i in range(2)]
        t1 = sb("pt1", [128, 512], F32)
        t2 = sb("pt2", [128, 512], F32)
        xb = [sb("pxb%d" % i, [128, 512], BF16) for i in range(2)]
        nrm = sb("pnrm", [128, 2, 128], F32)
        sq = sb("psq", [128, 512], F32)
        st = sb("pst", [128, 8], F32)
        ps = [es.enter_context(K.pst("pps%d" % i, [128, 512], F32)) for i in range(2)]
        tp = [es.enter_context(K.pst("ptp%d" % i, [128, 512], BF16)) for i in range(2)]
        r_w, r_hT = [Res(), Res()], [Res(), Res(), Res()]
        r_rope, r_FT, r_V, r_xs, r_t, r_xb = Res(), Res(), [Res(), Res()], [Res(), Res()], Res(), [Res(), Res()]
        r_ps, r_tp, r_nrm, r_st = [Res(), Res()], [Res(), Res()], Res(), Res()
        T.dma("sp", rope[:], d["ropetab"], writes=[r_rope])
        if l % 2 == 1:
            load_bc(K, nrm[:].rearrange("p a b -> p (a b)"), d["small2"][0:1, :], r_nrm)
        wv = d["w_in"][l].rearrange("(kc p) n -> p kc n", p=128)

        def loadw(cb):
            T.dma("pool", wch[cb % 2][:], wv[:, :, cb * 512:(cb + 1) * 512], writes=[r_w[cb % 2]])

        def loadh(n):
            tt = n % NTT
            T.dma("sp", hT[n % 3][:].rearrange("p a b -> p (a b)"), d["hT"][tt], reads=[R("hT")], writes=[r_hT[n % 3]])
        loadw(0)
        loadh(0)
        loadh(1)
        n = 0
        deferred = []

        def mk_B(tb, xbuf, b0, nbk, c0, c1, tt):
            def fn_():
                for k in range(nbk):
                    T.op("pe", lambda k=k: nc.tensor.transpose(out=tp[tb][:, (b0 + k) * 128:(b0 + k + 1) * 128],
                                                               in_=xb[xbuf][:, (b0 + k) * 128:(b0 + k + 1) * 128],
                                                               identity=K.ident_bf[:]),
                         reads=[r_xb[xbuf], R("ident")], writes=[r_tp[tb]], inc=(k == nbk - 1))
                T.op("act", lambda: nc.scalar.copy(out=FTst[:, b0:b0 + nbk, tt * 128:(tt + 1) * 128],
                                                   in_=tp[tb][:, c0:c1].rearrange("p (h e) -> p h e", e=128)),
                     reads=[r_tp[tb]], writes=[r_FT])
            return fn_

        def mk_flush(cb, btypes):
            def fn_():
                for bi, t in enumerate(btypes):
                    if t == "V":
                        continue
                    T.dma("sp", d["FT"][ftmap[cb * 4 + bi]], FTst[:, bi, :], reads=[r_FT], writes=[R("FT")])
            return fn_
        for cb in range(9):
            if cb + 1 < 9:
                loadw(cb + 1)
            btypes = types[cb * 4:(cb + 1) * 4]
            for tt in range(NTT):
                if n + 2 < 9 * NTT:
                    loadh(n + 2)
                hb_ = n % 3
                pb = n % 2
                for kc in range(16):
                    T.op("pe", lambda kc=kc: nc.tensor.matmul(ps[pb][:], lhsT=hT[hb_][:, kc, :], rhs=wch[cb % 2][:, kc, :],
                                                             start=(kc == 0), stop=(kc == 15)),
                         reads=[r_hT[hb_], r_w[cb % 2]], writes=[r_ps[pb]], inc=(kc == 15))
                for fn_ in deferred:
                    fn_()
                deferred = []
                groups = []
                for bi, t in enumerate(btypes):
                    if groups and groups[-1][0] == t:
                        groups[-1][2] += 1
                    else:
                        groups.append([t, bi, 1])
                for (t, b0, nbk) in groups:
                    c0, c1 = b0 * 128, (b0 + nbk) * 128
                    if t == "V":
                        vb = n % 2
                        T.op("act", lambda: nc.scalar.copy(out=Vst[vb][:, c0:c1], in_=ps[pb][:, c0:c1]),
                             reads=[r_ps[pb]], writes=[r_V[vb]])
                        v0 = vvmap[cb * 4 + b0]
                        T.dma("sp", d["VV"][tt * 128:(tt + 1) * 128, v0 * 128:(v0 + nbk) * 128], Vst[vb][:, c0:c1],
                              reads=[r_V[vb]], writes=[R("VV")])
                        continue
                    xbuf = n % 2
                    if t == "P":
                        T.op("act", lambda: nc.scalar.copy(out=xb[xbuf][:, c0:c1], in_=ps[pb][:, c0:c1]),
                             reads=[r_ps[pb]], writes=[r_xb[xbuf]])
                    else:
                        T.op("act", lambda: nc.scalar.copy(out=xs[xbuf][:, c0:c1], in_=ps[pb][:, c0:c1]),
                             reads=[r_ps[pb]], writes=[r_xs[xbuf]])
                        xv = xs[xbuf][:, c0:c1].rearrange("p (h e) -> p h e", e=128)
                        if t in ("NQ", "NK"):
                            wsel = 0 if t == "NQ" else 1
                            T.op("act", lambda: nc.scalar.activation(out=sq[:, c0:c1], in_=xs[xbuf][:, c0:c1], func=AF.Square),
                                 reads=[r_xs[xbuf]], writes=[r_t])
                            T.op("dve", lambda: nc.vector.tensor_reduce(out=st[:, 0:nbk], in_=sq[:, c0:c1].rearrange("p (h e) -> p h e", e=128),
                                                                        axis=mybir.AxisListType.X, op=ALU.add),
                                 reads=[r_t], writes=[r_st])
                            T.op("act", lambda: nc.scalar.activation(out=st[:, 0:nbk], in_=st[:, 0:nbk], func=AF.Sqrt,
                                                                     scale=1.0 / HD, bias=EPS),
                                 reads=[r_st], writes=[r_st])
                            T.op("dve", lambda: nc.vector.reciprocal(out=st[:, 4:4 + nbk], in_=st[:, 0:nbk]),
                                 reads=[r_st], writes=[r_st])
                            T.op("dve", lambda: nc.vector.tensor_tensor(out=xv, in0=xv,
                                                                        in1=st[:, 4:4 + nbk].unsqueeze(2).to_broadcast([128, nbk, 128]),
                                                                        op=ALU.mult),
                                 reads=[r_xs[xbuf], r_st], writes=[r_xs[xbuf]])
                            T.op("dve", lambda: nc.vector.tensor_tensor(out=xv, in0=xv,
                                                                        in1=nrm[:, wsel, :].unsqueeze(1).to_broadcast([128, nbk, 128]),
                                                                        op=ALU.mult),
                                 reads=[r_xs[xbuf], r_nrm], writes=[r_xs[xbuf]])
                        cosb = rope[:, tt, 0:HD].unsqueeze(1).to_broadcast([128, nbk, HD])
                        t1v = t1[:, c0:c1].rearrange("p (h e) -> p h e", e=128)
                        T.op("dve", lambda: nc.vector.tensor_tensor(out=t1v, in0=xv, in1=cosb, op=ALU.mult),
                             reads=[r_xs[xbuf], r_rope], writes=[r_t])
                        x5 = xs[xbuf][:, c0:c1].rearrange("p (h a j f) -> p (h a) j f", a=2, j=2, f=32)
                        t5 = t2[:, c0:c1].rearrange("p (h a j f) -> p (h a) j f", a=2, j=2, f=32)
                        s5 = rope[:, tt, HD:2 * HD].rearrange("p (a j f) -> p a j f", a=2, j=2, f=32)
                        for j in range(2):
                            for a in range(2):
                                T.op("dve", lambda j=j, a=a: nc.vector.tensor_tensor(
                                    out=t2[:, c0:c1].rearrange("p (h a j f) -> p h a j f", a=2, j=2, f=32)[:, :, a, j, :],
                                    in0=xs[xbuf][:, c0:c1].rearrange("p (h a j f) -> p h a j f", a=2, j=2, f=32)[:, :, a, 1 - j, :],
                                    in1=s5[:, a, j, :].unsqueeze(1).to_broadcast([128, nbk, 32]), op=ALU.mult),
                                    reads=[r_xs[xbuf], r_rope], writes=[r_t])
                        T.op("dve", lambda: nc.vector.tensor_tensor(out=xb[xbuf][:, c0:c1], in0=t1[:, c0:c1], in1=t2[:, c0:c1], op=ALU.add),
                             reads=[r_t], writes=[r_xb[xbuf]])
                    deferred.append(mk_B(n % 2, xbuf, b0, nbk, c0, c1, tt))
                n += 1
            deferred.append(mk_flush(cb, btypes))
        for fn_ in deferred:
            fn_()


def attn_group(K, A, qap, ncols, keys, dvp, finish):
    nc, T = K.nc, K.T
    nsub = ncols // 128
    n = len(keys)
    S_ps, P_sb, acc = A["S"], A["P"], A["acc"]
    r_S, r_P, r_acc = A["r_S"], A["r_P"], A["r_acc"]
    rq, rk, rv, rt = A["rq"], A["rk"], A["rv"], A["rt"]

    def qk(k):
        sbuf = A["cnt"] % 3
        A["scur"][k] = sbuf
        kT, _, _ = keys[k]
        T.op("pe", lambda: nc.tensor.matmul(S_ps[sbuf][:, 0:ncols], lhsT=kT, rhs=qap, start=True, stop=True),
             reads=[rq, rk], writes=[r_S[sbuf]])
        A["cnt"] += 1
    A["scur"] = {}
    qk(0)
    if n > 1:
        qk(1)
    for k in range(n):
        if k + 2 < n:
            qk(k + 2)
        sbuf = A["scur"][k]
        pb = A["pcnt"] % 3
        A["pcnt"] += 1
        _, v, tbl = keys[k]
        T.op("act", lambda: nc.scalar.activation(out=P_sb[pb][:, 0:ncols], in_=S_ps[sbuf][:, 0:ncols], func=AF.Exp, scale=SCALE),
             reads=[r_S[sbuf]], writes=[r_P[pb]])
        if tbl is not None:
            T.op("dve", lambda: nc.vector.tensor_tensor(out=P_sb[pb][:, 0:ncols], in0=P_sb[pb][:, 0:ncols], in1=tbl, op=ALU.mult),
                 reads=[rt], writes=[r_P[pb]])
        for s in range(nsub):
            T.op("pe", lambda s=s: nc.tensor.matmul(acc[s][:, 0:dvp], lhsT=P_sb[pb][:, s * 128:(s + 1) * 128], rhs=v,
                                                   start=(k == 0), stop=(k == n - 1)),
                 reads=[r_P[pb], rv], writes=[r_acc[s]], inc=(k == n - 1 or s == nsub - 1))
    for s in range(nsub):
        finish(s, acc[s], r_acc[s])


def attn_bufs(K, es, nacc=4):
    nc = K.nc
    A = {}
    A["S"] = [es.enter_context(K.pst("aS%d" % i, [128, 512], F32)) for i in range(3)]
    A["acc"] = [es.enter_context(K.pst("aacc%d" % i, [128, 512], F32)) for i in range(nacc)]
    A["P"] = [es.enter_context(K.sbt("aP%d" % i, [128, 512], BF16)) for i in range(3)]
    A["r_S"] = [Res(), Res(), Res()]
    A["r_P"] = [Res(), Res(), Res()]
    A["r_acc"] = [Res() for _ in range(nacc)]
    A["cnt"] = 0
    A["pcnt"] = 0
    A["rt"] = Res()
    return A


def phase_attn(K, l):
    nc, T, d, R = K.nc, K.T, K.d, K.R
    types = EVEN_TYPES if l % 2 == 0 else ODD_TYPES
    ftmap, vvmap = block_maps(types)
    FT, VV, OO = d["FT"], d["VV"], d["OO"]
    lat_tiles = list(range(2, NTT))

    def ftv(blk):
        return FT[ftmap[blk]].rearrange("p (t c) -> p t c", c=128)

    with ExitStack() as es:
        sb = lambda n, s, dt: es.enter_context(K.sbt(n, s, dt))
        A = attn_bufs(K, es)
        gsets = [dict(QA=sb("aQA%d" % i, [128, NTT, 4, 128], BF16), KT=sb("aKT%d" % i, [128, NT], BF16),
                      VA=sb("aVA%d" % i, [128, NTT, 129], BF16), rq=Res(), rk=Res(), rv=Res()) for i in range(2)]
        Ost = [sb("aO%d" % i, [128, 512], BF16) for i in range(2)]
        rc = [sb("arc%d" % i, [128, 4], F32) for i in range(2)]
        esk = sb("aesk", [128, 8], F32)
        bm = sb("abm", [128, 2, 4, 128], BF16)
        r_es, r_O, r_rc = Res(), [Res(), Res()], [Res(), Res()]
        A["rq"], A["rk"], A["rv"] = Res(), Res(), Res()
        if l % 2 == 0:
            load_bc(K, esk[:], d["small"][0:1, 0:8], r_es)
            T.op("act", lambda: nc.scalar.activation(out=esk[:], in_=esk[:], func=AF.Exp), reads=[r_es], writes=[r_es])
            T.dma("sp", bm[:], d["bandmask"], writes=[A["rt"]])
        on = 0

        def loadg(g):
            G_ = gsets[g % 2]
            for hh in range(4):
                T.dma("sp", G_["QA"][:, :, hh, :], ftv(g * 4 + hh), reads=[R("FT")], writes=[G_["rq"]])
            T.dma("sp", G_["KT"][:], FT[ftmap[8 + g]], reads=[R("FT")], writes=[G_["rk"]])
            T.op("dve", lambda: nc.vector.memset(G_["VA"][:, :, 128:129], 1.0), writes=[G_["rv"]])
            T.dma("sp", G_["VA"][:, :, 0:128], VV[:, vvmap[10 + g] * 128:(vvmap[10 + g] + 1) * 128].rearrange("(t p) c -> p t c", p=128),
                  reads=[R("VV")], writes=[G_["rv"]])
        loadg(0)
        for g in range(2):
            if g + 1 < 2:
                loadg(g + 1)
            G_ = gsets[g % 2]
            QA, KT, VA = G_["QA"], G_["KT"], G_["VA"]
            A["rq"], A["rk"], A["rv"] = G_["rq"], G_["rk"], G_["rv"]
            qtiles = list(range(NTT)) if l % 2 == 0 else lat_tiles
            for i in qtiles:
                if l % 2 == 0:
                    if i < 2:
                        kl = [(0, None), (1, None)]
                    else:
                        kl = [(0, None), (1, None)]
                        if i - 1 >= 2:
                            kl.append((i - 1, 0))
                        kl.append((i, None))
                        if i + 1 < NTT:
                            kl.append((i + 1, 1))
                else:
                    kl = [(j, None) for j in range(NTT)]
                keys = [(KT[:, j * 128:(j + 1) * 128], VA[:, j, :],
                         None if m is None else bm[:, m, :, :].rearrange("p h q -> p (h q)")) for (j, m) in kl]
                ob = on % 2
                on += 1

                def finish(s, acc, r_acc, ob=ob, g=g):
                    if l % 2 == 0:
                        T.op("dve", lambda: nc.vector.tensor_scalar(out=rc[ob][:, s:s + 1], in0=acc[:, 128:129],
                                                                    scalar1=esk[:, g * 4 + s:g * 4 + s + 1], scalar2=None, op0=ALU.add),
                             reads=[r_acc, r_es], writes=[r_rc[ob]])
                        T.op("dve", lambda: nc.vector.reciprocal(out=rc[ob][:, s:s + 1], in_=rc[ob][:, s:s + 1]),
                             reads=[r_rc[ob]], writes=[r_rc[ob]])
                    else:
                        T.op("dve", lambda: nc.vector.reciprocal(out=rc[ob][:, s:s + 1], in_=acc[:, 128:129]),
                             reads=[r_acc], writes=[r_rc[ob]])
                    T.op("dve", lambda: nc.vector.tensor_scalar(out=Ost[ob][:, s * 128:(s + 1) * 128], in0=acc[:, 0:128],
                                                                scalar1=rc[ob][:, s:s + 1], scalar2=None, op0=ALU.mult),
                         reads=[r_acc, r_rc[ob]], writes=[r_O[ob]])
                attn_group(K, A, QA[:, i, :, :].rearrange("p h c -> p (h c)"), 512, keys, 129, finish)
                T.dma("sp", OO[i * 128:(i + 1) * 128, g * 512:(g + 1) * 512], Ost[ob][:], reads=[r_O[ob]], writes=[R("OO")])
    T.barrier()
    if l % 2 == 0:
        attn_B(K, l, ftmap, vvmap)
    else:
        attn_D(K, l, ftmap, vvmap)


def attn_B(K, l, ftmap, vvmap):
    nc, T, d, R = K.nc, K.T, K.d, K.R
    FT, VV, OO = d["FT"], d["VV"], d["OO"]
    lam_init = lambda_init_for(l)
    with ExitStack() as es:
        sb = lambda n, s, dt: es.enter_context(K.sbt(n, s, dt))
        A = attn_bufs(K, es)
        bsets = [dict(QT=[sb("bQ%d_%d" % (i, m), [128, NT], BF16) for m in range(2)],
                      KT=[sb("bK%d_%d" % (i, m), [128, NT], BF16) for m in range(2)],
                      VB=sb("bV%d" % i, [128, NTT, 257], BF16), r_q=[Res(), Res()], r_k=[Res(), Res()], r_v=Res()) for i in range(2)]
        o1 = sb("bo1", [128, 4, 256], F32)
        o2 = sb("bo2", [128, 4, 256], F32)
        Ost = [sb("bO%d" % i, [128, 4, 256], BF16) for i in range(2)]
        rc = sb("brc", [128, 8], F32)
        lamv = sb("blam", [128, 4, 128], F32)
        lamt = sb("blamt", [128, 2, 128], F32)
        lams = sb("blams", [128, 4], F32)
        subw = sb("bsub", [128, 256], F32)
        sqj = sb("bsq", [128, 256], F32)
        r_o, r_O, r_rc, r_lam, r_sub = Res(), [Res(), Res()], Res(), Res(), Res()
        load_bc(K, lamv[:].rearrange("p a b -> p (a b)"), d["small"][0:1, 8:520], r_lam)
        load_bc(K, subw[:], d["small"][0:1, 520:776], r_sub)
        T.op("dve", lambda: nc.vector.tensor_tensor(out=lamt[:], in0=lamv[:, 0:4:2, :], in1=lamv[:, 1:4:2, :], op=ALU.mult),
             reads=[r_lam], writes=[r_lam])
        T.op("dve", lambda: nc.vector.tensor_reduce(out=lams[:, 0:2], in_=lamt[:], axis=mybir.AxisListType.X, op=ALU.add),
             reads=[r_lam], writes=[r_lam])
        T.op("act", lambda: nc.scalar.activation(out=lams[:, 0:2], in_=lams[:, 0:2], func=AF.Exp), reads=[r_lam], writes=[r_lam])
        T.op("dve", lambda: nc.vector.tensor_tensor(out=lams[:, 2:3], in0=lams[:, 1:2], in1=lams[:, 0:1], op=ALU.subtract),
             reads=[r_lam], writes=[r_lam])
        T.op("dve", lambda: nc.vector.tensor_scalar(out=lams[:, 2:3], in0=lams[:, 2:3], scalar1=-lam_init, scalar2=None, op0=ALU.add),
             reads=[r_lam], writes=[r_lam])
        T.op("dve", lambda: nc.vector.tensor_scalar(out=subw[:], in0=subw[:], scalar1=1.0 - lam_init, scalar2=None, op0=ALU.mult),
             reads=[r_sub], writes=[r_sub])
        on = 0

        def loadb(hb):
            B_ = bsets[hb % 2]
            for m in range(2):
                T.dma("sp", B_["QT"][m][:], FT[ftmap[12 + hb * 2 + m]], reads=[R("FT")], writes=[B_["r_q"][m]])
                T.dma("sp", B_["KT"][m][:], FT[ftmap[20 + hb * 2 + m]], reads=[R("FT")], writes=[B_["r_k"][m]])
            T.op("dve", lambda: nc.vector.memset(B_["VB"][:, :, 256:257], 1.0), writes=[B_["r_v"]])
            v0 = vvmap[28 + hb * 2]
            T.dma("sp", B_["VB"][:, :, 0:256], VV[:, v0 * 128:(v0 + 2) * 128].rearrange("(t p) c -> p t c", p=128),
                  reads=[R("VV")], writes=[B_["r_v"]])
        loadb(0)
        for hb in range(4):
            if hb + 1 < 4:
                loadb(hb + 1)
            B_ = bsets[hb % 2]
            QT, KT, VB = B_["QT"], B_["KT"], B_["VB"]
            r_q, r_k, r_v = B_["r_q"], B_["r_k"], B_["r_v"]
            chunks = [(0, 2, [0, 1])] + [(2 + 4 * c, 4, list(range(NTT))) for c in range(8)]
            for (t0, ntile, kts) in chunks:
                ob = on % 2
                on += 1
                for m in range(2):
                    A["rq"], A["rk"], A["rv"] = r_q[m], r_k[m], r_v
                    keys = [(KT[m][:, j * 128:(j + 1) * 128], VB[:, j, :], None) for j in kts]
                    dst = o1 if m == 0 else o2

                    def finish(s, acc, r_acc, m=m, dst=dst):
                        T.op("dve", lambda: nc.vector.reciprocal(out=rc[:, m * 4 + s:m * 4 + s + 1], in_=acc[:, 256:257]),
                             reads=[r_acc], writes=[r_rc])
                        T.op("dve", lambda: nc.vector.tensor_scalar(out=dst[:, s, :], in0=acc[:, 0:256],
                                                                    scalar1=rc[:, m * 4 + s:m * 4 + s + 1], scalar2=None, op0=ALU.mult),
                             reads=[r_acc, r_rc], writes=[r_o])
                    attn_group(K, A, QT[m][:, t0 * 128:(t0 + ntile) * 128], ntile * 128, keys, 257, finish)
                for s in range(ntile):
                    T.op("dve", lambda s=s: nc.vector.scalar_tensor_tensor(out=o1[:, s, :], in0=o2[:, s, :], scalar=lams[:, 2:3],
                                                                           in1=o1[:, s, :], op0=ALU.mult, op1=ALU.add),
                         reads=[r_o, r_lam], writes=[r_o])
                    T.op("act", lambda s=s: nc.scalar.activation(out=sqj[:], in_=o1[:, s, :], func=AF.Square, accum_out=rc[:, s:s + 1]),
                         reads=[r_o], writes=[r_rc])
                    T.op("act", lambda s=s: nc.scalar.activation(out=rc[:, s:s + 1], in_=rc[:, s:s + 1], func=AF.Sqrt,
                                                                 scale=1.0 / 256, bias=EPS),
                         reads=[r_rc], writes=[r_rc])
                    T.op("dve", lambda s=s: nc.vector.reciprocal(out=rc[:, s:s + 1], in_=rc[:, s:s + 1]), reads=[r_rc], writes=[r_rc])
                    T.op("dve", lambda s=s: nc.vector.scalar_tensor_tensor(out=Ost[ob][:, s, :], in0=o1[:, s, :], scalar=rc[:, s:s + 1],
                                                                           in1=subw[:], op0=ALU.mult, op1=ALU.mult),
                         reads=[r_o, r_rc, r_sub], writes=[r_O[ob]])
                T.dma("sp", OO[t0 * 128:(t0 + ntile) * 128, 1024 + hb * 256:1024 + (hb + 1) * 256].rearrange("(t p) c -> p t c", p=128),
                      Ost[ob][:, 0:ntile, :], reads=[r_O[ob]], writes=[R("OO")])


def attn_D(K, l, ftmap, vvmap):
    nc, T, d, R = K.nc, K.T, K.d, K.R
    FT, VV, OO = d["FT"], d["VV"], d["OO"]
    sets = d_tile_sets()
    with ExitStack() as es:
        sb = lambda n, s, dt: es.enter_context(K.sbt(n, s, dt))
        A = attn_bufs(K, es, nacc=1)
        dsets = [dict(QT=sb("dQ%d" % i, [128, NT], BF16), KT=sb("dK%d" % i, [128, NT], BF16), VD=sb("dV%d" % i, [128, NTT, 129], BF16),
                      tab=sb("dtab%d" % i, [128, 21, 128], BF16), rq=Res(), rk=Res(), rv=Res(), rt=Res()) for i in range(2)]
        raw = sb("draw", [128, 21, 128], F32)
        Ost = [sb("dO%d" % i, [128, 128], BF16) for i in range(2)]
        rc = sb("drc", [128, 2], F32)
        r_raw, r_O, r_rc = Res(), [Res(), Res()], Res()
        on = 0

        def loadd(h):
            D_ = dsets[h % 2]
            T.dma("sp", raw[:], d["rpbtab"][:, h, :, :], writes=[r_raw])
            T.op("act", lambda: nc.scalar.activation(out=D_["tab"][:], in_=raw[:], func=AF.Exp), reads=[r_raw], writes=[D_["rt"]])
            T.dma("sp", D_["QT"][:], FT[ftmap[12 + h]], reads=[R("FT")], writes=[D_["rq"]])
            T.dma("sp", D_["KT"][:], FT[ftmap[20 + h]], reads=[R("FT")], writes=[D_["rk"]])
            T.op("dve", lambda: nc.vector.memset(D_["VD"][:, :, 128:129], 1.0), writes=[D_["rv"]])
            v0 = vvmap[28 + h]
            T.dma("sp", D_["VD"][:, :, 0:128], VV[:, v0 * 128:(v0 + 1) * 128].rearrange("(t p) c -> p t c", p=128),
                  reads=[R("VV")], writes=[D_["rv"]])
        loadd(0)
        for h in range(8):
            if h + 1 < 8:
                loadd(h + 1)
            D_ = dsets[h % 2]
            QT, KT, VD, tab = D_["QT"], D_["KT"], D_["VD"], D_["tab"]
            A["rq"], A["rk"], A["rv"], A["rt"] = D_["rq"], D_["rk"], D_["rv"], D_["rt"]
            for i in range(32):
                js, ids = sets[i]
                keys = [(KT[:, j * 128:(j + 1) * 128], VD[:, j, :], None) for j in (0, 1)]
                keys += [(KT[:, (2 + j) * 128:(3 + j) * 128], VD[:, 2 + j, :], tab[:, tid, :]) for j, tid in zip(js, ids)]
                ob = on % 2
                on += 1

                def finish(s, acc, r_acc, ob=ob):
                    T.op("dve", lambda: nc.vector.reciprocal(out=rc[:, 0:1], in_=acc[:, 128:129]), reads=[r_acc], writes=[r_rc])
                    T.op("dve", lambda: nc.vector.tensor_scalar(out=Ost[ob][:], in0=acc[:, 0:128], scalar1=rc[:, 0:1],
                                                                scalar2=None, op0=ALU.mult),
                         reads=[r_acc, r_rc], writes=[r_O[ob]])
                attn_group(K, A, QT[:, (2 + i) * 128:(3 + i) * 128], 128, keys, 129, finish)
                T.dma("sp", OO[(2 + i) * 128:(3 + i) * 128, 1024 + h * 128:1024 + (h + 1) * 128], Ost[ob][:],
                      reads=[r_O[ob]], writes=[R("OO")])


def phase_outproj(K, l):
    nc, T, d, R = K.nc, K.T, K.d, K.R
    first_tt = 0 if l == 0 else 2
    with ExitStack() as es:
        sb = lambda n, s, dt: es.enter_context(K.sbt(n, s, dt))
        W = sb("oW", [128, 16, D], BF16)
        gt = [sb("ogt%d" % i, [128, D], F32) for i in range(2)]
        Ot = [sb("oO%d" % i, [128, D], BF16) for i in range(2)]
        OT = [sb("oOT%d" % i, [128, 16, 128], BF16) for i in range(2)]
        xt = [sb("oxt%d" % i, [128, D], F32) for i in range(2)]
        xn = [sb("oxn%d" % i, [128, D], F32) for i in range(2)]
        tp = [es.enter_context(K.pst("otp%d" % i, [128, 1024], BF16)) for i in range(2)]
        ps = [es.enter_context(K.pst("ops%d" % i, [128, 512], F32)) for i in range(2)]
        r_W, r_gt, r_O, r_OT, r_x, r_xn = Res(), Res(), [Res(), Res()], [Res(), Res()], [Res(), Res()], [Res(), Res()]
        r_tp, r_ps = [Res(), Res()], [Res(), Res()]
        wv = d["w_out"][l].rearrange("(kc p) n -> p kc n", p=128)
        for nb in range(4):
            T.dma("pool", W[:, :, nb * 512:(nb + 1) * 512], wv[:, :, nb * 512:(nb + 1) * 512], writes=[r_W])
        for w in range(2):
            load_bc(K, gt[w][:], d["modv"][l, w:w + 1, 2 * D:3 * D], r_gt, reads=[R("modv")])
        tiles = list(range(first_tt, NTT))

        def load(tt, b):
            T.dma("sp", Ot[b][:], d["OO"][tt * 128:(tt + 1) * 128, :], reads=[R("OO")], writes=[r_O[b]])
            load_x_tile(K, l, tt, xt[b], r_x[b])
        load(tiles[0], 0)
        pn = [0]

        def do_tr(n):
            b = n % 2
            for g in range(2):
                for k8 in range(8):
                    kc = g * 8 + k8
                    T.op("pe", lambda kc=kc, k8=k8, g=g: nc.tensor.transpose(out=tp[g][:, k8 * 128:(k8 + 1) * 128],
                                                                           in_=Ot[b][:, kc * 128:(kc + 1) * 128],
                                                                           identity=K.ident_bf[:]),
                         reads=[r_O[b], R("ident")], writes=[r_tp[g]], inc=(k8 == 7))
                T.op("act", lambda g=g: nc.scalar.copy(out=OT[b][:, g * 8:(g + 1) * 8, :].rearrange("p a b -> p (a b)"), in_=tp[g][:]),
                     reads=[r_tp[g]], writes=[r_OT[b]])

        def do_mm(n, tt):
            b = n % 2
            w = 1 if tt < 2 else 0
            for nb in range(4):
                pb = pn[0] % 2
                pn[0] += 1
                for kc in range(16):
                    T.op("pe", lambda kc=kc: nc.tensor.matmul(ps[pb][:], lhsT=OT[b][:, kc, :], rhs=W[:, kc, nb * 512:(nb + 1) * 512],
                                                             start=(kc == 0), stop=(kc == 15)),
                         reads=[r_OT[b], r_W], writes=[r_ps[pb]], inc=(kc == 15))
                T.op("dve", lambda: nc.vector.tensor_tensor(out=xn[b][:, nb * 512:(nb + 1) * 512], in0=ps[pb][:],
                                                            in1=gt[w][:, nb * 512:(nb + 1) * 512], op=ALU.mult),
                     reads=[r_ps[pb], r_gt], writes=[r_xn[b]])
                T.op("dve", lambda: nc.vector.tensor_tensor(out=xn[b][:, nb * 512:(nb + 1) * 512], in0=xn[b][:, nb * 512:(nb + 1) * 512],
                                                            in1=xt[b][:, nb * 512:(nb + 1) * 512], op=ALU.add),
                     reads=[r_xn[b], r_x[b]], writes=[r_xn[b]])
                T.dma("sp", d["XS%d_%d" % (l, nb)][tt * 128:(tt + 1) * 128, :], xn[b][:, nb * 512:(nb + 1) * 512],
                      reads=[r_xn[b]], writes=[R("XS%d_%d" % (l, nb))])
        do_tr(0)
        for n, tt in enumerate(tiles):
            if n + 1 < len(tiles):
                load(tiles[n + 1], (n + 1) % 2)
                do_tr(n + 1)
            do_mm(n, tt)


def phase_route(K, l):
    nc, T, d, R = K.nc, K.T, K.d, K.R
    sets = [(2, NTT, CAP)] + ([(0, 2, CAPX)] if l == 0 else [])
    with ExitStack() as es:
        sb = lambda n, s, dt: es.enter_context(K.sbt(n, s, dt))
        affT = sb("raffT", [NE, NT], F32)
        maskT = sb("rmaskT", [NE, NT], BF16)
        junk = sb("rjunk", [NE, S], BF16)
        bs = sb("rbs", [NE, 4], F32)
        mask = sb("rmask", [128, NTT, NE], BF16)
        slot = sb("rslot", [128, NTT, NE], F32)
        tmp = sb("rtmp", [128, NTT, NE], F32)
        Rt = sb("rR", [128, NTT, NE, 5], BF16)
        g1 = sb("rg1", [128, NTT, NE], BF16)
        r1 = sb("rr1", [128, NTT, NE], F32)
        tri = sb("rtri", [128, 256], BF16)
        iota = sb("riota", [128, 512], F32)
        tokab = sb("rtok", [128, NTT, 2], BF16)
        OH = [sb("rOH%d" % i, [128, 512], BF16) for i in range(3)]
        accs = sb("raccs", [128, 4, 5], F32)
        acc5 = sb("racc5", [8, 512], F32)
        r_a5 = Res()
        idf = sb("ridf", [128, 4], F32)
        tp = es.enter_context(K.pst("rtp", [128, 512], F32))
        tpb = es.enter_context(K.pst("rtpb", [128, 1024], BF16))
        pp = [es.enter_context(K.pst("rpp%d" % i, [128, 512], F32)) for i in range(2)]
        ap_ = [es.enter_context(K.pst("rap%d" % i, [128, 512], F32)) for i in range(2)]
        r_affT, r_maskT, r_bs, r_mask, r_slot, r_R, r_c, r_tp, r_tpb = Res(), Res(), Res(), Res(), Res(), Res(), Res(), Res(), Res()
        r_pp, r_ap, r_OH, r_acc, r_j = [Res(), Res()], [Res(), Res()], [Res(), Res(), Res()], Res(), Res()
        T.dma("sp", tri[:], d["tri"], writes=[r_c])
        T.dma("sp", iota[:], d["iota"], writes=[r_c])
        T.dma("sp", tokab[:], d["tokab"], writes=[r_c])
        r_aff = K.R("aff")
        tiles_all = list(range(0 if l == 0 else 2, NTT))
        for tt in tiles_all:
            T.op("pe", lambda tt=tt: nc.tensor.transpose(out=tp[0:NE, 0:128], in_=K.aff[:, tt, :], identity=K.ident_f[:]),
                 reads=[r_aff, R("ident")], writes=[r_tp])
            T.op("dve", lambda tt=tt: nc.vector.tensor_copy(out=affT[:, tt * 128:(tt + 1) * 128], in_=tp[0:NE, 0:128]),
                 reads=[r_tp], writes=[r_affT])
        for (ta, tb_, cap) in sets:
            c0, c1 = ta * 128, tb_ * 128
            T.op("dve", lambda: nc.vector.memset(bs[:, 0:1], 0.0), writes=[r_bs])
            for it in range(24):
                wk = 2.0 ** -(it + 1)
                T.op("dve", lambda wk=wk: nc.vector.tensor_scalar(out=bs[:, 1:2], in0=bs[:, 0:1], scalar1=wk, scalar2=None, op0=ALU.add),
                     reads=[r_bs], writes=[r_bs])
                T.op("dve", lambda: nc.vector.tensor_scalar(out=junk[:, 0:c1 - c0], in0=affT[:, c0:c1], scalar1=bs[:, 1:2], scalar2=0.0,
                                                            op0=ALU.is_ge, op1=ALU.add, accum_out=bs[:, 2:3]),
                     reads=[r_bs, r_affT], writes=[r_bs, r_j])
                T.op("dve", lambda: nc.vector.tensor_scalar(out=bs[:, 3:4], in0=bs[:, 2:3], scalar1=float(cap) - 0.5, scalar2=None,
                                                            op0=ALU.is_ge),
                     reads=[r_bs], writes=[r_bs])
                T.op("dve", lambda wk=wk: nc.vector.scalar_tensor_tensor(out=bs[:, 0:1], in0=bs[:, 3:4], scalar=wk, in1=bs[:, 0:1],
                                                                         op0=ALU.mult, op1=ALU.add),
                     reads=[r_bs], writes=[r_bs])
            T.op("dve", lambda: nc.vector.tensor_scalar(out=maskT[:, c0:c1], in0=affT[:, c0:c1], scalar1=bs[:, 0:1], scalar2=None,
                                                        op0=ALU.is_ge),
                 reads=[r_bs, r_affT], writes=[r_maskT])
        for tt in tiles_all:
            T.op("pe", lambda tt=tt: nc.tensor.transpose(out=tpb[:, 0:NE], in_=maskT[:, tt * 128:(tt + 1) * 128],
                                                         identity=K.ident_bf[0:NE, 0:NE]),
                 reads=[r_maskT, R("ident")], writes=[r_tpb])
            T.op("dve", lambda tt=tt: nc.vector.tensor_copy(out=mask[:, tt, :], in_=tpb[:, 0:NE]), reads=[r_tpb], writes=[r_mask])
        pn = 0
        for (ta, tb_, cap) in sets:
            for j in range(ta, tb_):
                pb = pn % 2
                pn += 1
                prev = list(range(ta, j))
                for ii, i in enumerate(prev):
                    T.op("pe", lambda i=i, ii=ii: nc.tensor.matmul(pp[pb][:, 0:NE], lhsT=tri[:, 0:128], rhs=mask[:, i, :],
                                                                  start=(ii == 0), stop=False),
                         reads=[r_mask, r_c], writes=[r_pp[pb]], inc=False)
                T.op("pe", lambda j=j: nc.tensor.matmul(pp[pb][:, 0:NE], lhsT=tri[:, 128:256], rhs=mask[:, j, :],
                                                       start=(len(prev) == 0), stop=True),
                     reads=[r_mask, r_c], writes=[r_pp[pb]])
                T.op("dve", lambda j=j: nc.vector.tensor_scalar(out=tmp[:, j, :], in0=mask[:, j, :], scalar1=-1.0, scalar2=10000.0,
                                                                op0=ALU.add, op1=ALU.mult),
                     reads=[r_mask], writes=[r_slot])
                T.op("dve", lambda j=j: nc.vector.tensor_tensor(out=slot[:, j, :], in0=tmp[:, j, :], in1=pp[pb][:, 0:NE], op=ALU.add),
                     reads=[r_pp[pb], r_slot], writes=[r_slot])
        T.op("dve", lambda: nc.vector.tensor_copy(out=Rt[:, :, :, 0:2], in_=tokab[:].unsqueeze(2).to_broadcast([128, NTT, NE, 2])),
             reads=[r_c], writes=[r_R])
        T.op("dve", lambda: nc.vector.tensor_copy(out=g1[:], in_=K.aff[:]), reads=[r_aff], writes=[r_R])
        T.op("dve", lambda: nc.vector.tensor_copy(out=Rt[:, :, :, 2], in_=g1[:]), reads=[r_R], writes=[r_R])
        T.op("dve", lambda: nc.vector.tensor_tensor(out=r1[:], in0=K.aff[:], in1=g1[:], op=ALU.subtract), reads=[r_aff, r_R], writes=[r_R])
        T.op("dve", lambda: nc.vector.tensor_copy(out=g1[:], in_=r1[:]), reads=[r_R], writes=[r_R])
        T.op("dve", lambda: nc.vector.tensor_copy(out=Rt[:, :, :, 3], in_=g1[:]), reads=[r_R], writes=[r_R])
        T.op("dve", lambda: nc.vector.tensor_tensor(out=r1[:], in0=r1[:], in1=g1[:], op=ALU.subtract), reads=[r_R], writes=[r_R])
        T.op("dve", lambda: nc.vector.tensor_copy(out=Rt[:, :, :, 4], in_=r1[:]), reads=[r_R], writes=[r_R])
        on = 0
        an = 0
        for (ta, tb_, cap) in sets:
            nsc = (cap + 127) // 128
            wid = min(cap, 512)
            for e in range(NE):
                ab = an % 2
                an += 1
                for j in range(ta, tb_):
                    ob = on % 3
                    on += 1
                    T.op("dve", lambda j=j, e=e: nc.vector.tensor_scalar(out=OH[ob][:, 0:wid], in0=iota[:, 0:wid],
                                                                          scalar1=slot[:, j, e:e + 1], scalar2=None, op0=ALU.is_equal),
                         reads=[r_slot, r_c], writes=[r_OH[ob]])
                    T.op("pe", lambda j=j, e=e: nc.tensor.matmul(ap_[ab][0:5, 0:wid], lhsT=Rt[:, j, e, :], rhs=OH[ob][:, 0:wid],
                                                                 start=(j == ta), stop=(j == tb_ - 1)),
                         reads=[r_OH[ob], r_R], writes=[r_ap[ab]], inc=True)
                T.op("dve", lambda: nc.vector.tensor_copy(out=acc5[0:5, 0:wid], in_=ap_[ab][0:5, 0:wid]), reads=[r_ap[ab]], writes=[r_a5])
                for sc in range(nsc):
                    m = min(128, cap - sc * 128)
                    T.op("pe", lambda sc=sc, m=m: nc.tensor.transpose(out=tp[0:m, sc * 8:sc * 8 + 5], in_=acc5[0:5, sc * 128:sc * 128 + m],
                                                                      identity=K.ident_f[0:5, 0:5]),
                         reads=[r_a5, R("ident")], writes=[r_tp])
                m = min(128, cap)
                av = tp[0:m, 0:32].rearrange("p (s c) -> p s c", c=8)
                T.op("dve", lambda: nc.vector.tensor_copy(out=accs[0:m, 0:nsc, :], in_=av[:, 0:nsc, 0:5]), reads=[r_tp], writes=[r_acc])
                T.op("dve", lambda: nc.vector.scalar_tensor_tensor(out=idf[0:m, 0:nsc], in0=accs[0:m, 0:nsc, 0], scalar=64.0,
                                                                   in1=accs[0:m, 0:nsc, 1], op0=ALU.mult, op1=ALU.add),
                     reads=[r_acc], writes=[r_acc])
                if cap == CAP:
                    idst, gdst = K.idx[:, e, :], K.gate[:, e, :]
                else:
                    idst, gdst = K.idxx[0:m, e:e + 1], K.gatex[0:m, e:e + 1]
                T.op("dve", lambda: nc.vector.tensor_copy(out=idst, in_=idf[0:m, 0:nsc]), reads=[r_acc], writes=[K.R("idx")])
                T.op("dve", lambda: nc.vector.tensor_tensor(out=gdst, in0=accs[0:m, 0:nsc, 2], in1=accs[0:m, 0:nsc, 3], op=ALU.add),
                     reads=[r_acc], writes=[K.R("idx")])
                T.op("dve", lambda: nc.vector.tensor_tensor(out=gdst, in0=gdst, in1=accs[0:m, 0:nsc, 4], op=ALU.add),
                     reads=[r_acc, K.R("idx")], writes=[K.R("idx")])


def phase_moe(K, l):
    nc, T, d, R = K.nc, K.T, K.d, K.R
    has_ctx = (l == 0)
    NS = CAP + (CAPX if has_ctx else 0)
    experts = K.dbg.get("experts", list(range(NE)))
    with ExitStack() as es:
        sb = lambda n, s, dt: es.enter_context(K.sbt(n, s, dt))
        NR = 6
        ring = [sb("mring%d" % i, [128, 16, 512], BF16) for i in range(NR)]
        xe = sb("mxe", [128, 4, D], BF16)
        xex = sb("mxex", [128, D], BF16)
        xeT = sb("mxeT", [128, 16, CAP + CAPX], BF16)
        hidT = sb("mhidT", [128, 12, CAP + CAPX], BF16)
        gt = [sb("mgt%d" % i, [128, D], F32) for i in range(2)]
        s1 = [sb("ms1%d" % i, [128, 512], F32) for i in range(2)]
        s1x = sb("ms1x", [128, 32], F32)
        yst = [sb("myst%d" % i, [128, 512], F32) for i in range(4)]
        GU = [es.enter_context(K.pst("mGU%d" % i, [128, 512], F32)) for i in range(4)]
        GX = es.enter_context(K.pst("mGX", [128, 512], F32))
        tp = [es.enter_context(K.pst("mtp%d" % i, [128, 1024], BF16)) for i in range(2)]
        r_ring = [Res() for _ in range(NR)]
        r_xe, r_xeT, r_hid, r_gt = Res(), Res(), Res(), Res()
        r_s1, r_s1x, r_y = [Res(), Res()], Res(), [Res() for _ in range(4)]
        r_GU, r_GX, r_tp = [Res() for _ in range(4)], Res(), [Res(), Res()]
        r_idx = K.R("idx")
        for w in range(2):
            load_bc(K, gt[w][:], d["modv"][l, w:w + 1, 5 * D:6 * D], r_gt, reads=[R("modv")])
        pieces = []
        for e in experts:
            for fb in range(3):
                pieces.append(("g", e, fb))
                pieces.append(("u", e, fb))
            for nb in range(4):
                pieces.append(("d", e, nb))
        pstate = dict(next=0)

        def issue_piece():
            i = pstate["next"]
            if i >= len(pieces):
                return
            kind, e, j = pieces[i]
            slot = i % NR
            if kind == "d":
                src = d["w_down"][l, e].rearrange("(fc p) n -> p fc n", p=128)[:, :, j * 512:(j + 1) * 512]
                T.dma("pool", ring[slot][:, 0:12, :], src, writes=[r_ring[slot]])
            else:
                wn = "w_gate" if kind == "g" else "w_up"
                src = d[wn][l, e].rearrange("(kc p) n -> p kc n", p=128)[:, :, j * 512:(j + 1) * 512]
                T.dma("pool", ring[slot][:], src, writes=[r_ring[slot]])
            pstate["next"] = i + 1

        def gather(e):
            for sc in range(4):
                T.dma("pool", xe[:, sc, :], d["H2"], reads=[R("H2"), r_idx], writes=[r_xe],
                      indirect=dict(out_offset=None, in_offset=bass.IndirectOffsetOnAxis(ap=K.idx[:, e, sc:sc + 1], axis=0)))
            if has_ctx:
                T.dma("pool", xex[0:CAPX, :], d["H2"], reads=[R("H2"), r_idx], writes=[r_xe],
                      indirect=dict(out_offset=None, in_offset=bass.IndirectOffsetOnAxis(ap=K.idxx[0:CAPX, e:e + 1], axis=0)))

        def transposes(e):
            tn = 0
            for sc in range(4):
                for g in range(2):
                    tb = tn % 2
                    tn += 1
                    for k8 in range(8):
                        kc = g * 8 + k8
                        T.op("pe", lambda kc=kc, k8=k8: nc.tensor.transpose(out=tp[tb][:, k8 * 128:(k8 + 1) * 128],
                                                                          in_=xe[:, sc, kc * 128:(kc + 1) * 128], identity=K.ident_bf[:]),
                             reads=[r_xe, R("ident")], writes=[r_tp[tb]], inc=(k8 == 7))
                    eng = "act" if g == 0 else "dve"
                    src = tp[tb][:].rearrange("p (a b) -> p a b", b=128)
                    dst = xeT[:, g * 8:(g + 1) * 8, sc * 128:(sc + 1) * 128]
                    if eng == "act":
                        T.op("act", lambda: nc.scalar.copy(out=dst, in_=src), reads=[r_tp[tb]], writes=[r_xeT])
                    else:
                        T.op("dve", lambda: nc.vector.tensor_copy(out=dst, in_=src), reads=[r_tp[tb]], writes=[r_xeT])
            if has_ctx:
                tb = tn % 2
                for kc in range(16):
                    T.op("pe", lambda kc=kc: nc.tensor.transpose(out=tp[tb][:, kc * 32:(kc + 1) * 32],
                                                                 in_=xex[0:CAPX, kc * 128:(kc + 1) * 128],
                                                                 identity=K.ident_bf[0:CAPX, 0:CAPX]),
                         reads=[r_xe, R("ident")], writes=[r_tp[tb]], inc=(kc == 15))
                T.op("dve", lambda: nc.vector.tensor_copy(out=xeT[:, :, CAP:CAP + CAPX],
                                                          in_=tp[tb][:, 0:512].rearrange("p (a b) -> p a b", b=32)),
                     reads=[r_tp[tb]], writes=[r_xeT])

        def ensure(upto):
            while pstate["next"] <= min(upto, len(pieces) - 1):
                issue_piece()
        pi = 0
        gn = 0
        yn = 0
        prev_sc = [[] for _ in range(4)]
        gather(experts[0])
        transposes(experts[0])
        for ei, e in enumerate(experts):
            if ei + 1 < len(experts):
                gather(experts[ei + 1])
            for fb in range(3):
                ensure(pi + NR - 1)
                sg, su = pi % NR, (pi + 1) % NR
                pi += 2
                for f4 in range(4):
                    fc = fb * 4 + f4
                    gb = gn % 2
                    gn += 1
                    G, U = GU[gb], GU[2 + gb]
                    for (ps_, slot_, rr) in ((G, sg, r_GU[gb]), (U, su, r_GU[2 + gb])):
                        for kc in range(16):
                            T.op("pe", lambda kc=kc, ps_=ps_, slot_=slot_: nc.tensor.matmul(
                                ps_[:, 0:CAP], lhsT=ring[slot_][:, kc, f4 * 128:(f4 + 1) * 128], rhs=xeT[:, kc, 0:CAP],
                                start=(kc == 0), stop=(kc == 15)),
                                reads=[r_ring[slot_], r_xeT], writes=[rr], inc=(kc == 15))
                    if has_ctx:
                        for ci, slot_ in enumerate((sg, su)):
                            for kc in range(16):
                                T.op("pe", lambda kc=kc, ci=ci, slot_=slot_: nc.tensor.matmul(
                                    GX[:, ci * 32:(ci + 1) * 32], lhsT=ring[slot_][:, kc, f4 * 128:(f4 + 1) * 128],
                                    rhs=xeT[:, kc, CAP:CAP + CAPX], start=(kc == 0), stop=(kc == 15)),
                                    reads=[r_ring[slot_], r_xeT], writes=[r_GX], inc=(kc == 15))
                    sbuf_ = gn % 2
                    T.op("act", lambda: nc.scalar.activation(out=s1[sbuf_][:], in_=G[:, 0:CAP], func=AF.Silu),
                         reads=[r_GU[gb]], writes=[r_s1[sbuf_]])
                    T.op("dve", lambda: nc.vector.tensor_tensor(out=hidT[:, fc, 0:CAP], in0=s1[sbuf_][:], in1=U[:, 0:CAP], op=ALU.mult),
                         reads=[r_s1[sbuf_], r_GU[2 + gb]], writes=[r_hid])
                    if has_ctx:
                        T.op("act", lambda: nc.scalar.activation(out=s1x[:], in_=GX[:, 0:32], func=AF.Silu),
                             reads=[r_GX], writes=[r_s1x])
                        T.op("dve", lambda: nc.vector.tensor_tensor(out=hidT[:, fc, CAP:CAP + CAPX], in0=s1x[:], in1=GX[:, 32:64], op=ALU.mult),
                             reads=[r_s1x, r_GX], writes=[r_hid])
            if ei + 1 < len(experts):
                transposes(experts[ei + 1])
            new_sc = [[] for _ in range(4)]
            for nb in range(4):
                ensure(pi + NR - 1)
                sd = pi % NR
                pi += 1
                for st_ in range(5 if has_ctx else 4):
                    m = 128 if st_ < 4 else CAPX
                    yb = yn % 4
                    yn += 1
                    Y = GU[yb]
                    for fc in range(12):
                        T.op("pe", lambda fc=fc: nc.tensor.matmul(Y[0:m, :], lhsT=hidT[:, fc, st_ * 128:st_ * 128 + m],
                                                                 rhs=ring[sd][:, fc, :], start=(fc == 0), stop=(fc == 11)),
                             reads=[r_hid, r_ring[sd]], writes=[r_GU[yb]], inc=(fc == 11))
                    if st_ < 4:
                        gsc, gtt, iap = K.gate[:, e, st_:st_ + 1], gt[0], K.idx[:, e, st_:st_ + 1]
                    else:
                        gsc, gtt, iap = K.gatex[0:m, e:e + 1], gt[1], K.idxx[0:m, e:e + 1]
                    T.op("dve", lambda: nc.vector.scalar_tensor_tensor(out=yst[yb][0:m, :], in0=Y[0:m, :], scalar=gsc,
                                                                       in1=gtt[0:m, nb * 512:(nb + 1) * 512], op0=ALU.mult, op1=ALU.mult),
                         reads=[r_GU[yb], r_idx, r_gt], writes=[r_y[yb]])
                    xsn = "XS%d_%d" % (l, nb)
                    tok = T.dma("pool", d[xsn], yst[yb][0:m, :], reads=[r_y[yb], r_idx, R(xsn)], writes=[],
                                extra_waits=prev_sc[nb],
                                indirect=dict(out_offset=bass.IndirectOffsetOnAxis(ap=iap, axis=0), in_offset=None,
                                              compute_op=ALU.add))
                    new_sc[nb].append(tok)
            prev_sc = new_sc
        for nb in range(4):
            rr = R("XS%d_%d" % (l, nb))
            for tok in prev_sc[nb]:
                rr.r[tok[0]] = tok


def phase_final(K):
    nc, T, d, R = K.nc, K.T, K.d, K.R
    l = 1
    with ExitStack() as es:
        sb = lambda n, s, dt: es.enter_context(K.sbt(n, s, dt))
        gb = sb("fgb", [128, D], F32)
        xt = [sb("fxt%d" % i, [128, D], F32) for i in range(2)]
        yo = [sb("fyo%d" % i, [128, D], F32) for i in range(2)]
        junk = sb("fjunk", [128, D], BF16)
        st = sb("fst", [128, 4], F32)
        r_g, r_x, r_y, r_st, r_j = Res(), [Res(), Res()], [Res(), Res()], Res(), Res()
        load_bc(K, gb[:], d["gvec"][4:5, :], r_g)
        tiles = list(range(2, NTT))

        def load(tt, b):
            for nb in range(4):
                T.dma("sp", xt[b][:, nb * 512:(nb + 1) * 512], d["XS%d_%d" % (l, nb)][tt * 128:(tt + 1) * 128, :],
                      reads=[R("XS%d_%d" % (l, nb))], writes=[r_x[b]])
        load(tiles[0], 0)
        for n, tt in enumerate(tiles):
            b = n % 2
            if n + 1 < len(tiles):
                load(tiles[n + 1], (n + 1) % 2)
            T.op("act", lambda: nc.scalar.activation(out=junk[:], in_=xt[b][:], func=AF.Square, accum_out=st[:, 0:1]),
                 reads=[r_x[b]], writes=[r_j, r_st])
            T.op("act", lambda: nc.scalar.activation(out=st[:, 1:2], in_=st[:, 0:1], func=AF.Sqrt, scale=1.0 / D, bias=EPS),
                 reads=[r_st], writes=[r_st])
            T.op("dve", lambda: nc.vector.reciprocal(out=st[:, 2:3], in_=st[:, 1:2]), reads=[r_st], writes=[r_st])
            T.op("dve", lambda: nc.vector.scalar_tensor_tensor(out=yo[b][:], in0=xt[b][:], scalar=st[:, 2:3], in1=gb[:],
                                                               op0=ALU.mult, op1=ALU.mult),
                 reads=[r_x[b], r_st, r_g], writes=[r_y[b]])
            T.dma("sp", d["out"][(tt - 2) * 128:(tt - 1) * 128, :], yo[b][:], reads=[r_y[b]], writes=[R("out")])


def make_in_maps(inputs, cores):
    c = const_pack()
    f = lambda a: np.ascontiguousarray(np.asarray(a, dtype=np.float32))
    small = np.zeros((1, 1024), np.float32)
    small[0, 0:8] = f(inputs["a_sink"])[0]
    small[0, 8:136] = f(inputs["b_lam_q1"])[0]
    small[0, 136:264] = f(inputs["b_lam_k1"])[0]
    small[0, 264:392] = f(inputs["b_lam_q2"])[0]
    small[0, 392:520] = f(inputs["b_lam_k2"])[0]
    small[0, 520:776] = f(inputs["b_subln"])[0]
    small2 = np.concatenate([f(inputs["c_q_norm"])[0], f(inputs["c_k_norm"])[0]])[None, :]
    gvec = np.stack([f(inputs["g_mix"])[0], f(inputs["g_mix"])[1], f(inputs["g_ffn"])[0], f(inputs["g_ffn"])[1],
                     f(inputs["g_final"])], axis=0)
    rpbtab = d_tables(f(inputs["d_rpb"])[0])
    shared = dict(
        w_ada=f(inputs["w_ada"]), b_ada=f(inputs["b_ada"]), gvec=np.ascontiguousarray(gvec), w_in=f(inputs["w_in"]),
        w_out=f(inputs["w_out"]), w_router=f(inputs["w_router"]), w_gate=f(inputs["w_gate"]), w_up=f(inputs["w_up"]),
        w_down=f(inputs["w_down"]), small=small, small2=np.ascontiguousarray(small2), rpbtab=rpbtab, **c)
    maps = []
    x = inputs["x"]
    ctx = inputs["ctx"]
    cc = f(inputs["c"])
    c_ctx = f(inputs["c_ctx"])
    for b in cores:
        m = dict(shared)
        m["x"] = f(x[b])
        m["ctx"] = f(ctx[b])
        m["cvec"] = np.ascontiguousarray(np.stack([cc[b], c_ctx], axis=0))
        maps.append(m)
    return maps


def kernel(**inputs):
    nc = build_program()
    maps = make_in_maps(inputs, list(range(8)))
    res = run_bass_kernel_spmd(nc, maps, core_ids=list(range(8)))
    out = np.stack([np.asarray(r["out"], dtype=np.float32) for r in res.results], axis=0)
    return out
```

```python
import numpy as np
import ml_dtypes
from contextlib import ExitStack
import concourse.bass as bass
import concourse.mybir as mybir
from concourse.bass_utils import run_bass_kernel_spmd

F32 = mybir.dt.float32
BF16 = mybir.dt.bfloat16
I32 = mybir.dt.int32
AF = mybir.ActivationFunctionType
ALU = mybir.AluOpType

D = 2048
S = 4096
CTX = 256
NT = S + CTX
NTT = NT // 128
INW = 4608
NE = 16
DE = 1536
CAP = 512
CAPX = 32
HD = 128
EPS = 1e-6
SCALE = HD ** -0.5
GRID_W = 64
NBF16 = ml_dtypes.bfloat16


def lambda_init_for(layer):
    import math
    return 0.8 - 0.6 * math.exp(-0.3 * layer)


class Res:
    __slots__ = ("w", "r")

    def __init__(self):
        self.w = None
        self.r = {}


class Trk:
    def __init__(self, nc, es):
        self.nc = nc
        self.eng = {}
        for name, e in (("pe", nc.tensor), ("act", nc.scalar), ("dve", nc.vector),
                        ("pool", nc.gpsimd), ("sp", nc.sync)):
            sem = es.enter_context(nc.semaphore("s_" + name)) if name != "sp" else None
            self.eng[name] = dict(e=e, sem=sem, n=0, waited={}, name=name)
        self.dp = {}
        for q, n in (("sp", 28), ("pool", 28), ("act", 6)):
            self.dp[q] = dict(sems=[es.enter_context(nc.semaphore("d_%s_%d" % (q, i))) for i in range(n)],
                              cnt=[0] * n, nxt=0)

    def _wait(self, en, tok):
        if tok is None:
            return
        key, sem, val = tok
        E = self.eng[en]
        if en == "pe" and key == "pe":
            return
        if E["waited"].get(key, 0) >= val:
            return
        E["e"].wait_ge(sem, val)
        E["waited"][key] = val

    def _deps(self, en, reads, writes):
        for r in reads:
            self._wait(en, r.w)
        for w in writes:
            self._wait(en, w.w)
            for t in list(w.r.values()):
                self._wait(en, t)

    def _upd(self, tok, reads, writes):
        for r in reads:
            old = r.r.get(tok[0])
            if old is None or old[2] < tok[2]:
                r.r[tok[0]] = tok
        for w in writes:
            w.w = tok
            w.r = {}

    def op(self, en, fn, reads=(), writes=(), inc=True):
        E = self.eng[en]
        self._deps(en, reads, writes)
        ins = fn()
        if inc:
            E["n"] += 1
            ins.then_inc(E["sem"], 1)
            tok = (en, E["sem"], E["n"])
        else:
            tok = (en, E["sem"], E["n"] + 1)
        self._upd(tok, reads, writes)
        return tok

    def dma(self, q, out, in_, reads=(), writes=(), indirect=None, extra_waits=(), **kw):
        P = self.dp[q]
        i = P["nxt"]
        P["nxt"] = (i + 1) % len(P["sems"])
        key = "d_%s_%d" % (q, i)
        sem = P["sems"][i]
        if P["cnt"][i] > 0:
            self._wait(q, (key, sem, P["cnt"][i]))
        self._deps(q, reads, writes)
        for t in extra_waits:
            self._wait(q, t)
        E = self.eng[q]
        if indirect is None:
            ins = E["e"].dma_start(out=out, in_=in_, **kw)
        else:
            ins = E["e"].indirect_dma_start(out=out, in_=in_, **indirect)
        P["cnt"][i] += 16
        ins.then_inc(sem, 16)
        tok = (key, sem, P["cnt"][i])
        self._upd(tok, reads, writes)
        return tok

    def all_tokens(self):
        toks = []
        for name, E in self.eng.items():
            if E["sem"] is not None and E["n"] > 0:
                toks.append((name, E["sem"], E["n"]))
        for q, P in self.dp.items():
            for i, s in enumerate(P["sems"]):
                if P["cnt"][i] > 0:
                    toks.append(("d_%s_%d" % (q, i), s, P["cnt"][i]))
        return toks

    def barrier(self):
        toks = self.all_tokens()
        for en in ("pe", "act", "dve", "pool", "sp"):
            for t in toks:
                self._wait(en, t)


class Ctx:
    pass


def rope_tables():
    t = np.arange(S)
    row = (t // GRID_W).astype(np.float32)
    col = (t % GRID_W).astype(np.float32)
    nf = HD // 4
    inv = (10000.0 ** (-np.arange(nf, dtype=np.float32) / nf)).astype(np.float32)
    ar = row[:, None] * inv
    ac = col[:, None] * inv
    ang = np.concatenate([ar, ar, ac, ac], axis=-1).astype(np.float32)
    cos = np.cos(ang).astype(np.float32)
    sin = np.sin(ang).astype(np.float32)
    sgn = np.ones(HD, np.float32)
    sgn[0:32] = -1.0
    sgn[64:96] = -1.0
    tab = np.zeros((NT, 2, HD), np.float32)
    tab[:CTX, 0, :] = 1.0
    tab[CTX:, 0, :] = cos
    tab[CTX:, 1, :] = sin * sgn
    return np.ascontiguousarray(tab.reshape(NTT, 128, 2 * HD).transpose(1, 0, 2))


def d_tile_sets():
    out = []
    for i in range(32):
        if i == 0:
            js = [0, 1, 2, 3]
            ids = [5 + k for k in range(4)]
        elif i == 1:
            js = [0, 1, 2, 3]
            ids = [9 + k for k in range(4)]
        elif i == 30:
            js = [28, 29, 30, 31]
            ids = [13 + k for k in range(4)]
        elif i == 31:
            js = [28, 29, 30, 31]
            ids = [17 + k for k in range(4)]
        else:
            js = [i - 2, i - 1, i, i + 1, i + 2]
            ids = [0, 1, 2, 3, 4]
        out.append((js, ids))
    return out


def d_tables(rpb):
    tabs = np.full((8, 21, 128, 128), -1e30, np.float32)
    sets = d_tile_sets()
    done = set()
    kl = np.arange(128)
    ql = np.arange(128)
    for i in (2, 0, 1, 30, 31):
        js, ids = sets[i]
        for j, tid in zip(js, ids):
            if tid in done:
                continue
            done.add(tid)
            kr = 2 * j + kl // 64
            kc = kl % 64
            qr = 2 * i + ql // 64
            qc = ql % 64
            rs = np.clip(qr - 4, 0, 56)
            cs = np.clip(qc - 8, 0, 48)
            vis = ((kr[:, None] >= rs[None, :]) & (kr[:, None] < rs[None, :] + 8)
                   & (kc[:, None] >= cs[None, :]) & (kc[:, None] < cs[None, :] + 16))
            ridx = np.clip(kr[:, None] - qr[None, :] + 7, 0, 14)
            cidx = np.clip(kc[:, None] - qc[None, :], -15, 15) + 15
            for h in range(8):
                vals = rpb[h][ridx, cidx]
                tabs[h, tid] = np.where(vis, vals, np.float32(-1e30))
    return np.ascontiguousarray(tabs.transpose(2, 0, 1, 3))


def const_pack():
    c = {}
    c["ident_bf"] = np.eye(128, dtype=np.float32).astype(NBF16)
    c["ident_f"] = np.eye(128, dtype=np.float32)
    c["ropetab"] = rope_tables()
    k = np.arange(128)[:, None]
    q = np.arange(128)[None, :]
    mu = (k >= q).astype(np.float32)
    ml = (k <= q).astype(np.float32)
    bm = np.zeros((128, 2, 4, 128), np.float32)
    bm[:, 0] = mu[:, None, :]
    bm[:, 1] = ml[:, None, :]
    c["bandmask"] = bm.astype(NBF16)
    c["tri"] = np.concatenate([np.ones((128, 128), np.float32),
                               (k < q).astype(np.float32)], axis=1).astype(NBF16)
    c["iota"] = np.tile(np.arange(512, dtype=np.float32)[None, :], (128, 1))
    rows = (np.arange(NTT)[None, :] * 128 + np.arange(128)[:, None])
    tk = np.zeros((128, NTT, 2), np.float32)
    tk[:, :, 0] = rows // 64
    tk[:, :, 1] = rows % 64
    c["tokab"] = tk.astype(NBF16)
    return c


EVEN_TYPES = ["R"] * 8 + ["R"] * 2 + ["V"] * 2 + ["R"] * 8 + ["R"] * 8 + ["V"] * 8
ODD_TYPES = ["NQ"] * 8 + ["NK"] * 2 + ["V"] * 2 + ["P"] * 8 + ["P"] * 8 + ["V"] * 8


def block_maps(types):
    ft, vv = {}, {}
    for b, t in enumerate(types):
        if t == "V":
            vv[b] = len(vv)
        else:
            ft[b] = len(ft)
    return ft, vv


def build_program(dbg=None):
    dbg = dbg or {}
    kinds = dbg.get("kinds", {})
    nc = bass.Bass("TRN2", target_bir_lowering=False)
    K = Ctx()
    K.nc = nc
    K.dbg = dbg
    _cnt = [0]

    def _sbt(name, shape, dt):
        _cnt[0] += 1
        return nc.sbuf_tensor("%s_%d" % (name, _cnt[0]), shape, dt)

    def _pst(name, shape, dt):
        _cnt[0] += 1
        return nc.psum_tensor("%s_%d" % (name, _cnt[0]), shape, dt)
    K.sbt = _sbt
    K.pst = _pst

    def dram(name, shape, dt, kind):
        shape = dbg.get("shapes", {}).get(name, shape)
        return nc.dram_tensor(name, list(shape), dt, kind=kinds.get(name, kind)).ap()

    d = {}
    K.d = d
    d["x"] = dram("x", [S, D], F32, "ExternalInput")
    d["ctx"] = dram("ctx", [CTX, D], F32, "ExternalInput")
    d["cvec"] = dram("cvec", [2, D], F32, "ExternalInput")
    d["w_ada"] = dram("w_ada", [2, D, 6 * D], F32, "ExternalInput")
    d["b_ada"] = dram("b_ada", [2, 6 * D], F32, "ExternalInput")
    d["gvec"] = dram("gvec", [5, D], F32, "ExternalInput")
    d["w_in"] = dram("w_in", [2, D, INW], F32, "ExternalInput")
    d["w_out"] = dram("w_out", [2, D, D], F32, "ExternalInput")
    d["w_router"] = dram("w_router", [2, D, NE], F32, "ExternalInput")
    d["w_gate"] = dram("w_gate", [2, NE, D, DE], F32, "ExternalInput")
    d["w_up"] = dram("w_up", [2, NE, D, DE], F32, "ExternalInput")
    d["w_down"] = dram("w_down", [2, NE, DE, D], F32, "ExternalInput")
    d["small"] = dram("small", [1, 1024], F32, "ExternalInput")
    d["small2"] = dram("small2", [1, 256], F32, "ExternalInput")
    d["rpbtab"] = dram("rpbtab", [128, 8, 21, 128], F32, "ExternalInput")
    d["ident_bf"] = dram("ident_bf", [128, 128], BF16, "ExternalInput")
    d["ident_f"] = dram("ident_f", [128, 128], F32, "ExternalInput")
    d["ropetab"] = dram("ropetab", [128, NTT, 2 * HD], F32, "ExternalInput")
    d["bandmask"] = dram("bandmask", [128, 2, 4, 128], BF16, "ExternalInput")
    d["tri"] = dram("tri", [128, 256], BF16, "ExternalInput")
    d["iota"] = dram("iota", [128, 512], F32, "ExternalInput")
    d["tokab"] = dram("tokab", [128, NTT, 2], BF16, "ExternalInput")
    d["out"] = dram("out", [S, D], F32, "ExternalOutput")
    d["modv"] = dram("modv", [2, 2, 6 * D], F32, "Internal")
    d["hT"] = dram("hT", [NTT, 128, 16 * 128], BF16, "Internal")
    d["FT"] = dram("FT", [26, 128, NT], BF16, "Internal")
    d["VV"] = dram("VV", [NT, 10 * 128], BF16, "Internal")
    d["OO"] = dram("OO", [NT, D], BF16, "Internal")
    d["H2"] = dram("H2", [NT, D], BF16, "Internal")
    for l in range(2):
        for nb in range(4):
            d["XS%d_%d" % (l, nb)] = dram("XS%d_%d" % (l, nb), [NT, 512], F32, "Internal")
    K.rs = {}

    def R(name):
        if name not in K.rs:
            K.rs[name] = Res()
        return K.rs[name]
    K.R = R

    with ExitStack() as es:
        T = Trk(nc, es)
        K.T = T
        K.ident_bf = es.enter_context(K.sbt("ident_bf_s", [128, 128], BF16))
        K.ident_f = es.enter_context(K.sbt("ident_f_s", [128, 128], F32))
        K.aff = es.enter_context(K.sbt("aff_tm", [128, NTT, NE], F32))
        K.idx = es.enter_context(K.sbt("idx_i", [128, NE, 4], I32))
        K.gate = es.enter_context(K.sbt("gate_f", [128, NE, 4], F32))
        K.idxx = es.enter_context(K.sbt("idxx_i", [128, NE], I32))
        K.gatex = es.enter_context(K.sbt("gatex_f", [128, NE], F32))
        T.dma("sp", K.ident_bf[:], d["ident_bf"], writes=[R("ident")])
        T.dma("sp", K.ident_f[:], d["ident_f"], writes=[R("ident")])

        phases = dbg.get("phases")

        def want(p):
            return phases is None or p in phases

        for l in dbg.get("layers", [0, 1]):
            if want("mod"):
                with nc.named_scope("mod%d" % l):
                    phase_mod(K, l)
                T.barrier()
            if want("norm1"):
                with nc.named_scope("norm1%d" % l):
                    phase_norm(K, l, 1)
                T.barrier()
            if want("proj"):
                with nc.named_scope("proj%d" % l):
                    phase_proj(K, l)
                T.barrier()
            if want("attn"):
                with nc.named_scope("attn%d" % l):
                    phase_attn(K, l)
                T.barrier()
            if want("outproj"):
                with nc.named_scope("outproj%d" % l):
                    phase_outproj(K, l)
                T.barrier()
            if want("norm2"):
                with nc.named_scope("norm2%d" % l):
                    phase_norm(K, l, 2)
                T.barrier()
            if want("route"):
                with nc.named_scope("route%d" % l):
                    phase_route(K, l)
                T.barrier()
            if want("moe"):
                with nc.named_scope("moe%d" % l):
                    phase_moe(K, l)
                T.barrier()
        if want("final"):
            phase_final(K)
        T.barrier()
        if dbg.get("dump"):
            for nm_, tl, shp, dt_ in (("dbg_aff", K.aff, [128, NTT * NE], F32), ("dbg_idx", K.idx, [128, NE * 4], I32),
                                     ("dbg_gate", K.gate, [128, NE * 4], F32), ("dbg_idxx", K.idxx, [128, NE], I32),
                                     ("dbg_gatex", K.gatex, [128, NE], F32)):
                o = nc.dram_tensor(nm_, shp, dt_, kind="ExternalOutput").ap()
                src = tl[:]
                if len(src.shape) == 3:
                    src = src.rearrange("p a b -> p (a b)")
                T.dma("sp", o, src)
            T.barrier()
    return nc


def xs_src(K, l, tt, nb):
    d = K.d
    if l == 0:
        if tt < 2:
            return d["ctx"][tt * 128:(tt + 1) * 128, nb * 512:(nb + 1) * 512]
        return d["x"][(tt - 2) * 128:(tt - 1) * 128, nb * 512:(nb + 1) * 512]
    return d["XS%d_%d" % (l - 1, nb)][tt * 128:(tt + 1) * 128, :]


def load_x_tile(K, l, tt, dst, res, stream_res=None):
    T = K.T
    for nb in range(4):
        rd = [K.R("XS%d_%d" % (l - 1, nb))] if l > 0 else []
        T.dma("sp", dst[:, nb * 512:(nb + 1) * 512], xs_src(K, l, tt, nb), reads=rd, writes=[res])


def phase_mod(K, l):
    nc, T, d, R = K.nc, K.T, K.d, K.R
    with ExitStack() as es:
        sb = lambda n, s, dt: es.enter_context(K.sbt(n, s, dt))
        cT = sb("cT", [128, 2, 16], F32)
        cS = sb("cS", [128, 2, 16], F32)
        condT = sb("condT", [128, 16, 2], BF16)
        wch = [sb("wch%d" % i, [128, 16, 512], BF16) for i in range(2)]
        modrow = sb("modrow", [2, 6 * D], F32)
        brow = sb("brow", [2, 6 * D], F32)
        ps = [es.enter_context(K.pst("mps%d" % i, [128, 512], F32)) for i in range(2)]
        r_c, r_cond, r_b, r_mod = Res(), Res(), Res(), Res()
        r_w = [Res(), Res()]
        r_ps = [Res(), Res()]
        with nc.allow_non_contiguous_dma(reason="tiny cond vector transpose load"):
            T.dma("sp", cT[:], d["cvec"].rearrange("w (kc p) -> p w kc", p=128), writes=[r_c])
        T.dma("sp", brow[:], d["b_ada"][l:l + 1, :].partition_broadcast(2), writes=[r_b])
        T.op("act", lambda: nc.scalar.activation(out=cS[:], in_=cT[:], func=AF.Silu), reads=[r_c], writes=[r_cond])
        T.op("dve", lambda: nc.vector.tensor_copy(out=condT[:].rearrange("p k w -> p w k"), in_=cS[:]),
             reads=[r_cond], writes=[r_cond])
        wv = d["w_ada"][l].rearrange("(kc p) n -> p kc n", p=128)
        NB = 24

        def loadw(nb):
            T.dma("pool", wch[nb % 2][:], wv[:, :, nb * 512:(nb + 1) * 512], writes=[r_w[nb % 2]])
        loadw(0)
        for nb in range(NB):
            if nb + 1 < NB:
                loadw(nb + 1)
            b = nb % 2
            for kc in range(16):
                T.op("pe", lambda kc=kc: nc.tensor.matmul(ps[b][0:2, :], lhsT=condT[:, kc, :], rhs=wch[b][:, kc, :],
                                                         start=(kc == 0), stop=(kc == 15)),
                     reads=[r_cond, r_w[b]], writes=[r_ps[b]], inc=(kc == 15))
            T.op("dve", lambda: nc.vector.tensor_tensor(out=modrow[:, nb * 512:(nb + 1) * 512], in0=ps[b][0:2, :],
                                                        in1=brow[:, nb * 512:(nb + 1) * 512], op=ALU.add),
                 reads=[r_ps[b], r_b], writes=[r_mod])
        for c0 in (D, 4 * D):
            T.op("dve", lambda c0=c0: nc.vector.tensor_scalar(out=modrow[:, c0:c0 + D], in0=modrow[:, c0:c0 + D],
                                                              scalar1=1.0, scalar2=None, op0=ALU.add),
                 reads=[r_mod], writes=[r_mod])
        T.dma("sp", d["modv"][l], modrow[:], reads=[r_mod], writes=[R("modv")])


def load_bc(K, dst_ap, src_row_ap, res, reads=()):
    K.T.dma("sp", dst_ap, src_row_ap.partition_broadcast(128), reads=list(reads), writes=[res])


def phase_norm(K, l, which):
    nc, T, d, R = K.nc, K.T, K.d, K.R
    first_tt = 0
    if which == 2:
        first_tt = 0 if l == 0 else 2
    with ExitStack() as es:
        sb = lambda n, s, dt: es.enter_context(K.sbt(n, s, dt))
        A = [sb("nA%d" % i, [128, D], F32) for i in range(2)]
        SH = [sb("nS%d" % i, [128, D], F32) for i in range(2)]
        gb = sb("ngb", [128, D], F32)
        xt = [sb("nxt%d" % i, [128, D], F32) for i in range(4)]
        junk_ = [sb("njunk%d" % i, [128, D], BF16) for i in range(2)]
        hf_ = [sb("nhf%d" % i, [128, D], F32) for i in range(2)]
        hb = [sb("nhb%d" % i, [128, D], BF16) for i in range(2)]
        hT = [sb("nhT%d" % i, [128, 16, 128], BF16) for i in range(2)]
        st_ = [sb("nst%d" % i, [128, 4], F32) for i in range(2)]
        wr = sb("nwr", [128, 16, NE], BF16)
        ex_ = [sb("nex%d" % i, [128, NE], F32) for i in range(2)]
        tp = [es.enter_context(K.pst("ntp%d" % i, [128, 1024], BF16)) for i in range(4)]
        lp_ = [es.enter_context(K.pst("nlp%d" % i, [128, 512], F32)) for i in range(2)]
        r_A, r_x, r_hb, r_hT = Res(), [Res(), Res(), Res(), Res()], [Res(), Res()], [Res(), Res()]
        r_tp, r_lp_, r_wr = [Res(), Res(), Res(), Res()], [Res(), Res()], Res()
        r_hf_, r_st_, r_junk_, r_ex_ = [Res(), Res()], [Res(), Res()], [Res(), Res()], [Res(), Res()]
        sh_c, sc_c = (0, D) if which == 1 else (3 * D, 4 * D)
        grow = l if which == 1 else 2 + l
        load_bc(K, gb[:], d["gvec"][grow:grow + 1, :], r_A)
        for w in range(2):
            load_bc(K, A[w][:], d["modv"][l, w:w + 1, sc_c:sc_c + D], r_A, reads=[R("modv")])
            load_bc(K, SH[w][:], d["modv"][l, w:w + 1, sh_c:sh_c + D], r_A, reads=[R("modv")])
        for w in range(2):
            T.op("dve", lambda w=w: nc.vector.tensor_tensor(out=A[w][:], in0=A[w][:], in1=gb[:], op=ALU.mult),
                 reads=[r_A], writes=[r_A])
        if which == 2:
            T.dma("pool", wr[:], d["w_router"][l].rearrange("(kc p) e -> p kc e", p=128), writes=[r_wr])
        tiles = list(range(first_tt, NTT))
        def load(tt, buf):
            if which == 1:
                load_x_tile(K, l, tt, xt[buf], r_x[buf])
            else:
                for nb in range(4):
                    T.dma("sp", xt[buf][:, nb * 512:(nb + 1) * 512],
                          d["XS%d_%d" % (l, nb)][tt * 128:(tt + 1) * 128, :],
                          reads=[R("XS%d_%d" % (l, nb))], writes=[r_x[buf]])
        def tile_ops(n, tt):
            b = n % 2
            w = 1 if tt < 2 else 0
            junk, hf, st, ex = junk_[b], hf_[b], st_[b], ex_[b]
            r_junk, r_hf, r_st, r_ex = r_junk_[b], r_hf_[b], r_st_[b], r_ex_[b]
            r_lpb = r_lp_[b]
            lpb = lp_[b]
            ops = []
            xb4 = n % 4
            ops.append(lambda: T.op("act", lambda: nc.scalar.activation(out=junk[:], in_=xt[xb4][:], func=AF.Square, accum_out=st[:, 0:1]),
                                    reads=[r_x[xb4]], writes=[r_junk, r_st]))
            ops.append(lambda: T.op("act", lambda: nc.scalar.activation(out=st[:, 1:2], in_=st[:, 0:1], func=AF.Ln, scale=1.0 / D, bias=EPS),
                                    reads=[r_st], writes=[r_st]))
            ops.append(lambda: T.op("act", lambda: nc.scalar.activation(out=st[:, 2:3], in_=st[:, 1:2], func=AF.Exp, scale=-0.5),
                                    reads=[r_st], writes=[r_st]))
            ops.append(lambda: T.op("dve", lambda: nc.vector.scalar_tensor_tensor(out=hf[:], in0=xt[xb4][:], scalar=st[:, 2:3], in1=A[w][:],
                                                                                  op0=ALU.mult, op1=ALU.mult),
                                    reads=[r_x[xb4], r_st, r_A], writes=[r_hf]))
            ops.append(lambda: T.op("dve", lambda: nc.vector.tensor_tensor(out=hb[b][:], in0=hf[:], in1=SH[w][:], op=ALU.add),
                                    reads=[r_hf, r_A], writes=[r_hb[b]]))
            if which == 2:
                ops.append(lambda: T.dma("sp", d["H2"][tt * 128:(tt + 1) * 128, :], hb[b][:], reads=[r_hb[b]], writes=[R("H2")]))

            def trg(g):
                tpg = tp[b * 2 + g]
                r_tpg = r_tp[b * 2 + g]
                for k8 in range(8):
                    kc = g * 8 + k8
                    T.op("pe", lambda kc=kc, k8=k8: nc.tensor.transpose(out=tpg[:, k8 * 128:(k8 + 1) * 128],
                                                                      in_=hb[b][:, kc * 128:(kc + 1) * 128], identity=K.ident_bf[:]),
                         reads=[r_hb[b], R("ident")], writes=[r_tpg], inc=(k8 == 7))
                T.op("act", lambda: nc.scalar.copy(out=hT[b][:, g * 8:(g + 1) * 8, :].rearrange("p a b -> p (a b)"), in_=tpg[:]),
                     reads=[r_tpg], writes=[r_hT[b]])
            ops.append(lambda: trg(0))
            ops.append(lambda: trg(1))
            if which == 1:
                ops.append(lambda: T.dma("sp", d["hT"][tt], hT[b][:].rearrange("p a b -> p (a b)"), reads=[r_hT[b]], writes=[R("hT")]))
            else:
                def router():
                    for kc in range(16):
                        T.op("pe", lambda kc=kc: nc.tensor.matmul(lpb[:, 0:NE], lhsT=hT[b][:, kc, :], rhs=wr[:, kc, :],
                                                                 start=(kc == 0), stop=(kc == 15)),
                             reads=[r_hT[b], r_wr], writes=[r_lpb], inc=(kc == 15))
                ops.append(router)
                ops.append(lambda: T.op("act", lambda: nc.scalar.activation(out=ex[:], in_=lpb[:, 0:NE], func=AF.Exp, accum_out=st[:, 3:4]),
                                        reads=[r_lpb], writes=[r_ex, r_st]))
                ops.append(lambda: T.op("dve", lambda: nc.vector.reciprocal(out=st[:, 3:4], in_=st[:, 3:4]), reads=[r_st], writes=[r_st]))
                ops.append(lambda: T.op("dve", lambda: nc.vector.tensor_scalar(out=K.aff[:, tt, :], in0=ex[:], scalar1=st[:, 3:4],
                                                                               scalar2=None, op0=ALU.mult),
                                        reads=[r_ex, r_st], writes=[K.R("aff")]))
            return ops

        for q0 in range(0, min(2, len(tiles))):
            load(tiles[q0], q0 % 4)
        for p0 in range(0, len(tiles), 2):
            for q0 in range(p0 + 2, min(p0 + 4, len(tiles))):
                load(tiles[q0], q0 % 4)
            oa = tile_ops(p0, tiles[p0])
            ob = tile_ops(p0 + 1, tiles[p0 + 1]) if p0 + 1 < len(tiles) else []
            for i in range(max(len(oa), len(ob))):
                if i < len(oa):
                    oa[i]()
                if i < len(ob):
                    ob[i]()


def phase_proj(K, l):
    nc, T, d, R = K.nc, K.T, K.d, K.R
    types = EVEN_TYPES if l % 2 == 0 else ODD_TYPES
    ftmap, vvmap = block_maps(types)
    with ExitStack() as es:
        sb = lambda n, s, dt: es.enter_context(K.sbt(n, s, dt))
        wch = [sb("pw%d" % i, [128, 16, 512], BF16) for i in range(2)]
        hT = [sb("phT%d" % i, [128, 16, 128], BF16) for i in range(3)]
        rope = sb("prope", [128, NTT, 2 * HD], F32)
        FTst = sb("pFT", [128, 4, NT], BF16)
        Vst = [sb("pV%d" % i, [128, 512], BF16) for i in range(2)]
        xs = [sb("pxs%d" % i, [128, 512], F32) for i in range(2)]
        t1 = sb("pt1", [128, 512], F32)
        t2 = sb("pt2", [128, 512], F32)
        xb = [sb("pxb%d" % i, [128, 512], BF16) for i in range(2)]
        nrm = sb("pnrm", [128, 2, 128], F32)
        sq = sb("psq", [128, 512], F32)
        st = sb("pst", [128, 8], F32)
        ps = [es.enter_context(K.pst("pps%d" % i, [128, 512], F32)) for i in range(2)]
        tp = [es.enter_context(K.pst("ptp%d" % i, [128, 512], BF16)) for i in range(2)]
        r_w, r_hT = [Res(), Res()], [Res(), Res(), Res()]
        r_rope, r_FT, r_V, r_xs, r_t, r_xb = Res(), Res(), [Res(), Res()], [Res(), Res()], Res(), [Res(), Res()]
        r_ps, r_tp, r_nrm, r_st = [Res(), Res()], [Res(), Res()], Res(), Res()
        T.dma("sp", rope[:], d["ropetab"], writes=[r_rope])
        if l % 2 == 1:
            load_bc(K, nrm[:].rearrange("p a b -> p (a b)"), d["small2"][0:1, :], r_nrm)
        wv = d["w_in"][l].rearrange("(kc p) n -> p kc n", p=128)

        def loadw(cb):
            T.dma("pool", wch[cb % 2][:], wv[:, :, cb * 512:(cb + 1) * 512], writes=[r_w[cb % 2]])

        def loadh(n):
            tt = n % NTT
            T.dma("sp", hT[n % 3][:].rearrange("p a b -> p (a b)"), d["hT"][tt], reads=[R("hT")], writes=[r_hT[n % 3]])
        loadw(0)
        loadh(0)
        loadh(1)
        n = 0
        deferred = []

        def mk_B(tb, xbuf, b0, nbk, c0, c1, tt):
            def fn_():
                for k in range(nbk):
                    T.op("pe", lambda k=k: nc.tensor.transpose(out=tp[tb][:, (b0 + k) * 128:(b0 + k + 1) * 128],
                                                               in_=xb[xbuf][:, (b0 + k) * 128:(b0 + k + 1) * 128],
                                                               identity=K.ident_bf[:]),
                         reads=[r_xb[xbuf], R("ident")], writes=[r_tp[tb]], inc=(k == nbk - 1))
                T.op("act", lambda: nc.scalar.copy(out=FTst[:, b0:b0 + nbk, tt * 128:(tt + 1) * 128],
                                                   in_=tp[tb][:, c0:c1].rearrange("p (h e) -> p h e", e=128)),
                     reads=[r_tp[tb]], writes=[r_FT])
            return fn_

        def mk_flush(cb, btypes):
            def fn_():
                for bi, t in enumerate(btypes):
                    if t == "V":
                        continue
                    T.dma("sp", d["FT"][ftmap[cb * 4 + bi]], FTst[:, bi, :], reads=[r_FT], writes=[R("FT")])
            return fn_
        for cb in range(9):
            if cb + 1 < 9:
                loadw(cb + 1)
            btypes = types[cb * 4:(cb + 1) * 4]
            for tt in range(NTT):
                if n + 2 < 9 * NTT:
                    loadh(n + 2)
                hb_ = n % 3
                pb = n % 2
                for kc in range(16):
                    T.op("pe", lambda kc=kc: nc.tensor.matmul(ps[pb][:], lhsT=hT[hb_][:, kc, :], rhs=wch[cb % 2][:, kc, :],
                                                             start=(kc == 0), stop=(kc == 15)),
                         reads=[r_hT[hb_], r_w[cb % 2]], writes=[r_ps[pb]], inc=(kc == 15))
                for fn_ in deferred:
                    fn_()
                deferred = []
                groups = []
                for bi, t in enumerate(btypes):
                    if groups and groups[-1][0] == t:
                        groups[-1][2] += 1
                    else:
                        groups.append([t, bi, 1])
                for (t, b0, nbk) in groups:
                    c0, c1 = b0 * 128, (b0 + nbk) * 128
                    if t == "V":
                        vb = n % 2
                        T.op("act", lambda: nc.scalar.copy(out=Vst[vb][:, c0:c1], in_=ps[pb][:, c0:c1]),
                             reads=[r_ps[pb]], writes=[r_V[vb]])
                        v0 = vvmap[cb * 4 + b0]
                        T.dma("sp", d["VV"][tt * 128:(tt + 1) * 128, v0 * 128:(v0 + nbk) * 128], Vst[vb][:, c0:c1],
                              reads=[r_V[vb]], writes=[R("VV")])
                        continue
                    xbuf = n % 2
                    if t == "P":
                        T.op("act", lambda: nc.scalar.copy(out=xb[xbuf][:, c0:c1], in_=ps[pb][:, c0:c1]),
                             reads=[r_ps[pb]], writes=[r_xb[xbuf]])
                    else:
                        T.op("act", lambda: nc.scalar.copy(out=xs[xbuf][:, c0:c1], in_=ps[pb][:, c0:c1]),
                             reads=[r_ps[pb]], writes=[r_xs[xbuf]])
                        xv = xs[xbuf][:, c0:c1].rearrange("p (h e) -> p h e", e=128)
                        if t in ("NQ", "NK"):
                            wsel = 0 if t == "NQ" else 1
                            T.op("act", lambda: nc.scalar.activation(out=sq[:, c0:c1], in_=xs[xbuf][:, c0:c1], func=AF.Square),
                                 reads=[r_xs[xbuf]], writes=[r_t])
                            T.op("dve", lambda: nc.vector.tensor_reduce(out=st[:, 0:nbk], in_=sq[:, c0:c1].rearrange("p (h e) -> p h e", e=128),
                                                                        axis=mybir.AxisListType.X, op=ALU.add),
                                 reads=[r_t], writes=[r_st])
                            T.op("act", lambda: nc.scalar.activation(out=st[:, 0:nbk], in_=st[:, 0:nbk], func=AF.Sqrt,
                                                                     scale=1.0 / HD, bias=EPS),
                                 reads=[r_st], writes=[r_st])
                            T.op("dve", lambda: nc.vector.reciprocal(out=st[:, 4:4 + nbk], in_=st[:, 0:nbk]),
                                 reads=[r_st], writes=[r_st])
                            T.op("dve", lambda: nc.vector.tensor_tensor(out=xv, in0=xv,
                                                                        in1=st[:, 4:4 + nbk].unsqueeze(2).to_broadcast([128, nbk, 128]),
                                                                        op=ALU.mult),
                                 reads=[r_xs[xbuf], r_st], writes=[r_xs[xbuf]])
                            T.op("dve", lambda: nc.vector.tensor_tensor(out=xv, in0=xv,
                                                                        in1=nrm[:, wsel, :].unsqueeze(1).to_broadcast([128, nbk, 128]),
                                                                        op=ALU.mult),
                                 reads=[r_xs[xbuf], r_nrm], writes=[r_xs[xbuf]])
                        cosb = rope[:, tt, 0:HD].unsqueeze(1).to_broadcast([128, nbk, HD])
                        t1v = t1[:, c0:c1].rearrange("p (h e) -> p h e", e=128)
                        T.op("dve", lambda: nc.vector.tensor_tensor(out=t1v, in0=xv, in1=cosb, op=ALU.mult),
                             reads=[r_xs[xbuf], r_rope], writes=[r_t])
                        x5 = xs[xbuf][:, c0:c1].rearrange("p (h a j f) -> p (h a) j f", a=2, j=2, f=32)
                        t5 = t2[:, c0:c1].rearrange("p (h a j f) -> p (h a) j f", a=2, j=2, f=32)
                        s5 = rope[:, tt, HD:2 * HD].rearrange("p (a j f) -> p a j f", a=2, j=2, f=32)
                        for j in range(2):
                            for a in range(2):
                                T.op("dve", lambda j=j, a=a: nc.vector.tensor_tensor(
                                    out=t2[:, c0:c1].rearrange("p (h a j f) -> p h a j f", a=2, j=2, f=32)[:, :, a, j, :],
                                    in0=xs[xbuf][:, c0:c1].rearrange("p (h a j f) -> p h a j f", a=2, j=2, f=32)[:, :, a, 1 - j, :],
                                    in1=s5[:, a, j, :].unsqueeze(1).to_broadcast([128, nbk, 32]), op=ALU.mult),
                                    reads=[r_xs[xbuf], r_rope], writes=[r_t])
                        T.op("dve", lambda: nc.vector.tensor_tensor(out=xb[xbuf][:, c0:c1], in0=t1[:, c0:c1], in1=t2[:, c0:c1], op=ALU.add),
                             reads=[r_t], writes=[r_xb[xbuf]])
                    deferred.append(mk_B(n % 2, xbuf, b0, nbk, c0, c1, tt))
                n += 1
            deferred.append(mk_flush(cb, btypes))
        for fn_ in deferred:
            fn_()


def attn_group(K, A, qap, ncols, keys, dvp, finish):
    nc, T = K.nc, K.T
    nsub = ncols // 128
    n = len(keys)
    S_ps, P_sb = A["S"], A["P"]
    nset = max(1, len(A["acc"]) // nsub)
    off = (A.get("gcnt", 0) % nset) * nsub
    A["gcnt"] = A.get("gcnt", 0) + 1
    acc = A["acc"][off:off + nsub]
    r_S, r_P, r_acc = A["r_S"], A["r_P"], A["r_acc"][off:off + nsub]
    rq, rk, rv, rt = A["rq"], A["rk"], A["rv"], A["rt"]

    def qk(k):
        sbuf = A["cnt"] % 3
        A["scur"][k] = sbuf
        kT, _, _ = keys[k]
        T.op("pe", lambda: nc.tensor.matmul(S_ps[sbuf][:, 0:ncols], lhsT=kT, rhs=qap, start=True, stop=True),
             reads=[rq, rk], writes=[r_S[sbuf]])
        A["cnt"] += 1
    A["scur"] = {}
    qk(0)
    if n > 1:
        qk(1)
    for k in range(n):
        if k + 2 < n:
            qk(k + 2)
        sbuf = A["scur"][k]
        pb = A["pcnt"] % 3
        A["pcnt"] += 1
        _, v, tbl = keys[k]
        T.op("act", lambda: nc.scalar.activation(out=P_sb[pb][:, 0:ncols], in_=S_ps[sbuf][:, 0:ncols], func=AF.Exp, scale=SCALE),
             reads=[r_S[sbuf]], writes=[r_P[pb]])
        if tbl is not None:
            T.op("dve", lambda: nc.vector.tensor_tensor(out=P_sb[pb][:, 0:ncols], in0=P_sb[pb][:, 0:ncols], in1=tbl, op=ALU.mult),
                 reads=[rt], writes=[r_P[pb]])
        for s in range(nsub):
            T.op("pe", lambda s=s: nc.tensor.matmul(acc[s][:, 0:dvp], lhsT=P_sb[pb][:, s * 128:(s + 1) * 128], rhs=v,
                                                   start=(k == 0), stop=(k == n - 1)),
                 reads=[r_P[pb], rv], writes=[r_acc[s]], inc=(k == n - 1 or s == nsub - 1))
    for s in range(nsub):
        finish(s, acc[s], r_acc[s])


def attn_bufs(K, es, nacc=4):
    nc = K.nc
    A = {}
    A["S"] = [es.enter_context(K.pst("aS%d" % i, [128, 512], F32)) for i in range(3)]
    A["acc"] = [es.enter_context(K.pst("aacc%d" % i, [128, 512], F32)) for i in range(nacc)]
    A["P"] = [es.enter_context(K.sbt("aP%d" % i, [128, 512], BF16)) for i in range(3)]
    A["r_S"] = [Res(), Res(), Res()]
    A["r_P"] = [Res(), Res(), Res()]
    A["r_acc"] = [Res() for _ in range(nacc)]
    A["cnt"] = 0
    A["pcnt"] = 0
    A["rt"] = Res()
    return A


def phase_attn(K, l):
    nc, T, d, R = K.nc, K.T, K.d, K.R
    types = EVEN_TYPES if l % 2 == 0 else ODD_TYPES
    ftmap, vvmap = block_maps(types)
    FT, VV, OO = d["FT"], d["VV"], d["OO"]
    lat_tiles = list(range(2, NTT))

    def ftv(blk):
        return FT[ftmap[blk]].rearrange("p (t c) -> p t c", c=128)

    with ExitStack() as es:
        sb = lambda n, s, dt: es.enter_context(K.sbt(n, s, dt))
        A = attn_bufs(K, es)
        gsets = [dict(QA=sb("aQA%d" % i, [128, NTT, 4, 128], BF16), KT=sb("aKT%d" % i, [128, NT], BF16),
                      VA=sb("aVA%d" % i, [128, NTT, 129], BF16), rq=Res(), rk=Res(), rv=Res()) for i in range(2)]
        Ost = [sb("aO%d" % i, [128, 512], BF16) for i in range(2)]
        rc = [sb("arc%d" % i, [128, 4], F32) for i in range(2)]
        esk = sb("aesk", [128, 8], F32)
        bm = sb("abm", [128, 2, 4, 128], BF16)
        r_es, r_O, r_rc = Res(), [Res(), Res()], [Res(), Res()]
        A["rq"], A["rk"], A["rv"] = Res(), Res(), Res()
        if l % 2 == 0:
            load_bc(K, esk[:], d["small"][0:1, 0:8], r_es)
            T.op("act", lambda: nc.scalar.activation(out=esk[:], in_=esk[:], func=AF.Exp), reads=[r_es], writes=[r_es])
            T.dma("sp", bm[:], d["bandmask"], writes=[A["rt"]])
        on = 0

        def loadg(g):
            G_ = gsets[g % 2]
            for hh in range(4):
                T.dma("sp", G_["QA"][:, :, hh, :], ftv(g * 4 + hh), reads=[R("FT")], writes=[G_["rq"]])
            T.dma("sp", G_["KT"][:], FT[ftmap[8 + g]], reads=[R("FT")], writes=[G_["rk"]])
            T.op("dve", lambda: nc.vector.memset(G_["VA"][:, :, 128:129], 1.0), writes=[G_["rv"]])
            T.dma("sp", G_["VA"][:, :, 0:128], VV[:, vvmap[10 + g] * 128:(vvmap[10 + g] + 1) * 128].rearrange("(t p) c -> p t c", p=128),
                  reads=[R("VV")], writes=[G_["rv"]])
        loadg(0)
        for g in range(2):
            if g + 1 < 2:
                loadg(g + 1)
            G_ = gsets[g % 2]
            QA, KT, VA = G_["QA"], G_["KT"], G_["VA"]
            A["rq"], A["rk"], A["rv"] = G_["rq"], G_["rk"], G_["rv"]
            qtiles = list(range(NTT)) if l % 2 == 0 else lat_tiles
            for i in qtiles:
                if l % 2 == 0:
                    if i < 2:
                        kl = [(0, None), (1, None)]
                    else:
                        kl = [(0, None), (1, None)]
                        if i - 1 >= 2:
                            kl.append((i - 1, 0))
                        kl.append((i, None))
                        if i + 1 < NTT:
                            kl.append((i + 1, 1))
                else:
                    kl = [(j, None) for j in range(NTT)]
                keys = [(KT[:, j * 128:(j + 1) * 128], VA[:, j, :],
                         None if m is None else bm[:, m, :, :].rearrange("p h q -> p (h q)")) for (j, m) in kl]
                ob = on % 2
                on += 1

                def finish(s, acc, r_acc, ob=ob, g=g):
                    if l % 2 == 0:
                        T.op("dve", lambda: nc.vector.tensor_scalar(out=rc[ob][:, s:s + 1], in0=acc[:, 128:129],
                                                                    scalar1=esk[:, g * 4 + s:g * 4 + s + 1], scalar2=None, op0=ALU.add),
                             reads=[r_acc, r_es], writes=[r_rc[ob]])
                        T.op("dve", lambda: nc.vector.reciprocal(out=rc[ob][:, s:s + 1], in_=rc[ob][:, s:s + 1]),
                             reads=[r_rc[ob]], writes=[r_rc[ob]])
                    else:
                        T.op("dve", lambda: nc.vector.reciprocal(out=rc[ob][:, s:s + 1], in_=acc[:, 128:129]),
                             reads=[r_acc], writes=[r_rc[ob]])
                    T.op("dve", lambda: nc.vector.tensor_scalar(out=Ost[ob][:, s * 128:(s + 1) * 128], in0=acc[:, 0:128],
                                                                scalar1=rc[ob][:, s:s + 1], scalar2=None, op0=ALU.mult),
                         reads=[r_acc, r_rc[ob]], writes=[r_O[ob]])
                attn_group(K, A, QA[:, i, :, :].rearrange("p h c -> p (h c)"), 512, keys, 129, finish)
                T.dma("sp", OO[i * 128:(i + 1) * 128, g * 512:(g + 1) * 512], Ost[ob][:], reads=[r_O[ob]], writes=[R("OO")])
    T.barrier()
    if l % 2 == 0:
        attn_B(K, l, ftmap, vvmap)
    else:
        attn_D(K, l, ftmap, vvmap)


def attn_B(K, l, ftmap, vvmap):
    nc, T, d, R = K.nc, K.T, K.d, K.R
    FT, VV, OO = d["FT"], d["VV"], d["OO"]
    lam_init = lambda_init_for(l)
    with ExitStack() as es:
        sb = lambda n, s, dt: es.enter_context(K.sbt(n, s, dt))
        A = attn_bufs(K, es)
        bsets = [dict(QT=[sb("bQ%d_%d" % (i, m), [128, NT], BF16) for m in range(2)],
                      KT=[sb("bK%d_%d" % (i, m), [128, NT], BF16) for m in range(2)],
                      VB=sb("bV%d" % i, [128, NTT, 257], BF16), r_q=[Res(), Res()], r_k=[Res(), Res()], r_v=Res()) for i in range(2)]
        o1 = sb("bo1", [128, 4, 256], F32)
        o2 = sb("bo2", [128, 4, 256], F32)
        Ost = [sb("bO%d" % i, [128, 4, 256], BF16) for i in range(2)]
        rc = sb("brc", [128, 8], F32)
        lamv = sb("blam", [128, 4, 128], F32)
        lamt = sb("blamt", [128, 2, 128], F32)
        lams = sb("blams", [128, 4], F32)
        subw = sb("bsub", [128, 256], F32)
        sqj = sb("bsq", [128, 256], F32)
        r_o, r_O, r_rc, r_lam, r_sub = Res(), [Res(), Res()], Res(), Res(), Res()
        load_bc(K, lamv[:].rearrange("p a b -> p (a b)"), d["small"][0:1, 8:520], r_lam)
        load_bc(K, subw[:], d["small"][0:1, 520:776], r_sub)
        T.op("dve", lambda: nc.vector.tensor_tensor(out=lamt[:], in0=lamv[:, 0:4:2, :], in1=lamv[:, 1:4:2, :], op=ALU.mult),
             reads=[r_lam], writes=[r_lam])
        T.op("dve", lambda: nc.vector.tensor_reduce(out=lams[:, 0:2], in_=lamt[:], axis=mybir.AxisListType.X, op=ALU.add),
             reads=[r_lam], writes=[r_lam])
        T.op("act", lambda: nc.scalar.activation(out=lams[:, 0:2], in_=lams[:, 0:2], func=AF.Exp), reads=[r_lam], writes=[r_lam])
        T.op("dve", lambda: nc.vector.tensor_tensor(out=lams[:, 2:3], in0=lams[:, 1:2], in1=lams[:, 0:1], op=ALU.subtract),
             reads=[r_lam], writes=[r_lam])
        T.op("dve", lambda: nc.vector.tensor_scalar(out=lams[:, 2:3], in0=lams[:, 2:3], scalar1=-lam_init, scalar2=None, op0=ALU.add),
             reads=[r_lam], writes=[r_lam])
        T.op("dve", lambda: nc.vector.tensor_scalar(out=subw[:], in0=subw[:], scalar1=1.0 - lam_init, scalar2=None, op0=ALU.mult),
             reads=[r_sub], writes=[r_sub])
        on = 0

        def loadb(hb):
            B_ = bsets[hb % 2]
            for m in range(2):
                T.dma("sp", B_["QT"][m][:], FT[ftmap[12 + hb * 2 + m]], reads=[R("FT")], writes=[B_["r_q"][m]])
                T.dma("sp", B_["KT"][m][:], FT[ftmap[20 + hb * 2 + m]], reads=[R("FT")], writes=[B_["r_k"][m]])
            T.op("dve", lambda: nc.vector.memset(B_["VB"][:, :, 256:257], 1.0), writes=[B_["r_v"]])
            v0 = vvmap[28 + hb * 2]
            T.dma("sp", B_["VB"][:, :, 0:256], VV[:, v0 * 128:(v0 + 2) * 128].rearrange("(t p) c -> p t c", p=128),
                  reads=[R("VV")], writes=[B_["r_v"]])
        loadb(0)
        for hb in range(4):
            if hb + 1 < 4:
                loadb(hb + 1)
            B_ = bsets[hb % 2]
            QT, KT, VB = B_["QT"], B_["KT"], B_["VB"]
            r_q, r_k, r_v = B_["r_q"], B_["r_k"], B_["r_v"]
            chunks = [(0, 2, [0, 1])] + [(2 + 4 * c, 4, list(range(NTT))) for c in range(8)]
            for (t0, ntile, kts) in chunks:
                ob = on % 2
                on += 1
                for m in range(2):
                    A["rq"], A["rk"], A["rv"] = r_q[m], r_k[m], r_v
                    keys = [(KT[m][:, j * 128:(j + 1) * 128], VB[:, j, :], None) for j in kts]
                    dst = o1 if m == 0 else o2

                    def finish(s, acc, r_acc, m=m, dst=dst):
                        T.op("dve", lambda: nc.vector.reciprocal(out=rc[:, m * 4 + s:m * 4 + s + 1], in_=acc[:, 256:257]),
                             reads=[r_acc], writes=[r_rc])
                        T.op("dve", lambda: nc.vector.tensor_scalar(out=dst[:, s, :], in0=acc[:, 0:256],
                                                                    scalar1=rc[:, m * 4 + s:m * 4 + s + 1], scalar2=None, op0=ALU.mult),
                             reads=[r_acc, r_rc], writes=[r_o])
                    attn_group(K, A, QT[m][:, t0 * 128:(t0 + ntile) * 128], ntile * 128, keys, 257, finish)
                for s in range(ntile):
                    T.op("dve", lambda s=s: nc.vector.scalar_tensor_tensor(out=o1[:, s, :], in0=o2[:, s, :], scalar=lams[:, 2:3],
                                                                           in1=o1[:, s, :], op0=ALU.mult, op1=ALU.add),
                         reads=[r_o, r_lam], writes=[r_o])
                    T.op("act", lambda s=s: nc.scalar.activation(out=sqj[:], in_=o1[:, s, :], func=AF.Square, accum_out=rc[:, s:s + 1]),
                         reads=[r_o], writes=[r_rc])
                    T.op("act", lambda s=s: nc.scalar.activation(out=rc[:, s:s + 1], in_=rc[:, s:s + 1], func=AF.Ln,
                                                                 scale=1.0 / 256, bias=EPS),
                         reads=[r_rc], writes=[r_rc])
                    T.op("act", lambda s=s: nc.scalar.activation(out=rc[:, s:s + 1], in_=rc[:, s:s + 1], func=AF.Exp, scale=-0.5),
                         reads=[r_rc], writes=[r_rc])
                    T.op("dve", lambda s=s: nc.vector.scalar_tensor_tensor(out=Ost[ob][:, s, :], in0=o1[:, s, :], scalar=rc[:, s:s + 1],
                                                                           in1=subw[:], op0=ALU.mult, op1=ALU.mult),
                         reads=[r_o, r_rc, r_sub], writes=[r_O[ob]])
                T.dma("sp", OO[t0 * 128:(t0 + ntile) * 128, 1024 + hb * 256:1024 + (hb + 1) * 256].rearrange("(t p) c -> p t c", p=128),
                      Ost[ob][:, 0:ntile, :], reads=[r_O[ob]], writes=[R("OO")])


def attn_D(K, l, ftmap, vvmap):
    nc, T, d, R = K.nc, K.T, K.d, K.R
    FT, VV, OO = d["FT"], d["VV"], d["OO"]
    sets = d_tile_sets()
    with ExitStack() as es:
        sb = lambda n, s, dt: es.enter_context(K.sbt(n, s, dt))
        A = attn_bufs(K, es, nacc=2)
        dsets = [dict(QT=sb("dQ%d" % i, [128, NT], BF16), KT=sb("dK%d" % i, [128, NT], BF16), VD=sb("dV%d" % i, [128, NTT, 129], BF16),
                      tab=sb("dtab%d" % i, [128, 21, 128], BF16), rq=Res(), rk=Res(), rv=Res(), rt=Res()) for i in range(2)]
        raw = sb("draw", [128, 21, 128], F32)
        Ost = [sb("dO%d" % i, [128, 128], BF16) for i in range(2)]
        rc = sb("drc", [128, 2], F32)
        r_raw, r_O, r_rc = Res(), [Res(), Res()], Res()
        on = 0

        def loadd(h):
            D_ = dsets[h % 2]
            T.dma("sp", raw[:], d["rpbtab"][:, h, :, :], writes=[r_raw])
            T.op("act", lambda: nc.scalar.activation(out=D_["tab"][:], in_=raw[:], func=AF.Exp), reads=[r_raw], writes=[D_["rt"]])
            T.dma("sp", D_["QT"][:], FT[ftmap[12 + h]], reads=[R("FT")], writes=[D_["rq"]])
            T.dma("sp", D_["KT"][:], FT[ftmap[20 + h]], reads=[R("FT")], writes=[D_["rk"]])
            T.op("dve", lambda: nc.vector.memset(D_["VD"][:, :, 128:129], 1.0), writes=[D_["rv"]])
            v0 = vvmap[28 + h]
            T.dma("sp", D_["VD"][:, :, 0:128], VV[:, v0 * 128:(v0 + 1) * 128].rearrange("(t p) c -> p t c", p=128),
                  reads=[R("VV")], writes=[D_["rv"]])
        loadd(0)
        for h in range(8):
            if h + 1 < 8:
                loadd(h + 1)
            D_ = dsets[h % 2]
            QT, KT, VD, tab = D_["QT"], D_["KT"], D_["VD"], D_["tab"]
            A["rq"], A["rk"], A["rv"], A["rt"] = D_["rq"], D_["rk"], D_["rv"], D_["rt"]
            for i in range(32):
                js, ids = sets[i]
                keys = [(KT[:, j * 128:(j + 1) * 128], VD[:, j, :], None) for j in (0, 1)]
                keys += [(KT[:, (2 + j) * 128:(3 + j) * 128], VD[:, 2 + j, :], tab[:, tid, :]) for j, tid in zip(js, ids)]
                ob = on % 2
                on += 1

                def finish(s, acc, r_acc, ob=ob):
                    T.op("dve", lambda: nc.vector.reciprocal(out=rc[:, 0:1], in_=acc[:, 128:129]), reads=[r_acc], writes=[r_rc])
                    T.op("dve", lambda: nc.vector.tensor_scalar(out=Ost[ob][:], in0=acc[:, 0:128], scalar1=rc[:, 0:1],
                                                                scalar2=None, op0=ALU.mult),
                         reads=[r_acc, r_rc], writes=[r_O[ob]])
                attn_group(K, A, QT[:, (2 + i) * 128:(3 + i) * 128], 128, keys, 129, finish)
                T.dma("sp", OO[(2 + i) * 128:(3 + i) * 128, 1024 + h * 128:1024 + (h + 1) * 128], Ost[ob][:],
                      reads=[r_O[ob]], writes=[R("OO")])


def phase_outproj(K, l):
    nc, T, d, R = K.nc, K.T, K.d, K.R
    first_tt = 0 if l == 0 else 2
    with ExitStack() as es:
        sb = lambda n, s, dt: es.enter_context(K.sbt(n, s, dt))
        W = sb("oW", [128, 16, D], BF16)
        gt = [sb("ogt%d" % i, [128, D], F32) for i in range(2)]
        Ot = [sb("oO%d" % i, [128, D], BF16) for i in range(2)]
        OT = [sb("oOT%d" % i, [128, 16, 128], BF16) for i in range(2)]
        xt = [sb("oxt%d" % i, [128, D], F32) for i in range(2)]
        xn = [sb("oxn%d" % i, [128, D], F32) for i in range(2)]
        tp = [es.enter_context(K.pst("otp%d" % i, [128, 1024], BF16)) for i in range(2)]
        ps = [es.enter_context(K.pst("ops%d" % i, [128, 512], F32)) for i in range(2)]
        r_W, r_gt, r_O, r_OT, r_x, r_xn = Res(), Res(), [Res(), Res()], [Res(), Res()], [Res(), Res()], [Res(), Res()]
        r_tp, r_ps = [Res(), Res()], [Res(), Res()]
        wv = d["w_out"][l].rearrange("(kc p) n -> p kc n", p=128)
        for nb in range(4):
            T.dma("pool", W[:, :, nb * 512:(nb + 1) * 512], wv[:, :, nb * 512:(nb + 1) * 512], writes=[r_W])
        for w in range(2):
            load_bc(K, gt[w][:], d["modv"][l, w:w + 1, 2 * D:3 * D], r_gt, reads=[R("modv")])
        tiles = list(range(first_tt, NTT))

        def load(tt, b):
            T.dma("sp", Ot[b][:], d["OO"][tt * 128:(tt + 1) * 128, :], reads=[R("OO")], writes=[r_O[b]])
            load_x_tile(K, l, tt, xt[b], r_x[b])
        load(tiles[0], 0)
        pn = [0]

        def do_tr(n):
            b = n % 2
            for g in range(2):
                for k8 in range(8):
                    kc = g * 8 + k8
                    T.op("pe", lambda kc=kc, k8=k8, g=g: nc.tensor.transpose(out=tp[g][:, k8 * 128:(k8 + 1) * 128],
                                                                           in_=Ot[b][:, kc * 128:(kc + 1) * 128],
                                                                           identity=K.ident_bf[:]),
                         reads=[r_O[b], R("ident")], writes=[r_tp[g]], inc=(k8 == 7))
                T.op("act", lambda g=g: nc.scalar.copy(out=OT[b][:, g * 8:(g + 1) * 8, :].rearrange("p a b -> p (a b)"), in_=tp[g][:]),
                     reads=[r_tp[g]], writes=[r_OT[b]])

        def do_mm(n, tt):
            b = n % 2
            w = 1 if tt < 2 else 0
            for nb in range(4):
                pb = pn[0] % 2
                pn[0] += 1
                for kc in range(16):
                    T.op("pe", lambda kc=kc: nc.tensor.matmul(ps[pb][:], lhsT=OT[b][:, kc, :], rhs=W[:, kc, nb * 512:(nb + 1) * 512],
                                                             start=(kc == 0), stop=(kc == 15)),
                         reads=[r_OT[b], r_W], writes=[r_ps[pb]], inc=(kc == 15))
                T.op("dve", lambda: nc.vector.tensor_tensor(out=xn[b][:, nb * 512:(nb + 1) * 512], in0=ps[pb][:],
                                                            in1=gt[w][:, nb * 512:(nb + 1) * 512], op=ALU.mult),
                     reads=[r_ps[pb], r_gt], writes=[r_xn[b]])
                T.op("dve", lambda: nc.vector.tensor_tensor(out=xn[b][:, nb * 512:(nb + 1) * 512], in0=xn[b][:, nb * 512:(nb + 1) * 512],
                                                            in1=xt[b][:, nb * 512:(nb + 1) * 512], op=ALU.add),
                     reads=[r_xn[b], r_x[b]], writes=[r_xn[b]])
                T.dma("sp", d["XS%d_%d" % (l, nb)][tt * 128:(tt + 1) * 128, :], xn[b][:, nb * 512:(nb + 1) * 512],
                      reads=[r_xn[b]], writes=[R("XS%d_%d" % (l, nb))])
        do_tr(0)
        for n, tt in enumerate(tiles):
            if n + 1 < len(tiles):
                load(tiles[n + 1], (n + 1) % 2)
                do_tr(n + 1)
            do_mm(n, tt)


def phase_route(K, l):
    nc, T, d, R = K.nc, K.T, K.d, K.R
    sets = [(2, NTT, CAP)] + ([(0, 2, CAPX)] if l == 0 else [])
    with ExitStack() as es:
        sb = lambda n, s, dt: es.enter_context(K.sbt(n, s, dt))
        affT = sb("raffT", [NE, NT], F32)
        maskT = sb("rmaskT", [NE, NT], BF16)
        junk = sb("rjunk", [NE, S], BF16)
        bs = sb("rbs", [NE, 4], F32)
        mask = sb("rmask", [128, NTT, NE], BF16)
        slot = sb("rslot", [128, NTT, NE], F32)
        tmp = sb("rtmp", [128, NTT, NE], F32)
        Rt = sb("rR", [128, NTT, NE, 5], BF16)
        g1 = sb("rg1", [128, NTT, NE], BF16)
        r1 = sb("rr1", [128, NTT, NE], F32)
        tri = sb("rtri", [128, 256], BF16)
        iota = sb("riota", [128, 512], F32)
        tokab = sb("rtok", [128, NTT, 2], BF16)
        OH = [sb("rOH%d" % i, [128, 512], BF16) for i in range(3)]
        accs = sb("raccs", [128, 4, 5], F32)
        acc5 = sb("racc5", [8, 512], F32)
        r_a5 = Res()
        idf = sb("ridf", [128, 4], F32)
        tp = es.enter_context(K.pst("rtp", [128, 512], F32))
        tpb = es.enter_context(K.pst("rtpb", [128, 1024], BF16))
        pp = [es.enter_context(K.pst("rpp%d" % i, [128, 512], F32)) for i in range(2)]
        ap_ = [es.enter_context(K.pst("rap%d" % i, [128, 512], F32)) for i in range(2)]
        r_affT, r_maskT, r_bs, r_mask, r_slot, r_R, r_c, r_tp, r_tpb = Res(), Res(), Res(), Res(), Res(), Res(), Res(), Res(), Res()
        r_pp, r_ap, r_OH, r_acc, r_j = [Res(), Res()], [Res(), Res()], [Res(), Res(), Res()], Res(), Res()
        T.dma("sp", tri[:], d["tri"], writes=[r_c])
        T.dma("sp", iota[:], d["iota"], writes=[r_c])
        T.dma("sp", tokab[:], d["tokab"], writes=[r_c])
        r_aff = K.R("aff")
        tiles_all = list(range(0 if l == 0 else 2, NTT))
        for tt in tiles_all:
            T.op("pe", lambda tt=tt: nc.tensor.transpose(out=tp[0:NE, 0:128], in_=K.aff[:, tt, :], identity=K.ident_f[:]),
                 reads=[r_aff, R("ident")], writes=[r_tp])
            T.op("dve", lambda tt=tt: nc.vector.tensor_copy(out=affT[:, tt * 128:(tt + 1) * 128], in_=tp[0:NE, 0:128]),
                 reads=[r_tp], writes=[r_affT])
        for (ta, tb_, cap) in sets:
            c0, c1 = ta * 128, tb_ * 128
            T.op("dve", lambda: nc.vector.memset(bs[:, 0:1], 0.0), writes=[r_bs])
            for it in range(24):
                wk = 2.0 ** -(it + 1)
                T.op("dve", lambda wk=wk: nc.vector.tensor_scalar(out=bs[:, 1:2], in0=bs[:, 0:1], scalar1=wk, scalar2=None, op0=ALU.add),
                     reads=[r_bs], writes=[r_bs])
                T.op("dve", lambda: nc.vector.tensor_scalar(out=junk[:, 0:c1 - c0], in0=affT[:, c0:c1], scalar1=bs[:, 1:2], scalar2=0.0,
                                                            op0=ALU.is_ge, op1=ALU.add, accum_out=bs[:, 2:3]),
                     reads=[r_bs, r_affT], writes=[r_bs, r_j])
                T.op("dve", lambda: nc.vector.tensor_scalar(out=bs[:, 3:4], in0=bs[:, 2:3], scalar1=float(cap) - 0.5, scalar2=None,
                                                            op0=ALU.is_ge),
                     reads=[r_bs], writes=[r_bs])
                T.op("dve", lambda wk=wk: nc.vector.scalar_tensor_tensor(out=bs[:, 0:1], in0=bs[:, 3:4], scalar=wk, in1=bs[:, 0:1],
                                                                         op0=ALU.mult, op1=ALU.add),
                     reads=[r_bs], writes=[r_bs])
            T.op("dve", lambda: nc.vector.tensor_scalar(out=maskT[:, c0:c1], in0=affT[:, c0:c1], scalar1=bs[:, 0:1], scalar2=None,
                                                        op0=ALU.is_ge),
                 reads=[r_bs, r_affT], writes=[r_maskT])
        for tt in tiles_all:
            T.op("pe", lambda tt=tt: nc.tensor.transpose(out=tpb[:, 0:NE], in_=maskT[:, tt * 128:(tt + 1) * 128],
                                                         identity=K.ident_bf[0:NE, 0:NE]),
                 reads=[r_maskT, R("ident")], writes=[r_tpb])
            T.op("dve", lambda tt=tt: nc.vector.tensor_copy(out=mask[:, tt, :], in_=tpb[:, 0:NE]), reads=[r_tpb], writes=[r_mask])
        pn = 0
        for (ta, tb_, cap) in sets:
            for j in range(ta, tb_):
                pb = pn % 2
                pn += 1
                prev = list(range(ta, j))
                for ii, i in enumerate(prev):
                    T.op("pe", lambda i=i, ii=ii: nc.tensor.matmul(pp[pb][:, 0:NE], lhsT=tri[:, 0:128], rhs=mask[:, i, :],
                                                                  start=(ii == 0), stop=False),
                         reads=[r_mask, r_c], writes=[r_pp[pb]], inc=False)
                T.op("pe", lambda j=j: nc.tensor.matmul(pp[pb][:, 0:NE], lhsT=tri[:, 128:256], rhs=mask[:, j, :],
                                                       start=(len(prev) == 0), stop=True),
                     reads=[r_mask, r_c], writes=[r_pp[pb]])
                T.op("dve", lambda j=j: nc.vector.tensor_scalar(out=tmp[:, j, :], in0=mask[:, j, :], scalar1=-1.0, scalar2=10000.0,
                                                                op0=ALU.add, op1=ALU.mult),
                     reads=[r_mask], writes=[r_slot])
                T.op("dve", lambda j=j: nc.vector.tensor_tensor(out=slot[:, j, :], in0=tmp[:, j, :], in1=pp[pb][:, 0:NE], op=ALU.add),
                     reads=[r_pp[pb], r_slot], writes=[r_slot])
        T.op("dve", lambda: nc.vector.tensor_copy(out=Rt[:, :, :, 0:2], in_=tokab[:].unsqueeze(2).to_broadcast([128, NTT, NE, 2])),
             reads=[r_c], writes=[r_R])
        T.op("dve", lambda: nc.vector.tensor_copy(out=g1[:], in_=K.aff[:]), reads=[r_aff], writes=[r_R])
        T.op("dve", lambda: nc.vector.tensor_copy(out=Rt[:, :, :, 2], in_=g1[:]), reads=[r_R], writes=[r_R])
        T.op("dve", lambda: nc.vector.tensor_tensor(out=r1[:], in0=K.aff[:], in1=g1[:], op=ALU.subtract), reads=[r_aff, r_R], writes=[r_R])
        T.op("dve", lambda: nc.vector.tensor_copy(out=g1[:], in_=r1[:]), reads=[r_R], writes=[r_R])
        T.op("dve", lambda: nc.vector.tensor_copy(out=Rt[:, :, :, 3], in_=g1[:]), reads=[r_R], writes=[r_R])
        T.op("dve", lambda: nc.vector.tensor_tensor(out=r1[:], in0=r1[:], in1=g1[:], op=ALU.subtract), reads=[r_R], writes=[r_R])
        T.op("dve", lambda: nc.vector.tensor_copy(out=Rt[:, :, :, 4], in_=r1[:]), reads=[r_R], writes=[r_R])
        on = 0
        an = 0
        for (ta, tb_, cap) in sets:
            nsc = (cap + 127) // 128
            wid = min(cap, 512)
            for e in range(NE):
                ab = an % 2
                an += 1
                for j in range(ta, tb_):
                    ob = on % 3
                    on += 1
                    T.op("dve", lambda j=j, e=e: nc.vector.tensor_scalar(out=OH[ob][:, 0:wid], in0=iota[:, 0:wid],
                                                                          scalar1=slot[:, j, e:e + 1], scalar2=None, op0=ALU.is_equal),
                         reads=[r_slot, r_c], writes=[r_OH[ob]])
                    T.op("pe", lambda j=j, e=e: nc.tensor.matmul(ap_[ab][0:5, 0:wid], lhsT=Rt[:, j, e, :], rhs=OH[ob][:, 0:wid],
                                                                 start=(j == ta), stop=(j == tb_ - 1)),
                         reads=[r_OH[ob], r_R], writes=[r_ap[ab]], inc=True)
                T.op("dve", lambda: nc.vector.tensor_copy(out=acc5[0:5, 0:wid], in_=ap_[ab][0:5, 0:wid]), reads=[r_ap[ab]], writes=[r_a5])
                for sc in range(nsc):
                    m = min(128, cap - sc * 128)
                    T.op("pe", lambda sc=sc, m=m: nc.tensor.transpose(out=tp[0:m, sc * 8:sc * 8 + 5], in_=acc5[0:5, sc * 128:sc * 128 + m],
                                                                      identity=K.ident_f[0:5, 0:5]),
                         reads=[r_a5, R("ident")], writes=[r_tp])
                m = min(128, cap)
                av = tp[0:m, 0:32].rearrange("p (s c) -> p s c", c=8)
                T.op("dve", lambda: nc.vector.tensor_copy(out=accs[0:m, 0:nsc, :], in_=av[:, 0:nsc, 0:5]), reads=[r_tp], writes=[r_acc])
                T.op("dve", lambda: nc.vector.scalar_tensor_tensor(out=idf[0:m, 0:nsc], in0=accs[0:m, 0:nsc, 0], scalar=64.0,
                                                                   in1=accs[0:m, 0:nsc, 1], op0=ALU.mult, op1=ALU.add),
                     reads=[r_acc], writes=[r_acc])
                if cap == CAP:
                    idst, gdst = K.idx[:, e, :], K.gate[:, e, :]
                else:
                    idst, gdst = K.idxx[0:m, e:e + 1], K.gatex[0:m, e:e + 1]
                T.op("dve", lambda: nc.vector.tensor_copy(out=idst, in_=idf[0:m, 0:nsc]), reads=[r_acc], writes=[K.R("idx")])
                T.op("dve", lambda: nc.vector.tensor_tensor(out=gdst, in0=accs[0:m, 0:nsc, 2], in1=accs[0:m, 0:nsc, 3], op=ALU.add),
                     reads=[r_acc], writes=[K.R("idx")])
                T.op("dve", lambda: nc.vector.tensor_tensor(out=gdst, in0=gdst, in1=accs[0:m, 0:nsc, 4], op=ALU.add),
                     reads=[r_acc, K.R("idx")], writes=[K.R("idx")])


def phase_moe(K, l):
    nc, T, d, R = K.nc, K.T, K.d, K.R
    has_ctx = (l == 0)
    NS = CAP + (CAPX if has_ctx else 0)
    experts = K.dbg.get("experts", list(range(NE)))
    with ExitStack() as es:
        sb = lambda n, s, dt: es.enter_context(K.sbt(n, s, dt))
        NR = 6
        ring = [sb("mring%d" % i, [128, 16, 512], BF16) for i in range(NR)]
        xe = sb("mxe", [128, 4, D], BF16)
        xex = sb("mxex", [128, D], BF16)
        xeT = sb("mxeT", [128, 16, CAP + CAPX], BF16)
        hidT = sb("mhidT", [128, 12, CAP + CAPX], BF16)
        gt = [sb("mgt%d" % i, [128, D], F32) for i in range(2)]
        s1 = [sb("ms1%d" % i, [128, 512], F32) for i in range(2)]
        s1x = sb("ms1x", [128, 32], F32)
        yst = [sb("myst%d" % i, [128, 512], F32) for i in range(4)]
        GU = [es.enter_context(K.pst("mGU%d" % i, [128, 512], F32)) for i in range(4)]
        GX = es.enter_context(K.pst("mGX", [128, 512], F32))
        tp = [es.enter_context(K.pst("mtp%d" % i, [128, 1024], BF16)) for i in range(2)]
        r_ring = [Res() for _ in range(NR)]
        r_xe, r_xeT, r_hid, r_gt = Res(), Res(), Res(), Res()
        r_s1, r_s1x, r_y = [Res(), Res()], Res(), [Res() for _ in range(4)]
        r_GU, r_GX, r_tp = [Res() for _ in range(4)], Res(), [Res(), Res()]
        r_idx = K.R("idx")
        for w in range(2):
            load_bc(K, gt[w][:], d["modv"][l, w:w + 1, 5 * D:6 * D], r_gt, reads=[R("modv")])
        pieces = []
        for e in experts:
            for fb in range(3):
                pieces.append(("g", e, fb))
                pieces.append(("u", e, fb))
            for nb in range(4):
                pieces.append(("d", e, nb))
        pstate = dict(next=0)

        def issue_piece():
            i = pstate["next"]
            if i >= len(pieces):
                return
            kind, e, j = pieces[i]
            slot = i % NR
            if kind == "d":
                src = d["w_down"][l, e].rearrange("(fc p) n -> p fc n", p=128)[:, :, j * 512:(j + 1) * 512]
                T.dma("pool", ring[slot][:, 0:12, :], src, writes=[r_ring[slot]])
            else:
                wn = "w_gate" if kind == "g" else "w_up"
                src = d[wn][l, e].rearrange("(kc p) n -> p kc n", p=128)[:, :, j * 512:(j + 1) * 512]
                T.dma("pool", ring[slot][:], src, writes=[r_ring[slot]])
            pstate["next"] = i + 1

        def gather(e):
            for sc in range(4):
                T.dma("pool", xe[:, sc, :], d["H2"], reads=[R("H2"), r_idx], writes=[r_xe],
                      indirect=dict(out_offset=None, in_offset=bass.IndirectOffsetOnAxis(ap=K.idx[:, e, sc:sc + 1], axis=0)))
            if has_ctx:
                T.dma("pool", xex[0:CAPX, :], d["H2"], reads=[R("H2"), r_idx], writes=[r_xe],
                      indirect=dict(out_offset=None, in_offset=bass.IndirectOffsetOnAxis(ap=K.idxx[0:CAPX, e:e + 1], axis=0)))

        def transposes(e):
            tn = 0
            for sc in range(4):
                for g in range(2):
                    tb = tn % 2
                    tn += 1
                    for k8 in range(8):
                        kc = g * 8 + k8
                        T.op("pe", lambda kc=kc, k8=k8: nc.tensor.transpose(out=tp[tb][:, k8 * 128:(k8 + 1) * 128],
                                                                          in_=xe[:, sc, kc * 128:(kc + 1) * 128], identity=K.ident_bf[:]),
                             reads=[r_xe, R("ident")], writes=[r_tp[tb]], inc=(k8 == 7))
                    eng = "act" if g == 0 else "dve"
                    src = tp[tb][:].rearrange("p (a b) -> p a b", b=128)
                    dst = xeT[:, g * 8:(g + 1) * 8, sc * 128:(sc + 1) * 128]
                    if eng == "act":
                        T.op("act", lambda: nc.scalar.copy(out=dst, in_=src), reads=[r_tp[tb]], writes=[r_xeT])
                    else:
                        T.op("dve", lambda: nc.vector.tensor_copy(out=dst, in_=src), reads=[r_tp[tb]], writes=[r_xeT])
            if has_ctx:
                tb = tn % 2
                for kc in range(16):
                    T.op("pe", lambda kc=kc: nc.tensor.transpose(out=tp[tb][:, kc * 32:(kc + 1) * 32],
                                                                 in_=xex[0:CAPX, kc * 128:(kc + 1) * 128],
                                                                 identity=K.ident_bf[0:CAPX, 0:CAPX]),
                         reads=[r_xe, R("ident")], writes=[r_tp[tb]], inc=(kc == 15))
                T.op("dve", lambda: nc.vector.tensor_copy(out=xeT[:, :, CAP:CAP + CAPX],
                                                          in_=tp[tb][:, 0:512].rearrange("p (a b) -> p a b", b=32)),
                     reads=[r_tp[tb]], writes=[r_xeT])

        def ensure(upto):
            while pstate["next"] <= min(upto, len(pieces) - 1):
                issue_piece()
        pi = 0
        gn = 0
        yn = 0
        prev_sc = [[] for _ in range(4)]
        gather(experts[0])
        transposes(experts[0])
        for ei, e in enumerate(experts):
            if ei + 1 < len(experts):
                gather(experts[ei + 1])
            for fb in range(3):
                ensure(pi + NR - 1)
                sg, su = pi % NR, (pi + 1) % NR
                pi += 2
                for f4 in range(4):
                    fc = fb * 4 + f4
                    gb = gn % 2
                    gn += 1
                    G, U = GU[gb], GU[2 + gb]
                    for (ps_, slot_, rr) in ((G, sg, r_GU[gb]), (U, su, r_GU[2 + gb])):
                        for kc in range(16):
                            T.op("pe", lambda kc=kc, ps_=ps_, slot_=slot_: nc.tensor.matmul(
                                ps_[:, 0:CAP], lhsT=ring[slot_][:, kc, f4 * 128:(f4 + 1) * 128], rhs=xeT[:, kc, 0:CAP],
                                start=(kc == 0), stop=(kc == 15)),
                                reads=[r_ring[slot_], r_xeT], writes=[rr], inc=(kc == 15))
                    if has_ctx:
                        for ci, slot_ in enumerate((sg, su)):
                            for kc in range(16):
                                T.op("pe", lambda kc=kc, ci=ci, slot_=slot_: nc.tensor.matmul(
                                    GX[:, ci * 32:(ci + 1) * 32], lhsT=ring[slot_][:, kc, f4 * 128:(f4 + 1) * 128],
                                    rhs=xeT[:, kc, CAP:CAP + CAPX], start=(kc == 0), stop=(kc == 15)),
                                    reads=[r_ring[slot_], r_xeT], writes=[r_GX], inc=(kc == 15))
                    sbuf_ = gn % 2
                    T.op("act", lambda: nc.scalar.activation(out=s1[sbuf_][:], in_=G[:, 0:CAP], func=AF.Silu),
                         reads=[r_GU[gb]], writes=[r_s1[sbuf_]])
                    T.op("dve", lambda: nc.vector.tensor_tensor(out=hidT[:, fc, 0:CAP], in0=s1[sbuf_][:], in1=U[:, 0:CAP], op=ALU.mult),
                         reads=[r_s1[sbuf_], r_GU[2 + gb]], writes=[r_hid])
                    if has_ctx:
                        T.op("act", lambda: nc.scalar.activation(out=s1x[:], in_=GX[:, 0:32], func=AF.Silu),
                             reads=[r_GX], writes=[r_s1x])
                        T.op("dve", lambda: nc.vector.tensor_tensor(out=hidT[:, fc, CAP:CAP + CAPX], in0=s1x[:], in1=GX[:, 32:64], op=ALU.mult),
                             reads=[r_s1x, r_GX], writes=[r_hid])
            if ei + 1 < len(experts):
                transposes(experts[ei + 1])
            new_sc = [[] for _ in range(4)]
            for nb in range(4):
                ensure(pi + NR - 1)
                sd = pi % NR
                pi += 1
                for st_ in range(5 if has_ctx else 4):
                    m = 128 if st_ < 4 else CAPX
                    yb = yn % 4
                    yn += 1
                    Y = GU[yb]
                    for fc in range(12):
                        T.op("pe", lambda fc=fc: nc.tensor.matmul(Y[0:m, :], lhsT=hidT[:, fc, st_ * 128:st_ * 128 + m],
                                                                 rhs=ring[sd][:, fc, :], start=(fc == 0), stop=(fc == 11)),
                             reads=[r_hid, r_ring[sd]], writes=[r_GU[yb]], inc=(fc == 11))
                    if st_ < 4:
                        gsc, gtt, iap = K.gate[:, e, st_:st_ + 1], gt[0], K.idx[:, e, st_:st_ + 1]
                    else:
                        gsc, gtt, iap = K.gatex[0:m, e:e + 1], gt[1], K.idxx[0:m, e:e + 1]
                    T.op("dve", lambda: nc.vector.scalar_tensor_tensor(out=yst[yb][0:m, :], in0=Y[0:m, :], scalar=gsc,
                                                                       in1=gtt[0:m, nb * 512:(nb + 1) * 512], op0=ALU.mult, op1=ALU.mult),
                         reads=[r_GU[yb], r_idx, r_gt], writes=[r_y[yb]])
                    xsn = "XS%d_%d" % (l, nb)
                    tok = T.dma("pool", d[xsn], yst[yb][0:m, :], reads=[r_y[yb], r_idx, R(xsn)], writes=[],
                                extra_waits=prev_sc[nb],
                                indirect=dict(out_offset=bass.IndirectOffsetOnAxis(ap=iap, axis=0), in_offset=None,
                                              compute_op=ALU.add))
                    new_sc[nb].append(tok)
            prev_sc = new_sc
        for nb in range(4):
            rr = R("XS%d_%d" % (l, nb))
            for tok in prev_sc[nb]:
                rr.r[tok[0]] = tok


def phase_final(K):
    nc, T, d, R = K.nc, K.T, K.d, K.R
    l = 1
    with ExitStack() as es:
        sb = lambda n, s, dt: es.enter_context(K.sbt(n, s, dt))
        gb = sb("fgb", [128, D], F32)
        xt = [sb("fxt%d" % i, [128, D], F32) for i in range(2)]
        yo = [sb("fyo%d" % i, [128, D], F32) for i in range(2)]
        junk = sb("fjunk", [128, D], BF16)
        st = sb("fst", [128, 4], F32)
        r_g, r_x, r_y, r_st, r_j = Res(), [Res(), Res()], [Res(), Res()], Res(), Res()
        load_bc(K, gb[:], d["gvec"][4:5, :], r_g)
        tiles = list(range(2, NTT))

        def load(tt, b):
            for nb in range(4):
                T.dma("sp", xt[b][:, nb * 512:(nb + 1) * 512], d["XS%d_%d" % (l, nb)][tt * 128:(tt + 1) * 128, :],
                      reads=[R("XS%d_%d" % (l, nb))], writes=[r_x[b]])
        load(tiles[0], 0)
        for n, tt in enumerate(tiles):
            b = n % 2
            if n + 1 < len(tiles):
                load(tiles[n + 1], (n + 1) % 2)
            T.op("act", lambda: nc.scalar.activation(out=junk[:], in_=xt[b][:], func=AF.Square, accum_out=st[:, 0:1]),
                 reads=[r_x[b]], writes=[r_j, r_st])
            T.op("act", lambda: nc.scalar.activation(out=st[:, 1:2], in_=st[:, 0:1], func=AF.Sqrt, scale=1.0 / D, bias=EPS),
                 reads=[r_st], writes=[r_st])
            T.op("dve", lambda: nc.vector.reciprocal(out=st[:, 2:3], in_=st[:, 1:2]), reads=[r_st], writes=[r_st])
            T.op("dve", lambda: nc.vector.scalar_tensor_tensor(out=yo[b][:], in0=xt[b][:], scalar=st[:, 2:3], in1=gb[:],
                                                               op0=ALU.mult, op1=ALU.mult),
                 reads=[r_x[b], r_st, r_g], writes=[r_y[b]])
            T.dma("sp", d["out"][(tt - 2) * 128:(tt - 1) * 128, :], yo[b][:], reads=[r_y[b]], writes=[R("out")])


def make_in_maps(inputs, cores):
    c = const_pack()
    f = lambda a: np.ascontiguousarray(np.asarray(a, dtype=np.float32))
    small = np.zeros((1, 1024), np.float32)
    small[0, 0:8] = f(inputs["a_sink"])[0]
    small[0, 8:136] = f(inputs["b_lam_q1"])[0]
    small[0, 136:264] = f(inputs["b_lam_k1"])[0]
    small[0, 264:392] = f(inputs["b_lam_q2"])[0]
    small[0, 392:520] = f(inputs["b_lam_k2"])[0]
    small[0, 520:776] = f(inputs["b_subln"])[0]
    small2 = np.concatenate([f(inputs["c_q_norm"])[0], f(inputs["c_k_norm"])[0]])[None, :]
    gvec = np.stack([f(inputs["g_mix"])[0], f(inputs["g_mix"])[1], f(inputs["g_ffn"])[0], f(inputs["g_ffn"])[1],
                     f(inputs["g_final"])], axis=0)
    rpbtab = d_tables(f(inputs["d_rpb"])[0])
    shared = dict(
        w_ada=f(inputs["w_ada"]), b_ada=f(inputs["b_ada"]), gvec=np.ascontiguousarray(gvec), w_in=f(inputs["w_in"]),
        w_out=f(inputs["w_out"]), w_router=f(inputs["w_router"]), w_gate=f(inputs["w_gate"]), w_up=f(inputs["w_up"]),
        w_down=f(inputs["w_down"]), small=small, small2=np.ascontiguousarray(small2), rpbtab=rpbtab, **c)
    maps = []
    x = inputs["x"]
    ctx = inputs["ctx"]
    cc = f(inputs["c"])
    c_ctx = f(inputs["c_ctx"])
    for b in cores:
        m = dict(shared)
        m["x"] = f(x[b])
        m["ctx"] = f(ctx[b])
        m["cvec"] = np.ascontiguousarray(np.stack([cc[b], c_ctx], axis=0))
        maps.append(m)
    return maps


def kernel(**inputs):
    nc = build_program()
    maps = make_in_maps(inputs, list(range(8)))
    res = run_bass_kernel_spmd(nc, maps, core_ids=list(range(8)))
    out = np.stack([np.asarray(r["out"], dtype=np.float32) for r in res.results], axis=0)
    return out
```

```python
import numpy as np
import ml_dtypes
from contextlib import ExitStack
import concourse.bass as bass
import concourse.mybir as mybir
from concourse.bass_utils import run_bass_kernel_spmd

F32 = mybir.dt.float32
BF16 = mybir.dt.bfloat16
I32 = mybir.dt.int32
AF = mybir.ActivationFunctionType
ALU = mybir.AluOpType

D = 2048
S = 4096
CTX = 256
NT = S + CTX
NTT = NT // 128
INW = 4608
NE = 16
DE = 1536
CAP = 512
CAPX = 32
HD = 128
EPS = 1e-6
SCALE = HD ** -0.5
GRID_W = 64
NBF16 = ml_dtypes.bfloat16


def lambda_init_for(layer):
    import math
    return 0.8 - 0.6 * math.exp(-0.3 * layer)


class Res:
    __slots__ = ("w", "r")

    def __init__(self):
        self.w = None
        self.r = {}


class Trk:
    def __init__(self, nc, es):
        self.nc = nc
        self.eng = {}
        for name, e in (("pe", nc.tensor), ("act", nc.scalar), ("dve", nc.vector),
                        ("pool", nc.gpsimd), ("sp", nc.sync)):
            sem = es.enter_context(nc.semaphore("s_" + name)) if name != "sp" else None
            self.eng[name] = dict(e=e, sem=sem, n=0, waited={}, name=name)
        self.dp = {}
        for q, n in (("sp", 28), ("pool", 28), ("act", 6)):
            self.dp[q] = dict(sems=[es.enter_context(nc.semaphore("d_%s_%d" % (q, i))) for i in range(n)],
                              cnt=[0] * n, nxt=0)

    def _wait(self, en, tok):
        if tok is None:
            return
        key, sem, val = tok
        E = self.eng[en]
        if en == "pe" and key == "pe":
            return
        if E["waited"].get(key, 0) >= val:
            return
        E["e"].wait_ge(sem, val)
        E["waited"][key] = val

    def _deps(self, en, reads, writes):
        for r in reads:
            self._wait(en, r.w)
        for w in writes:
            self._wait(en, w.w)
            for t in list(w.r.values()):
                self._wait(en, t)

    def _upd(self, tok, reads, writes):
        for r in reads:
            old = r.r.get(tok[0])
            if old is None or old[2] < tok[2]:
                r.r[tok[0]] = tok
        for w in writes:
            w.w = tok
            w.r = {}

    def op(self, en, fn, reads=(), writes=(), inc=True):
        E = self.eng[en]
        self._deps(en, reads, writes)
        ins = fn()
        if inc:
            E["n"] += 1
            ins.then_inc(E["sem"], 1)
            tok = (en, E["sem"], E["n"])
        else:
            tok = (en, E["sem"], E["n"] + 1)
        self._upd(tok, reads, writes)
        return tok

    def dma(self, q, out, in_, reads=(), writes=(), indirect=None, extra_waits=(), **kw):
        P = self.dp[q]
        i = P["nxt"]
        P["nxt"] = (i + 1) % len(P["sems"])
        key = "d_%s_%d" % (q, i)
        sem = P["sems"][i]
        if P["cnt"][i] > 0:
            self._wait(q, (key, sem, P["cnt"][i]))
        self._deps(q, reads, writes)
        for t in extra_waits:
            self._wait(q, t)
        E = self.eng[q]
        if indirect is None:
            ins = E["e"].dma_start(out=out, in_=in_, **kw)
        else:
            ins = E["e"].indirect_dma_start(out=out, in_=in_, **indirect)
        P["cnt"][i] += 16
        ins.then_inc(sem, 16)
        tok = (key, sem, P["cnt"][i])
        self._upd(tok, reads, writes)
        return tok

    def all_tokens(self):
        toks = []
        for name, E in self.eng.items():
            if E["sem"] is not None and E["n"] > 0:
                toks.append((name, E["sem"], E["n"]))
        for q, P in self.dp.items():
            for i, s in enumerate(P["sems"]):
                if P["cnt"][i] > 0:
                    toks.append(("d_%s_%d" % (q, i), s, P["cnt"][i]))
        return toks

    def barrier(self):
        toks = self.all_tokens()
        for en in ("pe", "act", "dve", "pool", "sp"):
            for t in toks:
                self._wait(en, t)


class Ctx:
    pass


def rope_tables():
    t = np.arange(S)
    row = (t // GRID_W).astype(np.float32)
    col = (t % GRID_W).astype(np.float32)
    nf = HD // 4
    inv = (10000.0 ** (-np.arange(nf, dtype=np.float32) / nf)).astype(np.float32)
    ar = row[:, None] * inv
    ac = col[:, None] * inv
    ang = np.concatenate([ar, ar, ac, ac], axis=-1).astype(np.float32)
    cos = np.cos(ang).astype(np.float32)
    sin = np.sin(ang).astype(np.float32)
    sgn = np.ones(HD, np.float32)
    sgn[0:32] = -1.0
    sgn[64:96] = -1.0
    tab = np.zeros((NT, 2, HD), np.float32)
    tab[:CTX, 0, :] = 1.0
    tab[CTX:, 0, :] = cos
    tab[CTX:, 1, :] = sin * sgn
    return np.ascontiguousarray(tab.reshape(NTT, 128, 2 * HD).transpose(1, 0, 2))


def d_tile_sets():
    out = []
    for i in range(32):
        if i == 0:
            js = [0, 1, 2, 3]
            ids = [5 + k for k in range(4)]
        elif i == 1:
            js = [0, 1, 2, 3]
            ids = [9 + k for k in range(4)]
        elif i == 30:
            js = [28, 29, 30, 31]
            ids = [13 + k for k in range(4)]
        elif i == 31:
            js = [28, 29, 30, 31]
            ids = [17 + k for k in range(4)]
        else:
            js = [i - 2, i - 1, i, i + 1, i + 2]
            ids = [0, 1, 2, 3, 4]
        out.append((js, ids))
    return out


def d_tables(rpb):
    tabs = np.full((8, 21, 128, 128), -1e30, np.float32)
    sets = d_tile_sets()
    done = set()
    kl = np.arange(128)
    ql = np.arange(128)
    for i in (2, 0, 1, 30, 31):
        js, ids = sets[i]
        for j, tid in zip(js, ids):
            if tid in done:
                continue
            done.add(tid)
            kr = 2 * j + kl // 64
            kc = kl % 64
            qr = 2 * i + ql // 64
            qc = ql % 64
            rs = np.clip(qr - 4, 0, 56)
            cs = np.clip(qc - 8, 0, 48)
            vis = ((kr[:, None] >= rs[None, :]) & (kr[:, None] < rs[None, :] + 8)
                   & (kc[:, None] >= cs[None, :]) & (kc[:, None] < cs[None, :] + 16))
            ridx = np.clip(kr[:, None] - qr[None, :] + 7, 0, 14)
            cidx = np.clip(kc[:, None] - qc[None, :], -15, 15) + 15
            for h in range(8):
                vals = rpb[h][ridx, cidx]
                tabs[h, tid] = np.where(vis, vals, np.float32(-1e30))
    return np.ascontiguousarray(tabs.transpose(2, 0, 1, 3))


def const_pack():
    c = {}
    c["ident_bf"] = np.eye(128, dtype=np.float32).astype(NBF16)
    c["ident_f"] = np.eye(128, dtype=np.float32)
    c["ropetab"] = rope_tables()
    k = np.arange(128)[:, None]
    q = np.arange(128)[None, :]
    mu = (k >= q).astype(np.float32)
    ml = (k <= q).astype(np.float32)
    bm = np.zeros((128, 2, 4, 128), np.float32)
    bm[:, 0] = mu[:, None, :]
    bm[:, 1] = ml[:, None, :]
    c["bandmask"] = bm.astype(NBF16)
    c["tri"] = np.concatenate([np.ones((128, 128), np.float32),
                               (k < q).astype(np.float32)], axis=1).astype(NBF16)
    c["iota"] = np.tile(np.arange(512, dtype=np.float32)[None, :], (128, 1))
    rows = (np.arange(NTT)[None, :] * 128 + np.arange(128)[:, None])
    tk = np.zeros((128, NTT, 2), np.float32)
    tk[:, :, 0] = rows // 64
    tk[:, :, 1] = rows % 64
    c["tokab"] = tk.astype(NBF16)
    return c


EVEN_TYPES = ["R"] * 8 + ["R"] * 2 + ["V"] * 2 + ["R"] * 8 + ["R"] * 8 + ["V"] * 8
ODD_TYPES = ["NQ"] * 8 + ["NK"] * 2 + ["V"] * 2 + ["P"] * 8 + ["P"] * 8 + ["V"] * 8


def block_maps(types):
    ft, vv = {}, {}
    for b, t in enumerate(types):
        if t == "V":
            vv[b] = len(vv)
        else:
            ft[b] = len(ft)
    return ft, vv


def build_program(dbg=None):
    dbg = dbg or {}
    kinds = dbg.get("kinds", {})
    nc = bass.Bass("TRN2", target_bir_lowering=False)
    K = Ctx()
    K.nc = nc
    K.dbg = dbg
    _cnt = [0]

    def _sbt(name, shape, dt):
        _cnt[0] += 1
        return nc.sbuf_tensor("%s_%d" % (name, _cnt[0]), shape, dt)

    def _pst(name, shape, dt):
        _cnt[0] += 1
        return nc.psum_tensor("%s_%d" % (name, _cnt[0]), shape, dt)
    K.sbt = _sbt
    K.pst = _pst

    def dram(name, shape, dt, kind):
        shape = dbg.get("shapes", {}).get(name, shape)
        return nc.dram_tensor(name, list(shape), dt, kind=kinds.get(name, kind)).ap()

    d = {}
    K.d = d
    d["x"] = dram("x", [S, D], F32, "ExternalInput")
    d["ctx"] = dram("ctx", [CTX, D], F32, "ExternalInput")
    d["cvec"] = dram("cvec", [2, D], F32, "ExternalInput")
    d["w_ada"] = dram("w_ada", [2, D, 6 * D], F32, "ExternalInput")
    d["b_ada"] = dram("b_ada", [2, 6 * D], F32, "ExternalInput")
    d["gvec"] = dram("gvec", [5, D], F32, "ExternalInput")
    d["w_in"] = dram("w_in", [2, D, INW], F32, "ExternalInput")
    d["w_out"] = dram("w_out", [2, D, D], F32, "ExternalInput")
    d["w_router"] = dram("w_router", [2, D, NE], F32, "ExternalInput")
    d["w_gate"] = dram("w_gate", [2, NE, D, DE], F32, "ExternalInput")
    d["w_up"] = dram("w_up", [2, NE, D, DE], F32, "ExternalInput")
    d["w_down"] = dram("w_down", [2, NE, DE, D], F32, "ExternalInput")
    d["small"] = dram("small", [1, 1024], F32, "ExternalInput")
    d["small2"] = dram("small2", [1, 256], F32, "ExternalInput")
    d["rpbtab"] = dram("rpbtab", [128, 8, 21, 128], F32, "ExternalInput")
    d["ident_bf"] = dram("ident_bf", [128, 128], BF16, "ExternalInput")
    d["ident_f"] = dram("ident_f", [128, 128], F32, "ExternalInput")
    d["ropetab"] = dram("ropetab", [128, NTT, 2 * HD], F32, "ExternalInput")
    d["bandmask"] = dram("bandmask", [128, 2, 4, 128], BF16, "ExternalInput")
    d["tri"] = dram("tri", [128, 256], BF16, "ExternalInput")
    d["iota"] = dram("iota", [128, 512], F32, "ExternalInput")
    d["tokab"] = dram("tokab", [128, NTT, 2], BF16, "ExternalInput")
    d["out"] = dram("out", [S, D], F32, "ExternalOutput")
    d["modv"] = dram("modv", [2, 2, 6 * D], F32, "Internal")
    d["hT"] = dram("hT", [NTT, 128, 16 * 128], BF16, "Internal")
    d["FT"] = dram("FT", [26, 128, NT], BF16, "Internal")
    d["VV"] = dram("VV", [NT, 10 * 128], BF16, "Internal")
    d["OO"] = dram("OO", [NT, D], BF16, "Internal")
    d["H2"] = dram("H2", [NT, D], BF16, "Internal")
    for l in range(2):
        for nb in range(4):
            d["XS%d_%d" % (l, nb)] = dram("XS%d_%d" % (l, nb), [NT, 512], F32, "Internal")
    K.rs = {}

    def R(name):
        if name not in K.rs:
            K.rs[name] = Res()
        return K.rs[name]
    K.R = R

    with ExitStack() as es:
        T = Trk(nc, es)
        K.T = T
        K.ident_bf = es.enter_context(K.sbt("ident_bf_s", [128, 128], BF16))
        K.ident_f = es.enter_context(K.sbt("ident_f_s", [128, 128], F32))
        K.aff = es.enter_context(K.sbt("aff_tm", [128, NTT, NE], F32))
        K.idx = es.enter_context(K.sbt("idx_i", [128, NE, 4], I32))
        K.gate = es.enter_context(K.sbt("gate_f", [128, NE, 4], F32))
        K.idxx = es.enter_context(K.sbt("idxx_i", [128, NE], I32))
        K.gatex = es.enter_context(K.sbt("gatex_f", [128, NE], F32))
        T.dma("sp", K.ident_bf[:], d["ident_bf"], writes=[R("ident")])
        T.dma("sp", K.ident_f[:], d["ident_f"], writes=[R("ident")])

        phases = dbg.get("phases")

        def want(p):
            return phases is None or p in phases

        for l in dbg.get("layers", [0, 1]):
            if want("mod"):
                with nc.named_scope("mod%d" % l):
                    phase_mod(K, l)
                T.barrier()
            if want("norm1"):
                with nc.named_scope("norm1%d" % l):
                    phase_norm(K, l, 1)
                T.barrier()
            if want("proj"):
                with nc.named_scope("proj%d" % l):
                    phase_proj(K, l)
                T.barrier()
            if want("attn"):
                with nc.named_scope("attn%d" % l):
                    phase_attn(K, l)
                T.barrier()
            if want("outproj"):
                with nc.named_scope("outproj%d" % l):
                    phase_outproj(K, l)
                T.barrier()
            if want("norm2"):
                with nc.named_scope("norm2%d" % l):
                    phase_norm(K, l, 2)
                T.barrier()
            if want("route"):
                with nc.named_scope("route%d" % l):
                    phase_route(K, l)
                T.barrier()
            if want("moe"):
                with nc.named_scope("moe%d" % l):
                    phase_moe(K, l)
                T.barrier()
        if want("final"):
            phase_final(K)
        T.barrier()
        if dbg.get("dump"):
            for nm_, tl, shp, dt_ in (("dbg_aff", K.aff, [128, NTT * NE], F32), ("dbg_idx", K.idx, [128, NE * 4], I32),
                                     ("dbg_gate", K.gate, [128, NE * 4], F32), ("dbg_idxx", K.idxx, [128, NE], I32),
                                     ("dbg_gatex", K.gatex, [128, NE], F32)):
                o = nc.dram_tensor(nm_, shp, dt_, kind="ExternalOutput").ap()
                src = tl[:]
                if len(src.shape) == 3:
                    src = src.rearrange("p a b -> p (a b)")
                T.dma("sp", o, src)
            T.barrier()
    return nc


def xs_src(K, l, tt, nb):
    d = K.d
    if l == 0:
        if tt < 2:
            return d["ctx"][tt * 128:(tt + 1) * 128, nb * 512:(nb + 1) * 512]
        return d["x"][(tt - 2) * 128:(tt - 1) * 128, nb * 512:(nb + 1) * 512]
    return d["XS%d_%d" % (l - 1, nb)][tt * 128:(tt + 1) * 128, :]


def load_x_tile(K, l, tt, dst, res, stream_res=None):
    T = K.T
    if l == 0:
        src = K.d["ctx"][tt * 128:(tt + 1) * 128, :] if tt < 2 else K.d["x"][(tt - 2) * 128:(tt - 1) * 128, :]
        T.dma("sp", dst[:], src, writes=[res])
        return
    for nb in range(4):
        rd = [K.R("XS%d_%d" % (l - 1, nb))] if l > 0 else []
        T.dma("sp", dst[:, nb * 512:(nb + 1) * 512], xs_src(K, l, tt, nb), reads=rd, writes=[res])


def phase_mod(K, l):
    nc, T, d, R = K.nc, K.T, K.d, K.R
    with ExitStack() as es:
        sb = lambda n, s, dt: es.enter_context(K.sbt(n, s, dt))
        cT = sb("cT", [128, 2, 16], F32)
        cS = sb("cS", [128, 2, 16], F32)
        condT = sb("condT", [128, 16, 2], BF16)
        wch = [sb("wch%d" % i, [128, 16, 512], BF16) for i in range(2)]
        modrow = sb("modrow", [2, 6 * D], F32)
        brow = sb("brow", [2, 6 * D], F32)
        ps = [es.enter_context(K.pst("mps%d" % i, [128, 512], F32)) for i in range(2)]
        r_c, r_cond, r_b, r_mod = Res(), Res(), Res(), Res()
        r_w = [Res(), Res()]
        r_ps = [Res(), Res()]
        with nc.allow_non_contiguous_dma(reason="tiny cond vector transpose load"):
            T.dma("sp", cT[:], d["cvec"].rearrange("w (kc p) -> p w kc", p=128), writes=[r_c])
        T.dma("sp", brow[:], d["b_ada"][l:l + 1, :].partition_broadcast(2), writes=[r_b])
        T.op("act", lambda: nc.scalar.activation(out=cS[:], in_=cT[:], func=AF.Silu), reads=[r_c], writes=[r_cond])
        T.op("dve", lambda: nc.vector.tensor_copy(out=condT[:].rearrange("p k w -> p w k"), in_=cS[:]),
             reads=[r_cond], writes=[r_cond])
        wv = d["w_ada"][l].rearrange("(kc p) n -> p kc n", p=128)
        NB = 24

        def loadw(nb):
            T.dma("pool", wch[nb % 2][:], wv[:, :, nb * 512:(nb + 1) * 512], writes=[r_w[nb % 2]])
        loadw(0)
        for nb in range(NB):
            if nb + 1 < NB:
                loadw(nb + 1)
            b = nb % 2
            for kc in range(16):
                T.op("pe", lambda kc=kc: nc.tensor.matmul(ps[b][0:2, :], lhsT=condT[:, kc, :], rhs=wch[b][:, kc, :],
                                                         start=(kc == 0), stop=(kc == 15)),
                     reads=[r_cond, r_w[b]], writes=[r_ps[b]], inc=(kc == 15))
            T.op("dve", lambda: nc.vector.tensor_tensor(out=modrow[:, nb * 512:(nb + 1) * 512], in0=ps[b][0:2, :],
                                                        in1=brow[:, nb * 512:(nb + 1) * 512], op=ALU.add),
                 reads=[r_ps[b], r_b], writes=[r_mod])
        for c0 in (D, 4 * D):
            T.op("dve", lambda c0=c0: nc.vector.tensor_scalar(out=modrow[:, c0:c0 + D], in0=modrow[:, c0:c0 + D],
                                                              scalar1=1.0, scalar2=None, op0=ALU.add),
                 reads=[r_mod], writes=[r_mod])
        T.dma("sp", d["modv"][l], modrow[:], reads=[r_mod], writes=[R("modv")])


def load_bc(K, dst_ap, src_row_ap, res, reads=()):
    K.T.dma("sp", dst_ap, src_row_ap.partition_broadcast(128), reads=list(reads), writes=[res])


def phase_norm(K, l, which):
    nc, T, d, R = K.nc, K.T, K.d, K.R
    first_tt = 0
    if which == 2:
        first_tt = 0 if l == 0 else 2
    with ExitStack() as es:
        sb = lambda n, s, dt: es.enter_context(K.sbt(n, s, dt))
        A = [sb("nA%d" % i, [128, D], F32) for i in range(2)]
        SH = [sb("nS%d" % i, [128, D], F32) for i in range(2)]
        gb = sb("ngb", [128, D], F32)
        xt = [sb("nxt%d" % i, [128, D], F32) for i in range(4)]
        junk_ = [sb("njunk%d" % i, [128, D], BF16) for i in range(2)]
        hf_ = [sb("nhf%d" % i, [128, D], F32) for i in range(2)]
        hb = [sb("nhb%d" % i, [128, D], BF16) for i in range(2)]
        hT = [sb("nhT%d" % i, [128, 16, 128], BF16) for i in range(2)]
        st_ = [sb("nst%d" % i, [128, 4], F32) for i in range(2)]
        wr = sb("nwr", [128, 16, NE], BF16)
        ex_ = [sb("nex%d" % i, [128, NE], F32) for i in range(2)]
        tp = [es.enter_context(K.pst("ntp%d" % i, [128, 1024], BF16)) for i in range(4)]
        lp_ = [es.enter_context(K.pst("nlp%d" % i, [128, 512], F32)) for i in range(2)]
        r_A, r_x, r_hb, r_hT = Res(), [Res(), Res(), Res(), Res()], [Res(), Res()], [Res(), Res()]
        r_tp, r_lp_, r_wr = [Res(), Res(), Res(), Res()], [Res(), Res()], Res()
        r_hf_, r_st_, r_junk_, r_ex_ = [Res(), Res()], [Res(), Res()], [Res(), Res()], [Res(), Res()]
        sh_c, sc_c = (0, D) if which == 1 else (3 * D, 4 * D)
        grow = l if which == 1 else 2 + l
        load_bc(K, gb[:], d["gvec"][grow:grow + 1, :], r_A)
        for w in range(2):
            load_bc(K, A[w][:], d["modv"][l, w:w + 1, sc_c:sc_c + D], r_A, reads=[R("modv")])
            load_bc(K, SH[w][:], d["modv"][l, w:w + 1, sh_c:sh_c + D], r_A, reads=[R("modv")])
        for w in range(2):
            T.op("dve", lambda w=w: nc.vector.tensor_tensor(out=A[w][:], in0=A[w][:], in1=gb[:], op=ALU.mult),
                 reads=[r_A], writes=[r_A])
        if which == 2:
            T.dma("pool", wr[:], d["w_router"][l].rearrange("(kc p) e -> p kc e", p=128), writes=[r_wr])
        tiles = list(range(first_tt, NTT))
        def load(tt, buf):
            if which == 1:
                load_x_tile(K, l, tt, xt[buf], r_x[buf])
            else:
                for nb in range(4):
                    T.dma("sp", xt[buf][:, nb * 512:(nb + 1) * 512],
                          d["XS%d_%d" % (l, nb)][tt * 128:(tt + 1) * 128, :],
                          reads=[R("XS%d_%d" % (l, nb))], writes=[r_x[buf]])
        def tile_ops(n, tt):
            b = n % 2
            w = 1 if tt < 2 else 0
            junk, hf, st, ex = junk_[b], hf_[b], st_[b], ex_[b]
            r_junk, r_hf, r_st, r_ex = r_junk_[b], r_hf_[b], r_st_[b], r_ex_[b]
            r_lpb = r_lp_[b]
            lpb = lp_[b]
            ops = []
            xb4 = n % 4
            ops.append(lambda: T.op("act", lambda: nc.scalar.activation(out=junk[:], in_=xt[xb4][:], func=AF.Square, accum_out=st[:, 0:1]),
                                    reads=[r_x[xb4]], writes=[r_junk, r_st]))
            ops.append(lambda: T.op("act", lambda: nc.scalar.activation(out=st[:, 1:2], in_=st[:, 0:1], func=AF.Sqrt, scale=1.0 / D, bias=EPS),
                                    reads=[r_st], writes=[r_st]))
            ops.append(lambda: T.op("dve", lambda: nc.vector.reciprocal(out=st[:, 2:3], in_=st[:, 1:2]), reads=[r_st], writes=[r_st]))
            ops.append(lambda: T.op("dve", lambda: nc.vector.scalar_tensor_tensor(out=hf[:], in0=xt[xb4][:], scalar=st[:, 2:3], in1=A[w][:],
                                                                                  op0=ALU.mult, op1=ALU.mult),
                                    reads=[r_x[xb4], r_st, r_A], writes=[r_hf]))
            ops.append(lambda: T.op("dve", lambda: nc.vector.tensor_tensor(out=hb[b][:], in0=hf[:], in1=SH[w][:], op=ALU.add),
                                    reads=[r_hf, r_A], writes=[r_hb[b]]))
            if which == 2:
                ops.append(lambda: T.dma("sp", d["H2"][tt * 128:(tt + 1) * 128, :], hb[b][:], reads=[r_hb[b]], writes=[R("H2")]))

            def trg(g):
                tpg = tp[b * 2 + g]
                r_tpg = r_tp[b * 2 + g]
                for k8 in range(8):
                    kc = g * 8 + k8
                    T.op("pe", lambda kc=kc, k8=k8: nc.tensor.transpose(out=tpg[:, k8 * 128:(k8 + 1) * 128],
                                                                      in_=hb[b][:, kc * 128:(kc + 1) * 128], identity=K.ident_bf[:]),
                         reads=[r_hb[b], R("ident")], writes=[r_tpg], inc=(k8 == 7))
                T.op("act", lambda: nc.scalar.copy(out=hT[b][:, g * 8:(g + 1) * 8, :].rearrange("p a b -> p (a b)"), in_=tpg[:]),
                     reads=[r_tpg], writes=[r_hT[b]])
            ops.append(lambda: trg(0))
            ops.append(lambda: trg(1))
            if which == 1:
                ops.append(lambda: T.dma("sp", d["hT"][tt], hT[b][:].rearrange("p a b -> p (a b)"), reads=[r_hT[b]], writes=[R("hT")]))
            else:
                def router():
                    for kc in range(16):
                        T.op("pe", lambda kc=kc: nc.tensor.matmul(lpb[:, 0:NE], lhsT=hT[b][:, kc, :], rhs=wr[:, kc, :],
                                                                 start=(kc == 0), stop=(kc == 15)),
                             reads=[r_hT[b], r_wr], writes=[r_lpb], inc=(kc == 15))
                ops.append(router)
                ops.append(lambda: T.op("act", lambda: nc.scalar.activation(out=ex[:], in_=lpb[:, 0:NE], func=AF.Exp, accum_out=st[:, 3:4]),
                                        reads=[r_lpb], writes=[r_ex, r_st]))
                ops.append(lambda: T.op("dve", lambda: nc.vector.reciprocal(out=st[:, 3:4], in_=st[:, 3:4]), reads=[r_st], writes=[r_st]))
                ops.append(lambda: T.op("dve", lambda: nc.vector.tensor_scalar(out=K.aff[:, tt, :], in0=ex[:], scalar1=st[:, 3:4],
                                                                               scalar2=None, op0=ALU.mult),
                                        reads=[r_ex, r_st], writes=[K.R("aff")]))
            return ops

        for q0 in range(0, min(2, len(tiles))):
            load(tiles[q0], q0 % 4)
        for p0 in range(0, len(tiles), 2):
            for q0 in range(p0 + 2, min(p0 + 4, len(tiles))):
                load(tiles[q0], q0 % 4)
            oa = tile_ops(p0, tiles[p0])
            ob = tile_ops(p0 + 1, tiles[p0 + 1]) if p0 + 1 < len(tiles) else []
            for i in range(max(len(oa), len(ob))):
                if i < len(oa):
                    oa[i]()
                if i < len(ob):
                    ob[i]()


def phase_proj(K, l):
    nc, T, d, R = K.nc, K.T, K.d, K.R
    types = EVEN_TYPES if l % 2 == 0 else ODD_TYPES
    ftmap, vvmap = block_maps(types)
    with ExitStack() as es:
        sb = lambda n, s, dt: es.enter_context(K.sbt(n, s, dt))
        wch = [sb("pw%d" % i, [128, 16, 512], BF16) for i in range(2)]
        hT = [sb("phT%d" % i, [128, 16, 128], BF16) for i in range(4)]
        rope = sb("prope", [128, NTT, 2 * HD], F32)
        FTst = sb("pFT", [128, 4, NT], BF16)
        Vst = [sb("pV%d" % i, [128, 512], BF16) for i in range(2)]
        xs = [sb("pxs%d" % i, [128, 512], F32) for i in range(2)]
        t1 = sb("pt1", [128, 512], F32)
        t2 = sb("pt2", [128, 512], F32)
        xb = [sb("pxb%d" % i, [128, 512], BF16) for i in range(2)]
        nrm = sb("pnrm", [128, 2, 128], F32)
        sq = sb("psq", [128, 512], F32)
        st = sb("pst", [128, 8], F32)
        ps = [es.enter_context(K.pst("pps%d" % i, [128, 512], F32)) for i in range(2)]
        tp = [es.enter_context(K.pst("ptp%d" % i, [128, 512], BF16)) for i in range(2)]
        r_w, r_hT = [Res(), Res()], [Res(), Res(), Res(), Res()]
        r_rope, r_FT, r_V, r_xs, r_t, r_xb = Res(), Res(), [Res(), Res()], [Res(), Res()], Res(), [Res(), Res()]
        r_ps, r_tp, r_nrm, r_st = [Res(), Res()], [Res(), Res()], Res(), Res()
        T.dma("sp", rope[:], d["ropetab"], writes=[r_rope])
        if l % 2 == 1:
            load_bc(K, nrm[:].rearrange("p a b -> p (a b)"), d["small2"][0:1, :], r_nrm)
        wv = d["w_in"][l].rearrange("(kc p) n -> p kc n", p=128)

        def loadw(cb):
            T.dma("pool", wch[cb % 2][:], wv[:, :, cb * 512:(cb + 1) * 512], writes=[r_w[cb % 2]])

        def loadh(n):
            tt = n % NTT
            T.dma("sp", hT[n % 4][:].rearrange("p a b -> p (a b)"), d["hT"][tt], reads=[R("hT")], writes=[r_hT[n % 4]])
        loadw(0)
        loadh(0)
        loadh(1)
        loadh(2)
        n = 0
        deferred = []

        def mk_B(tb, xbuf, b0, nbk, c0, c1, tt):
            def fn_():
                for k in range(nbk):
                    T.op("pe", lambda k=k: nc.tensor.transpose(out=tp[tb][:, (b0 + k) * 128:(b0 + k + 1) * 128],
                                                               in_=xb[xbuf][:, (b0 + k) * 128:(b0 + k + 1) * 128],
                                                               identity=K.ident_bf[:]),
                         reads=[r_xb[xbuf], R("ident")], writes=[r_tp[tb]], inc=(k == nbk - 1))
                T.op("act", lambda: nc.scalar.copy(out=FTst[:, b0:b0 + nbk, tt * 128:(tt + 1) * 128],
                                                   in_=tp[tb][:, c0:c1].rearrange("p (h e) -> p h e", e=128)),
                     reads=[r_tp[tb]], writes=[r_FT])
            return fn_

        def mk_flush(cb, btypes):
            def fn_():
                for bi, t in enumerate(btypes):
                    if t == "V":
                        continue
                    T.dma("sp", d["FT"][ftmap[cb * 4 + bi]], FTst[:, bi, :], reads=[r_FT], writes=[R("FT")])
            return fn_
        for cb in range(9):
            if cb + 1 < 9:
                loadw(cb + 1)
            btypes = types[cb * 4:(cb + 1) * 4]
            for tt in range(NTT):
                if n + 3 < 9 * NTT:
                    loadh(n + 3)
                hb_ = n % 4
                pb = n % 2
                for kc in range(16):
                    T.op("pe", lambda kc=kc: nc.tensor.matmul(ps[pb][:], lhsT=hT[hb_][:, kc, :], rhs=wch[cb % 2][:, kc, :],
                                                             start=(kc == 0), stop=(kc == 15)),
                         reads=[r_hT[hb_], r_w[cb % 2]], writes=[r_ps[pb]], inc=(kc == 15))
                for fn_ in deferred:
                    fn_()
                deferred = []
                groups = []
                for bi, t in enumerate(btypes):
                    if groups and groups[-1][0] == t:
                        groups[-1][2] += 1
                    else:
                        groups.append([t, bi, 1])
                for (t, b0, nbk) in groups:
                    c0, c1 = b0 * 128, (b0 + nbk) * 128
                    if t == "V":
                        vb = n % 2
                        T.op("act", lambda: nc.scalar.copy(out=Vst[vb][:, c0:c1], in_=ps[pb][:, c0:c1]),
                             reads=[r_ps[pb]], writes=[r_V[vb]])
                        v0 = vvmap[cb * 4 + b0]
                        T.dma("sp", d["VV"][tt * 128:(tt + 1) * 128, v0 * 128:(v0 + nbk) * 128], Vst[vb][:, c0:c1],
                              reads=[r_V[vb]], writes=[R("VV")])
                        continue
                    xbuf = n % 2
                    if t == "P":
                        T.op("act", lambda: nc.scalar.copy(out=xb[xbuf][:, c0:c1], in_=ps[pb][:, c0:c1]),
                             reads=[r_ps[pb]], writes=[r_xb[xbuf]])
                    else:
                        T.op("act", lambda: nc.scalar.copy(out=xs[xbuf][:, c0:c1], in_=ps[pb][:, c0:c1]),
                             reads=[r_ps[pb]], writes=[r_xs[xbuf]])
                        xv = xs[xbuf][:, c0:c1].rearrange("p (h e) -> p h e", e=128)
                        if t in ("NQ", "NK"):
                            wsel = 0 if t == "NQ" else 1
                            T.op("act", lambda: nc.scalar.activation(out=sq[:, c0:c1], in_=xs[xbuf][:, c0:c1], func=AF.Square),
                                 reads=[r_xs[xbuf]], writes=[r_t])
                            T.op("dve", lambda: nc.vector.tensor_reduce(out=st[:, 0:nbk], in_=sq[:, c0:c1].rearrange("p (h e) -> p h e", e=128),
                                                                        axis=mybir.AxisListType.X, op=ALU.add),
                                 reads=[r_t], writes=[r_st])
                            T.op("act", lambda: nc.scalar.activation(out=st[:, 0:nbk], in_=st[:, 0:nbk], func=AF.Sqrt,
                                                                     scale=1.0 / HD, bias=EPS),
                                 reads=[r_st], writes=[r_st])
                            T.op("dve", lambda: nc.vector.reciprocal(out=st[:, 4:4 + nbk], in_=st[:, 0:nbk]),
                                 reads=[r_st], writes=[r_st])
                            T.op("dve", lambda: nc.vector.tensor_tensor(out=xv, in0=xv,
                                                                        in1=st[:, 4:4 + nbk].unsqueeze(2).to_broadcast([128, nbk, 128]),
                                                                        op=ALU.mult),
                                 reads=[r_xs[xbuf], r_st], writes=[r_xs[xbuf]])
                            T.op("dve", lambda: nc.vector.tensor_tensor(out=xv, in0=xv,
                                                                        in1=nrm[:, wsel, :].unsqueeze(1).to_broadcast([128, nbk, 128]),
                                                                        op=ALU.mult),
                                 reads=[r_xs[xbuf], r_nrm], writes=[r_xs[xbuf]])
                        cosb = rope[:, tt, 0:HD].unsqueeze(1).to_broadcast([128, nbk, HD])
                        t1v = t1[:, c0:c1].rearrange("p (h e) -> p h e", e=128)
                        T.op("dve", lambda: nc.vector.tensor_tensor(out=t1v, in0=xv, in1=cosb, op=ALU.mult),
                             reads=[r_xs[xbuf], r_rope], writes=[r_t])
                        x5 = xs[xbuf][:, c0:c1].rearrange("p (h a j f) -> p (h a) j f", a=2, j=2, f=32)
                        t5 = t2[:, c0:c1].rearrange("p (h a j f) -> p (h a) j f", a=2, j=2, f=32)
                        s5 = rope[:, tt, HD:2 * HD].rearrange("p (a j f) -> p a j f", a=2, j=2, f=32)
                        for j in range(2):
                            for a in range(2):
                                T.op("dve", lambda j=j, a=a: nc.vector.tensor_tensor(
                                    out=t2[:, c0:c1].rearrange("p (h a j f) -> p h a j f", a=2, j=2, f=32)[:, :, a, j, :],
                                    in0=xs[xbuf][:, c0:c1].rearrange("p (h a j f) -> p h a j f", a=2, j=2, f=32)[:, :, a, 1 - j, :],
                                    in1=s5[:, a, j, :].unsqueeze(1).to_broadcast([128, nbk, 32]), op=ALU.mult),
                                    reads=[r_xs[xbuf], r_rope], writes=[r_t])
                        T.op("dve", lambda: nc.vector.tensor_tensor(out=xb[xbuf][:, c0:c1], in0=t1[:, c0:c1], in1=t2[:, c0:c1], op=ALU.add),
                             reads=[r_t], writes=[r_xb[xbuf]])
                    deferred.append(mk_B(n % 2, xbuf, b0, nbk, c0, c1, tt))
                n += 1
            deferred.append(mk_flush(cb, btypes))
        for fn_ in deferred:
            fn_()


def attn_group(K, A, qap, ncols, keys, dvp, finish):
    nc, T = K.nc, K.T
    nsub = ncols // 128
    n = len(keys)
    S_ps, P_sb = A["S"], A["P"]
    nset = max(1, len(A["acc"]) // nsub)
    off = (A.get("gcnt", 0) % nset) * nsub
    A["gcnt"] = A.get("gcnt", 0) + 1
    acc = A["acc"][off:off + nsub]
    r_S, r_P, r_acc = A["r_S"], A["r_P"], A["r_acc"][off:off + nsub]
    rq, rk, rv, rt = A["rq"], A["rk"], A["rv"], A["rt"]

    def qk(k):
        sbuf = A["cnt"] % 3
        A["scur"][k] = sbuf
        kT, _, _ = keys[k]
        T.op("pe", lambda: nc.tensor.matmul(S_ps[sbuf][:, 0:ncols], lhsT=kT, rhs=qap, start=True, stop=True),
             reads=[rq, rk], writes=[r_S[sbuf]])
        A["cnt"] += 1
    A["scur"] = {}
    qk(0)
    if n > 1:
        qk(1)
    for k in range(n):
        if k + 2 < n:
            qk(k + 2)
        sbuf = A["scur"][k]
        pb = A["pcnt"] % 3
        A["pcnt"] += 1
        _, v, tbl = keys[k]
        T.op("act", lambda: nc.scalar.activation(out=P_sb[pb][:, 0:ncols], in_=S_ps[sbuf][:, 0:ncols], func=AF.Exp, scale=SCALE),
             reads=[r_S[sbuf]], writes=[r_P[pb]])
        if tbl is not None:
            T.op("dve", lambda: nc.vector.tensor_tensor(out=P_sb[pb][:, 0:ncols], in0=P_sb[pb][:, 0:ncols], in1=tbl, op=ALU.mult),
                 reads=[rt], writes=[r_P[pb]])
        for s in range(nsub):
            T.op("pe", lambda s=s: nc.tensor.matmul(acc[s][:, 0:dvp], lhsT=P_sb[pb][:, s * 128:(s + 1) * 128], rhs=v,
                                                   start=(k == 0), stop=(k == n - 1)),
                 reads=[r_P[pb], rv], writes=[r_acc[s]], inc=(k == n - 1 or s == nsub - 1))
    for s in range(nsub):
        finish(s, acc[s], r_acc[s])


def attn_bufs(K, es, nacc=4):
    nc = K.nc
    A = {}
    A["S"] = [es.enter_context(K.pst("aS%d" % i, [128, 512], F32)) for i in range(3)]
    A["acc"] = [es.enter_context(K.pst("aacc%d" % i, [128, 512], F32)) for i in range(nacc)]
    A["P"] = [es.enter_context(K.sbt("aP%d" % i, [128, 512], BF16)) for i in range(3)]
    A["r_S"] = [Res(), Res(), Res()]
    A["r_P"] = [Res(), Res(), Res()]
    A["r_acc"] = [Res() for _ in range(nacc)]
    A["cnt"] = 0
    A["pcnt"] = 0
    A["rt"] = Res()
    return A


def phase_attn(K, l):
    nc, T, d, R = K.nc, K.T, K.d, K.R
    types = EVEN_TYPES if l % 2 == 0 else ODD_TYPES
    ftmap, vvmap = block_maps(types)
    FT, VV, OO = d["FT"], d["VV"], d["OO"]
    lat_tiles = list(range(2, NTT))

    def ftv(blk):
        return FT[ftmap[blk]].rearrange("p (t c) -> p t c", c=128)

    with ExitStack() as es:
        sb = lambda n, s, dt: es.enter_context(K.sbt(n, s, dt))
        A = attn_bufs(K, es)
        gsets = [dict(QA=sb("aQA%d" % i, [128, NTT, 4, 128], BF16), KT=sb("aKT%d" % i, [128, NT], BF16),
                      VA=sb("aVA%d" % i, [128, NTT, 129], BF16), rq=Res(), rk=Res(), rv=Res()) for i in range(2)]
        Ost = [sb("aO%d" % i, [128, 512], BF16) for i in range(2)]
        rc = [sb("arc%d" % i, [128, 4], F32) for i in range(2)]
        esk = sb("aesk", [128, 8], F32)
        bm = sb("abm", [128, 2, 4, 128], BF16)
        r_es, r_O, r_rc = Res(), [Res(), Res()], [Res(), Res()]
        A["rq"], A["rk"], A["rv"] = Res(), Res(), Res()
        if l % 2 == 0:
            load_bc(K, esk[:], d["small"][0:1, 0:8], r_es)
            T.op("act", lambda: nc.scalar.activation(out=esk[:], in_=esk[:], func=AF.Exp), reads=[r_es], writes=[r_es])
            T.dma("sp", bm[:], d["bandmask"], writes=[A["rt"]])
        on = 0

        def loadg(g):
            G_ = gsets[g % 2]
            for hh in range(4):
                T.dma("sp", G_["QA"][:, :, hh, :], ftv(g * 4 + hh), reads=[R("FT")], writes=[G_["rq"]])
            T.dma("sp", G_["KT"][:], FT[ftmap[8 + g]], reads=[R("FT")], writes=[G_["rk"]])
            T.op("dve", lambda: nc.vector.memset(G_["VA"][:, :, 128:129], 1.0), writes=[G_["rv"]])
            T.dma("sp", G_["VA"][:, :, 0:128], VV[:, vvmap[10 + g] * 128:(vvmap[10 + g] + 1) * 128].rearrange("(t p) c -> p t c", p=128),
                  reads=[R("VV")], writes=[G_["rv"]])
        loadg(0)
        for g in range(2):
            if g + 1 < 2:
                loadg(g + 1)
            G_ = gsets[g % 2]
            QA, KT, VA = G_["QA"], G_["KT"], G_["VA"]
            A["rq"], A["rk"], A["rv"] = G_["rq"], G_["rk"], G_["rv"]
            qtiles = list(range(NTT)) if l % 2 == 0 else lat_tiles
            for i in qtiles:
                if l % 2 == 0:
                    if i < 2:
                        kl = [(0, None), (1, None)]
                    else:
                        kl = [(0, None), (1, None)]
                        if i - 1 >= 2:
                            kl.append((i - 1, 0))
                        kl.append((i, None))
                        if i + 1 < NTT:
                            kl.append((i + 1, 1))
                else:
                    kl = [(j, None) for j in range(NTT)]
                keys = [(KT[:, j * 128:(j + 1) * 128], VA[:, j, :],
                         None if m is None else bm[:, m, :, :].rearrange("p h q -> p (h q)")) for (j, m) in kl]
                ob = on % 2
                on += 1

                def finish(s, acc, r_acc, ob=ob, g=g):
                    if l % 2 == 0:
                        T.op("dve", lambda: nc.vector.tensor_scalar(out=rc[ob][:, s:s + 1], in0=acc[:, 128:129],
                                                                    scalar1=esk[:, g * 4 + s:g * 4 + s + 1], scalar2=None, op0=ALU.add),
                             reads=[r_acc, r_es], writes=[r_rc[ob]])
                        T.op("dve", lambda: nc.vector.reciprocal(out=rc[ob][:, s:s + 1], in_=rc[ob][:, s:s + 1]),
                             reads=[r_rc[ob]], writes=[r_rc[ob]])
                    else:
                        T.op("dve", lambda: nc.vector.reciprocal(out=rc[ob][:, s:s + 1], in_=acc[:, 128:129]),
                             reads=[r_acc], writes=[r_rc[ob]])
                    T.op("dve", lambda: nc.vector.tensor_scalar(out=Ost[ob][:, s * 128:(s + 1) * 128], in0=acc[:, 0:128],
                                                                scalar1=rc[ob][:, s:s + 1], scalar2=None, op0=ALU.mult),
                         reads=[r_acc, r_rc[ob]], writes=[r_O[ob]])
                attn_group(K, A, QA[:, i, :, :].rearrange("p h c -> p (h c)"), 512, keys, 129, finish)
                T.dma("sp", OO[i * 128:(i + 1) * 128, g * 512:(g + 1) * 512], Ost[ob][:], reads=[r_O[ob]], writes=[R("OO")])
    T.barrier()
    if l % 2 == 0:
        attn_B(K, l, ftmap, vvmap)
    else:
        attn_D(K, l, ftmap, vvmap)


def attn_B(K, l, ftmap, vvmap):
    nc, T, d, R = K.nc, K.T, K.d, K.R
    FT, VV, OO = d["FT"], d["VV"], d["OO"]
    lam_init = lambda_init_for(l)
    with ExitStack() as es:
        sb = lambda n, s, dt: es.enter_context(K.sbt(n, s, dt))
        A = attn_bufs(K, es)
        bsets = [dict(QT=[sb("bQ%d_%d" % (i, m), [128, NT], BF16) for m in range(2)],
                      KT=[sb("bK%d_%d" % (i, m), [128, NT], BF16) for m in range(2)],
                      VB=sb("bV%d" % i, [128, NTT, 257], BF16), r_q=[Res(), Res()], r_k=[Res(), Res()], r_v=Res()) for i in range(2)]
        o1 = sb("bo1", [128, 4, 256], F32)
        o2 = sb("bo2", [128, 4, 256], F32)
        Ost = [sb("bO%d" % i, [128, 4, 256], BF16) for i in range(2)]
        rc = sb("brc", [128, 8], F32)
        lamv = sb("blam", [128, 4, 128], F32)
        lamt = sb("blamt", [128, 2, 128], F32)
        lams = sb("blams", [128, 4], F32)
        subw = sb("bsub", [128, 256], F32)
        sqj = sb("bsq", [128, 256], F32)
        r_o, r_O, r_rc, r_lam, r_sub = Res(), [Res(), Res()], Res(), Res(), Res()
        load_bc(K, lamv[:].rearrange("p a b -> p (a b)"), d["small"][0:1, 8:520], r_lam)
        load_bc(K, subw[:], d["small"][0:1, 520:776], r_sub)
        T.op("dve", lambda: nc.vector.tensor_tensor(out=lamt[:], in0=lamv[:, 0:4:2, :], in1=lamv[:, 1:4:2, :], op=ALU.mult),
             reads=[r_lam], writes=[r_lam])
        T.op("dve", lambda: nc.vector.tensor_reduce(out=lams[:, 0:2], in_=lamt[:], axis=mybir.AxisListType.X, op=ALU.add),
             reads=[r_lam], writes=[r_lam])
        T.op("act", lambda: nc.scalar.activation(out=lams[:, 0:2], in_=lams[:, 0:2], func=AF.Exp), reads=[r_lam], writes=[r_lam])
        T.op("dve", lambda: nc.vector.tensor_tensor(out=lams[:, 2:3], in0=lams[:, 1:2], in1=lams[:, 0:1], op=ALU.subtract),
             reads=[r_lam], writes=[r_lam])
        T.op("dve", lambda: nc.vector.tensor_scalar(out=lams[:, 2:3], in0=lams[:, 2:3], scalar1=-lam_init, scalar2=None, op0=ALU.add),
             reads=[r_lam], writes=[r_lam])
        T.op("dve", lambda: nc.vector.tensor_scalar(out=subw[:], in0=subw[:], scalar1=1.0 - lam_init, scalar2=None, op0=ALU.mult),
             reads=[r_sub], writes=[r_sub])
        on = 0

        def loadb(hb):
            B_ = bsets[hb % 2]
            for m in range(2):
                T.dma("sp", B_["QT"][m][:], FT[ftmap[12 + hb * 2 + m]], reads=[R("FT")], writes=[B_["r_q"][m]])
                T.dma("sp", B_["KT"][m][:], FT[ftmap[20 + hb * 2 + m]], reads=[R("FT")], writes=[B_["r_k"][m]])
            T.op("dve", lambda: nc.vector.memset(B_["VB"][:, :, 256:257], 1.0), writes=[B_["r_v"]])
            v0 = vvmap[28 + hb * 2]
            T.dma("sp", B_["VB"][:, :, 0:256], VV[:, v0 * 128:(v0 + 2) * 128].rearrange("(t p) c -> p t c", p=128),
                  reads=[R("VV")], writes=[B_["r_v"]])
        loadb(0)
        for hb in range(4):
            if hb + 1 < 4:
                loadb(hb + 1)
            B_ = bsets[hb % 2]
            QT, KT, VB = B_["QT"], B_["KT"], B_["VB"]
            r_q, r_k, r_v = B_["r_q"], B_["r_k"], B_["r_v"]
            chunks = [(0, 2, [0, 1])] + [(2 + 4 * c, 4, list(range(NTT))) for c in range(8)]
            for (t0, ntile, kts) in chunks:
                ob = on % 2
                on += 1
                for m in range(2):
                    A["rq"], A["rk"], A["rv"] = r_q[m], r_k[m], r_v
                    keys = [(KT[m][:, j * 128:(j + 1) * 128], VB[:, j, :], None) for j in kts]
                    dst = o1 if m == 0 else o2

                    def finish(s, acc, r_acc, m=m, dst=dst):
                        T.op("dve", lambda: nc.vector.reciprocal(out=rc[:, m * 4 + s:m * 4 + s + 1], in_=acc[:, 256:257]),
                             reads=[r_acc], writes=[r_rc])
                        T.op("dve", lambda: nc.vector.tensor_scalar(out=dst[:, s, :], in0=acc[:, 0:256],
                                                                    scalar1=rc[:, m * 4 + s:m * 4 + s + 1], scalar2=None, op0=ALU.mult),
                             reads=[r_acc, r_rc], writes=[r_o])
                    attn_group(K, A, QT[m][:, t0 * 128:(t0 + ntile) * 128], ntile * 128, keys, 257, finish)
                for s in range(ntile):
                    T.op("dve", lambda s=s: nc.vector.scalar_tensor_tensor(out=o1[:, s, :], in0=o2[:, s, :], scalar=lams[:, 2:3],
                                                                           in1=o1[:, s, :], op0=ALU.mult, op1=ALU.add),
                         reads=[r_o, r_lam], writes=[r_o])
                    T.op("act", lambda s=s: nc.scalar.activation(out=sqj[:], in_=o1[:, s, :], func=AF.Square, accum_out=rc[:, s:s + 1]),
                         reads=[r_o], writes=[r_rc])
                    T.op("act", lambda s=s: nc.scalar.activation(out=rc[:, s:s + 1], in_=rc[:, s:s + 1], func=AF.Sqrt,
                                                                 scale=1.0 / 256, bias=EPS),
                         reads=[r_rc], writes=[r_rc])
                    T.op("dve", lambda s=s: nc.vector.reciprocal(out=rc[:, s:s + 1], in_=rc[:, s:s + 1]), reads=[r_rc], writes=[r_rc])
                    T.op("dve", lambda s=s: nc.vector.scalar_tensor_tensor(out=Ost[ob][:, s, :], in0=o1[:, s, :], scalar=rc[:, s:s + 1],
                                                                           in1=subw[:], op0=ALU.mult, op1=ALU.mult),
                         reads=[r_o, r_rc, r_sub], writes=[r_O[ob]])
                T.dma("sp", OO[t0 * 128:(t0 + ntile) * 128, 1024 + hb * 256:1024 + (hb + 1) * 256].rearrange("(t p) c -> p t c", p=128),
                      Ost[ob][:, 0:ntile, :], reads=[r_O[ob]], writes=[R("OO")])


def attn_D(K, l, ftmap, vvmap):
    nc, T, d, R = K.nc, K.T, K.d, K.R
    FT, VV, OO = d["FT"], d["VV"], d["OO"]
    sets = d_tile_sets()
    with ExitStack() as es:
        sb = lambda n, s, dt: es.enter_context(K.sbt(n, s, dt))
        A = attn_bufs(K, es, nacc=2)
        dsets = [dict(QT=sb("dQ%d" % i, [128, NT], BF16), KT=sb("dK%d" % i, [128, NT], BF16), VD=sb("dV%d" % i, [128, NTT, 129], BF16),
                      tab=sb("dtab%d" % i, [128, 21, 128], BF16), rq=Res(), rk=Res(), rv=Res(), rt=Res()) for i in range(2)]
        raw = sb("draw", [128, 21, 128], F32)
        Ost = [sb("dO%d" % i, [128, 128], BF16) for i in range(2)]
        rc = sb("drc", [128, 2], F32)
        r_raw, r_O, r_rc = Res(), [Res(), Res()], Res()
        on = 0

        def loadd(h):
            D_ = dsets[h % 2]
            T.dma("sp", raw[:], d["rpbtab"][:, h, :, :], writes=[r_raw])
            T.op("act", lambda: nc.scalar.activation(out=D_["tab"][:], in_=raw[:], func=AF.Exp), reads=[r_raw], writes=[D_["rt"]])
            T.dma("sp", D_["QT"][:], FT[ftmap[12 + h]], reads=[R("FT")], writes=[D_["rq"]])
            T.dma("sp", D_["KT"][:], FT[ftmap[20 + h]], reads=[R("FT")], writes=[D_["rk"]])
            T.op("dve", lambda: nc.vector.memset(D_["VD"][:, :, 128:129], 1.0), writes=[D_["rv"]])
            v0 = vvmap[28 + h]
            T.dma("sp", D_["VD"][:, :, 0:128], VV[:, v0 * 128:(v0 + 1) * 128].rearrange("(t p) c -> p t c", p=128),
                  reads=[R("VV")], writes=[D_["rv"]])
        loadd(0)
        for h in range(8):
            if h + 1 < 8:
                loadd(h + 1)
            D_ = dsets[h % 2]
            QT, KT, VD, tab = D_["QT"], D_["KT"], D_["VD"], D_["tab"]
            A["rq"], A["rk"], A["rv"], A["rt"] = D_["rq"], D_["rk"], D_["rv"], D_["rt"]
            for i in range(32):
                js, ids = sets[i]
                keys = [(KT[:, j * 128:(j + 1) * 128], VD[:, j, :], None) for j in (0, 1)]
                keys += [(KT[:, (2 + j) * 128:(3 + j) * 128], VD[:, 2 + j, :], tab[:, tid, :]) for j, tid in zip(js, ids)]
                ob = on % 2
                on += 1

                def finish(s, acc, r_acc, ob=ob):
                    T.op("dve", lambda: nc.vector.reciprocal(out=rc[:, 0:1], in_=acc[:, 128:129]), reads=[r_acc], writes=[r_rc])
                    T.op("dve", lambda: nc.vector.tensor_scalar(out=Ost[ob][:], in0=acc[:, 0:128], scalar1=rc[:, 0:1],
                                                                scalar2=None, op0=ALU.mult),
                         reads=[r_acc, r_rc], writes=[r_O[ob]])
                attn_group(K, A, QT[:, (2 + i) * 128:(3 + i) * 128], 128, keys, 129, finish)
                T.dma("sp", OO[(2 + i) * 128:(3 + i) * 128, 1024 + h * 128:1024 + (h + 1) * 128], Ost[ob][:],
                      reads=[r_O[ob]], writes=[R("OO")])


def phase_outproj(K, l):
    nc, T, d, R = K.nc, K.T, K.d, K.R
    first_tt = 0 if l == 0 else 2
    with ExitStack() as es:
        sb = lambda n, s, dt: es.enter_context(K.sbt(n, s, dt))
        W = sb("oW", [128, 16, D], BF16)
        gt = [sb("ogt%d" % i, [128, D], F32) for i in range(2)]
        Ot = [sb("oO%d" % i, [128, D], BF16) for i in range(2)]
        OT = [sb("oOT%d" % i, [128, 16, 128], BF16) for i in range(2)]
        xt = [sb("oxt%d" % i, [128, D], F32) for i in range(2)]
        xn = [sb("oxn%d" % i, [128, D], F32) for i in range(2)]
        tp = [es.enter_context(K.pst("otp%d" % i, [128, 1024], BF16)) for i in range(2)]
        ps = [es.enter_context(K.pst("ops%d" % i, [128, 512], F32)) for i in range(2)]
        r_W, r_gt, r_O, r_OT, r_x, r_xn = Res(), Res(), [Res(), Res()], [Res(), Res()], [Res(), Res()], [Res(), Res()]
        r_tp, r_ps = [Res(), Res()], [Res(), Res()]
        wv = d["w_out"][l].rearrange("(kc p) n -> p kc n", p=128)
        for nb in range(4):
            T.dma("pool", W[:, :, nb * 512:(nb + 1) * 512], wv[:, :, nb * 512:(nb + 1) * 512], writes=[r_W])
        for w in range(2):
            load_bc(K, gt[w][:], d["modv"][l, w:w + 1, 2 * D:3 * D], r_gt, reads=[R("modv")])
        tiles = list(range(first_tt, NTT))

        def load(tt, b):
            T.dma("sp", Ot[b][:], d["OO"][tt * 128:(tt + 1) * 128, :], reads=[R("OO")], writes=[r_O[b]])
            load_x_tile(K, l, tt, xt[b], r_x[b])
        load(tiles[0], 0)
        pn = [0]

        def do_tr(n):
            b = n % 2
            for g in range(2):
                for k8 in range(8):
                    kc = g * 8 + k8
                    T.op("pe", lambda kc=kc, k8=k8, g=g: nc.tensor.transpose(out=tp[g][:, k8 * 128:(k8 + 1) * 128],
                                                                           in_=Ot[b][:, kc * 128:(kc + 1) * 128],
                                                                           identity=K.ident_bf[:]),
                         reads=[r_O[b], R("ident")], writes=[r_tp[g]], inc=(k8 == 7))
                T.op("act", lambda g=g: nc.scalar.copy(out=OT[b][:, g * 8:(g + 1) * 8, :].rearrange("p a b -> p (a b)"), in_=tp[g][:]),
                     reads=[r_tp[g]], writes=[r_OT[b]])

        def do_mm(n, tt):
            b = n % 2
            w = 1 if tt < 2 else 0
            for nb in range(4):
                pb = pn[0] % 2
                pn[0] += 1
                for kc in range(16):
                    T.op("pe", lambda kc=kc: nc.tensor.matmul(ps[pb][:], lhsT=OT[b][:, kc, :], rhs=W[:, kc, nb * 512:(nb + 1) * 512],
                                                             start=(kc == 0), stop=(kc == 15)),
                         reads=[r_OT[b], r_W], writes=[r_ps[pb]], inc=(kc == 15))
                T.op("dve", lambda: nc.vector.tensor_tensor(out=xn[b][:, nb * 512:(nb + 1) * 512], in0=ps[pb][:],
                                                            in1=gt[w][:, nb * 512:(nb + 1) * 512], op=ALU.mult),
                     reads=[r_ps[pb], r_gt], writes=[r_xn[b]])
                T.op("dve", lambda: nc.vector.tensor_tensor(out=xn[b][:, nb * 512:(nb + 1) * 512], in0=xn[b][:, nb * 512:(nb + 1) * 512],
                                                            in1=xt[b][:, nb * 512:(nb + 1) * 512], op=ALU.add),
                     reads=[r_xn[b], r_x[b]], writes=[r_xn[b]])
                T.dma("sp", d["XS%d_%d" % (l, nb)][tt * 128:(tt + 1) * 128, :], xn[b][:, nb * 512:(nb + 1) * 512],
                      reads=[r_xn[b]], writes=[R("XS%d_%d" % (l, nb))])
        do_tr(0)
        for n, tt in enumerate(tiles):
            if n + 1 < len(tiles):
                load(tiles[n + 1], (n + 1) % 2)
                do_tr(n + 1)
            do_mm(n, tt)


def phase_route(K, l):
    nc, T, d, R = K.nc, K.T, K.d, K.R
    sets = [(2, NTT, CAP)] + ([(0, 2, CAPX)] if l == 0 else [])
    with ExitStack() as es:
        sb = lambda n, s, dt: es.enter_context(K.sbt(n, s, dt))
        affT = sb("raffT", [NE, NT], F32)
        maskT = sb("rmaskT", [NE, NT], BF16)
        junk = sb("rjunk", [NE, S], BF16)
        bs = sb("rbs", [NE, 4], F32)
        mask = sb("rmask", [128, NTT, NE], BF16)
        slot = sb("rslot", [128, NTT, NE], F32)
        tmp = sb("rtmp", [128, NTT, NE], F32)
        Rt = sb("rR", [128, NTT, NE, 5], BF16)
        g1 = sb("rg1", [128, NTT, NE], BF16)
        r1 = sb("rr1", [128, NTT, NE], F32)
        tri = sb("rtri", [128, 256], BF16)
        iota = sb("riota", [128, 512], F32)
        tokab = sb("rtok", [128, NTT, 2], BF16)
        OH = [sb("rOH%d" % i, [128, 512], BF16) for i in range(3)]
        accs = sb("raccs", [128, 4, 5], F32)
        acc5 = sb("racc5", [8, 512], F32)
        r_a5 = Res()
        idf = sb("ridf", [128, 4], F32)
        tp = es.enter_context(K.pst("rtp", [128, 512], F32))
        tpb = es.enter_context(K.pst("rtpb", [128, 1024], BF16))
        pp = [es.enter_context(K.pst("rpp%d" % i, [128, 512], F32)) for i in range(2)]
        ap_ = [es.enter_context(K.pst("rap%d" % i, [128, 512], F32)) for i in range(2)]
        r_affT, r_maskT, r_bs, r_mask, r_slot, r_R, r_c, r_tp, r_tpb = Res(), Res(), Res(), Res(), Res(), Res(), Res(), Res(), Res()
        r_pp, r_ap, r_OH, r_acc, r_j = [Res(), Res()], [Res(), Res()], [Res(), Res(), Res()], Res(), Res()
        T.dma("sp", tri[:], d["tri"], writes=[r_c])
        T.dma("sp", iota[:], d["iota"], writes=[r_c])
        T.dma("sp", tokab[:], d["tokab"], writes=[r_c])
        r_aff = K.R("aff")
        tiles_all = list(range(0 if l == 0 else 2, NTT))
        for tt in tiles_all:
            T.op("pe", lambda tt=tt: nc.tensor.transpose(out=tp[0:NE, 0:128], in_=K.aff[:, tt, :], identity=K.ident_f[:]),
                 reads=[r_aff, R("ident")], writes=[r_tp])
            T.op("dve", lambda tt=tt: nc.vector.tensor_copy(out=affT[:, tt * 128:(tt + 1) * 128], in_=tp[0:NE, 0:128]),
                 reads=[r_tp], writes=[r_affT])
        for (ta, tb_, cap) in sets:
            c0, c1 = ta * 128, tb_ * 128
            T.op("dve", lambda: nc.vector.memset(bs[:, 0:1], 0.0), writes=[r_bs])
            for it in range(24):
                wk = 2.0 ** -(it + 1)
                T.op("dve", lambda wk=wk: nc.vector.tensor_scalar(out=bs[:, 1:2], in0=bs[:, 0:1], scalar1=wk, scalar2=None, op0=ALU.add),
                     reads=[r_bs], writes=[r_bs])
                T.op("dve", lambda: nc.vector.tensor_scalar(out=junk[:, 0:c1 - c0], in0=affT[:, c0:c1], scalar1=bs[:, 1:2], scalar2=0.0,
                                                            op0=ALU.is_ge, op1=ALU.add, accum_out=bs[:, 2:3]),
                     reads=[r_bs, r_affT], writes=[r_bs, r_j])
                T.op("dve", lambda: nc.vector.tensor_scalar(out=bs[:, 3:4], in0=bs[:, 2:3], scalar1=float(cap) - 0.5, scalar2=None,
                                                            op0=ALU.is_ge),
                     reads=[r_bs], writes=[r_bs])
                T.op("dve", lambda wk=wk: nc.vector.scalar_tensor_tensor(out=bs[:, 0:1], in0=bs[:, 3:4], scalar=wk, in1=bs[:, 0:1],
                                                                         op0=ALU.mult, op1=ALU.add),
                     reads=[r_bs], writes=[r_bs])
            T.op("dve", lambda: nc.vector.tensor_scalar(out=maskT[:, c0:c1], in0=affT[:, c0:c1], scalar1=bs[:, 0:1], scalar2=None,
                                                        op0=ALU.is_ge),
                 reads=[r_bs, r_affT], writes=[r_maskT])
        for tt in tiles_all:
            T.op("pe", lambda tt=tt: nc.tensor.transpose(out=tpb[:, 0:NE], in_=maskT[:, tt * 128:(tt + 1) * 128],
                                                         identity=K.ident_bf[0:NE, 0:NE]),
                 reads=[r_maskT, R("ident")], writes=[r_tpb])
            T.op("dve", lambda tt=tt: nc.vector.tensor_copy(out=mask[:, tt, :], in_=tpb[:, 0:NE]), reads=[r_tpb], writes=[r_mask])
        pn = 0
        for (ta, tb_, cap) in sets:
            for j in range(ta, tb_):
                pb = pn % 2
                pn += 1
                prev = list(range(ta, j))
                for ii, i in enumerate(prev):
                    T.op("pe", lambda i=i, ii=ii: nc.tensor.matmul(pp[pb][:, 0:NE], lhsT=tri[:, 0:128], rhs=mask[:, i, :],
                                                                  start=(ii == 0), stop=False),
                         reads=[r_mask, r_c], writes=[r_pp[pb]], inc=False)
                T.op("pe", lambda j=j: nc.tensor.matmul(pp[pb][:, 0:NE], lhsT=tri[:, 128:256], rhs=mask[:, j, :],
                                                       start=(len(prev) == 0), stop=True),
                     reads=[r_mask, r_c], writes=[r_pp[pb]])
                T.op("dve", lambda j=j: nc.vector.tensor_scalar(out=tmp[:, j, :], in0=mask[:, j, :], scalar1=-1.0, scalar2=10000.0,
                                                                op0=ALU.add, op1=ALU.mult),
                     reads=[r_mask], writes=[r_slot])
                T.op("dve", lambda j=j: nc.vector.tensor_tensor(out=slot[:, j, :], in0=tmp[:, j, :], in1=pp[pb][:, 0:NE], op=ALU.add),
                     reads=[r_pp[pb], r_slot], writes=[r_slot])
        T.op("dve", lambda: nc.vector.tensor_copy(out=Rt[:, :, :, 0:2], in_=tokab[:].unsqueeze(2).to_broadcast([128, NTT, NE, 2])),
             reads=[r_c], writes=[r_R])
        T.op("dve", lambda: nc.vector.tensor_copy(out=g1[:], in_=K.aff[:]), reads=[r_aff], writes=[r_R])
        T.op("dve", lambda: nc.vector.tensor_copy(out=Rt[:, :, :, 2], in_=g1[:]), reads=[r_R], writes=[r_R])
        T.op("dve", lambda: nc.vector.tensor_tensor(out=r1[:], in0=K.aff[:], in1=g1[:], op=ALU.subtract), reads=[r_aff, r_R], writes=[r_R])
        T.op("dve", lambda: nc.vector.tensor_copy(out=g1[:], in_=r1[:]), reads=[r_R], writes=[r_R])
        T.op("dve", lambda: nc.vector.tensor_copy(out=Rt[:, :, :, 3], in_=g1[:]), reads=[r_R], writes=[r_R])
        T.op("dve", lambda: nc.vector.tensor_tensor(out=r1[:], in0=r1[:], in1=g1[:], op=ALU.subtract), reads=[r_R], writes=[r_R])
        T.op("dve", lambda: nc.vector.tensor_copy(out=Rt[:, :, :, 4], in_=r1[:]), reads=[r_R], writes=[r_R])
        on = 0
        an = 0
        for (ta, tb_, cap) in sets:
            nsc = (cap + 127) // 128
            wid = min(cap, 512)
            for e in range(NE):
                ab = an % 2
                an += 1
                for j in range(ta, tb_):
                    ob = on % 3
                    on += 1
                    T.op("dve", lambda j=j, e=e: nc.vector.tensor_scalar(out=OH[ob][:, 0:wid], in0=iota[:, 0:wid],
                                                                          scalar1=slot[:, j, e:e + 1], scalar2=None, op0=ALU.is_equal),
                         reads=[r_slot, r_c], writes=[r_OH[ob]])
                    T.op("pe", lambda j=j, e=e: nc.tensor.matmul(ap_[ab][0:5, 0:wid], lhsT=Rt[:, j, e, :], rhs=OH[ob][:, 0:wid],
                                                                 start=(j == ta), stop=(j == tb_ - 1)),
                         reads=[r_OH[ob], r_R], writes=[r_ap[ab]], inc=True)
                T.op("dve", lambda: nc.vector.tensor_copy(out=acc5[0:5, 0:wid], in_=ap_[ab][0:5, 0:wid]), reads=[r_ap[ab]], writes=[r_a5])
                for sc in range(nsc):
                    m = min(128, cap - sc * 128)
                    T.op("pe", lambda sc=sc, m=m: nc.tensor.transpose(out=tp[0:m, sc * 8:sc * 8 + 5], in_=acc5[0:5, sc * 128:sc * 128 + m],
                                                                      identity=K.ident_f[0:5, 0:5]),
                         reads=[r_a5, R("ident")], writes=[r_tp])
                m = min(128, cap)
                av = tp[0:m, 0:32].rearrange("p (s c) -> p s c", c=8)
                T.op("dve", lambda: nc.vector.tensor_copy(out=accs[0:m, 0:nsc, :], in_=av[:, 0:nsc, 0:5]), reads=[r_tp], writes=[r_acc])
                T.op("dve", lambda: nc.vector.scalar_tensor_tensor(out=idf[0:m, 0:nsc], in0=accs[0:m, 0:nsc, 0], scalar=64.0,
                                                                   in1=accs[0:m, 0:nsc, 1], op0=ALU.mult, op1=ALU.add),
                     reads=[r_acc], writes=[r_acc])
                if cap == CAP:
                    idst, gdst = K.idx[:, e, :], K.gate[:, e, :]
                else:
                    idst, gdst = K.idxx[0:m, e:e + 1], K.gatex[0:m, e:e + 1]
                T.op("dve", lambda: nc.vector.tensor_copy(out=idst, in_=idf[0:m, 0:nsc]), reads=[r_acc], writes=[K.R("idx")])
                T.op("dve", lambda: nc.vector.tensor_tensor(out=gdst, in0=accs[0:m, 0:nsc, 2], in1=accs[0:m, 0:nsc, 3], op=ALU.add),
                     reads=[r_acc], writes=[K.R("idx")])
                T.op("dve", lambda: nc.vector.tensor_tensor(out=gdst, in0=gdst, in1=accs[0:m, 0:nsc, 4], op=ALU.add),
                     reads=[r_acc, K.R("idx")], writes=[K.R("idx")])


def phase_moe(K, l):
    nc, T, d, R = K.nc, K.T, K.d, K.R
    has_ctx = (l == 0)
    NS = CAP + (CAPX if has_ctx else 0)
    experts = K.dbg.get("experts", list(range(NE)))
    with ExitStack() as es:
        sb = lambda n, s, dt: es.enter_context(K.sbt(n, s, dt))
        NR = 6
        ring = [sb("mring%d" % i, [128, 16, 512], BF16) for i in range(NR)]
        xe = sb("mxe", [128, 4, D], BF16)
        xex = sb("mxex", [128, D], BF16)
        xeT = sb("mxeT", [128, 16, CAP + CAPX], BF16)
        hidT = sb("mhidT", [128, 12, CAP + CAPX], BF16)
        gt = [sb("mgt%d" % i, [128, D], F32) for i in range(2)]
        s1 = [sb("ms1%d" % i, [128, 512], F32) for i in range(2)]
        s1x = sb("ms1x", [128, 32], F32)
        yst = [sb("myst%d" % i, [128, 512], F32) for i in range(4)]
        GU = [es.enter_context(K.pst("mGU%d" % i, [128, 512], F32)) for i in range(4)]
        GX = es.enter_context(K.pst("mGX", [128, 512], F32))
        tp = [es.enter_context(K.pst("mtp%d" % i, [128, 1024], BF16)) for i in range(2)]
        r_ring = [Res() for _ in range(NR)]
        r_xe, r_xeT, r_hid, r_gt = Res(), Res(), Res(), Res()
        r_s1, r_s1x, r_y = [Res(), Res()], Res(), [Res() for _ in range(4)]
        r_GU, r_GX, r_tp = [Res() for _ in range(4)], Res(), [Res(), Res()]
        r_idx = K.R("idx")
        for w in range(2):
            load_bc(K, gt[w][:], d["modv"][l, w:w + 1, 5 * D:6 * D], r_gt, reads=[R("modv")])
        pieces = []
        for e in experts:
            for fb in range(3):
                pieces.append(("g", e, fb))
                pieces.append(("u", e, fb))
            for nb in range(4):
                pieces.append(("d", e, nb))
        pstate = dict(next=0)

        def issue_piece():
            i = pstate["next"]
            if i >= len(pieces):
                return
            kind, e, j = pieces[i]
            slot = i % NR
            if kind == "d":
                src = d["w_down"][l, e].rearrange("(fc p) n -> p fc n", p=128)[:, :, j * 512:(j + 1) * 512]
                T.dma("pool", ring[slot][:, 0:12, :], src, writes=[r_ring[slot]])
            else:
                wn = "w_gate" if kind == "g" else "w_up"
                src = d[wn][l, e].rearrange("(kc p) n -> p kc n", p=128)[:, :, j * 512:(j + 1) * 512]
                T.dma("pool", ring[slot][:], src, writes=[r_ring[slot]])
            pstate["next"] = i + 1

        def gather(e):
            for sc in range(4):
                T.dma("pool", xe[:, sc, :], d["H2"], reads=[R("H2"), r_idx], writes=[r_xe],
                      indirect=dict(out_offset=None, in_offset=bass.IndirectOffsetOnAxis(ap=K.idx[:, e, sc:sc + 1], axis=0)))
            if has_ctx:
                T.dma("pool", xex[0:CAPX, :], d["H2"], reads=[R("H2"), r_idx], writes=[r_xe],
                      indirect=dict(out_offset=None, in_offset=bass.IndirectOffsetOnAxis(ap=K.idxx[0:CAPX, e:e + 1], axis=0)))

        def transposes(e):
            tn = 0
            for sc in range(4):
                for g in range(2):
                    tb = tn % 2
                    tn += 1
                    for k8 in range(8):
                        kc = g * 8 + k8
                        T.op("pe", lambda kc=kc, k8=k8: nc.tensor.transpose(out=tp[tb][:, k8 * 128:(k8 + 1) * 128],
                                                                          in_=xe[:, sc, kc * 128:(kc + 1) * 128], identity=K.ident_bf[:]),
                             reads=[r_xe, R("ident")], writes=[r_tp[tb]], inc=(k8 == 7))
                    eng = "act" if g == 0 else "dve"
                    src = tp[tb][:].rearrange("p (a b) -> p a b", b=128)
                    dst = xeT[:, g * 8:(g + 1) * 8, sc * 128:(sc + 1) * 128]
                    if eng == "act":
                        T.op("act", lambda: nc.scalar.copy(out=dst, in_=src), reads=[r_tp[tb]], writes=[r_xeT])
                    else:
                        T.op("dve", lambda: nc.vector.tensor_copy(out=dst, in_=src), reads=[r_tp[tb]], writes=[r_xeT])
            if has_ctx:
                tb = tn % 2
                for kc in range(16):
                    T.op("pe", lambda kc=kc: nc.tensor.transpose(out=tp[tb][:, kc * 32:(kc + 1) * 32],
                                                                 in_=xex[0:CAPX, kc * 128:(kc + 1) * 128],
                                                                 identity=K.ident_bf[0:CAPX, 0:CAPX]),
                         reads=[r_xe, R("ident")], writes=[r_tp[tb]], inc=(kc == 15))
                T.op("dve", lambda: nc.vector.tensor_copy(out=xeT[:, :, CAP:CAP + CAPX],
                                                          in_=tp[tb][:, 0:512].rearrange("p (a b) -> p a b", b=32)),
                     reads=[r_tp[tb]], writes=[r_xeT])

        def ensure(upto):
            while pstate["next"] <= min(upto, len(pieces) - 1):
                issue_piece()
        pi = 0
        gn = 0
        yn = 0
        prev_sc = [[] for _ in range(4)]
        gather(experts[0])
        transposes(experts[0])
        for ei, e in enumerate(experts):
            if ei + 1 < len(experts):
                gather(experts[ei + 1])
            for fb in range(3):
                ensure(pi + NR - 1)
                sg, su = pi % NR, (pi + 1) % NR
                pi += 2
                for f4 in range(4):
                    fc = fb * 4 + f4
                    gb = gn % 2
                    gn += 1
                    G, U = GU[gb], GU[2 + gb]
                    for (ps_, slot_, rr) in ((G, sg, r_GU[gb]), (U, su, r_GU[2 + gb])):
                        for kc in range(16):
                            T.op("pe", lambda kc=kc, ps_=ps_, slot_=slot_: nc.tensor.matmul(
                                ps_[:, 0:CAP], lhsT=ring[slot_][:, kc, f4 * 128:(f4 + 1) * 128], rhs=xeT[:, kc, 0:CAP],
                                start=(kc == 0), stop=(kc == 15)),
                                reads=[r_ring[slot_], r_xeT], writes=[rr], inc=(kc == 15))
                    if has_ctx:
                        for ci, slot_ in enumerate((sg, su)):
                            for kc in range(16):
                                T.op("pe", lambda kc=kc, ci=ci, slot_=slot_: nc.tensor.matmul(
                                    GX[:, ci * 32:(ci + 1) * 32], lhsT=ring[slot_][:, kc, f4 * 128:(f4 + 1) * 128],
                                    rhs=xeT[:, kc, CAP:CAP + CAPX], start=(kc == 0), stop=(kc == 15)),
                                    reads=[r_ring[slot_], r_xeT], writes=[r_GX], inc=(kc == 15))
                    sbuf_ = gn % 2
                    T.op("act", lambda: nc.scalar.activation(out=s1[sbuf_][:], in_=G[:, 0:CAP], func=AF.Silu),
                         reads=[r_GU[gb]], writes=[r_s1[sbuf_]])
                    T.op("dve", lambda: nc.vector.tensor_tensor(out=hidT[:, fc, 0:CAP], in0=s1[sbuf_][:], in1=U[:, 0:CAP], op=ALU.mult),
                         reads=[r_s1[sbuf_], r_GU[2 + gb]], writes=[r_hid])
                    if has_ctx:
                        T.op("act", lambda: nc.scalar.activation(out=s1x[:], in_=GX[:, 0:32], func=AF.Silu),
                             reads=[r_GX], writes=[r_s1x])
                        T.op("dve", lambda: nc.vector.tensor_tensor(out=hidT[:, fc, CAP:CAP + CAPX], in0=s1x[:], in1=GX[:, 32:64], op=ALU.mult),
                             reads=[r_s1x, r_GX], writes=[r_hid])
            if ei + 1 < len(experts):
                transposes(experts[ei + 1])
            new_sc = [[] for _ in range(4)]
            for nb in range(4):
                ensure(pi + NR - 1)
                sd = pi % NR
                pi += 1
                for st_ in range(5 if has_ctx else 4):
                    m = 128 if st_ < 4 else CAPX
                    yb = yn % 4
                    yn += 1
                    Y = GU[yb]
                    for fc in range(12):
                        T.op("pe", lambda fc=fc: nc.tensor.matmul(Y[0:m, :], lhsT=hidT[:, fc, st_ * 128:st_ * 128 + m],
                                                                 rhs=ring[sd][:, fc, :], start=(fc == 0), stop=(fc == 11)),
                             reads=[r_hid, r_ring[sd]], writes=[r_GU[yb]], inc=(fc == 11))
                    if st_ < 4:
                        gsc, gtt, iap = K.gate[:, e, st_:st_ + 1], gt[0], K.idx[:, e, st_:st_ + 1]
                    else:
                        gsc, gtt, iap = K.gatex[0:m, e:e + 1], gt[1], K.idxx[0:m, e:e + 1]
                    T.op("dve", lambda: nc.vector.scalar_tensor_tensor(out=yst[yb][0:m, :], in0=Y[0:m, :], scalar=gsc,
                                                                       in1=gtt[0:m, nb * 512:(nb + 1) * 512], op0=ALU.mult, op1=ALU.mult),
                         reads=[r_GU[yb], r_idx, r_gt], writes=[r_y[yb]])
                    xsn = "XS%d_%d" % (l, nb)
                    tok = T.dma("pool", d[xsn], yst[yb][0:m, :], reads=[r_y[yb], r_idx, R(xsn)], writes=[],
                                extra_waits=prev_sc[nb],
                                indirect=dict(out_offset=bass.IndirectOffsetOnAxis(ap=iap, axis=0), in_offset=None,
                                              compute_op=ALU.add))
                    new_sc[nb].append(tok)
            prev_sc = new_sc
        for nb in range(4):
            rr = R("XS%d_%d" % (l, nb))
            for tok in prev_sc[nb]:
                rr.r[tok[0]] = tok


def phase_final(K):
    nc, T, d, R = K.nc, K.T, K.d, K.R
    l = 1
    with ExitStack() as es:
        sb = lambda n, s, dt: es.enter_context(K.sbt(n, s, dt))
        gb = sb("fgb", [128, D], F32)
        xt = [sb("fxt%d" % i, [128, D], F32) for i in range(2)]
        yo = [sb("fyo%d" % i, [128, D], F32) for i in range(2)]
        junk = sb("fjunk", [128, D], BF16)
        st = sb("fst", [128, 4], F32)
        r_g, r_x, r_y, r_st, r_j = Res(), [Res(), Res()], [Res(), Res()], Res(), Res()
        load_bc(K, gb[:], d["gvec"][4:5, :], r_g)
        tiles = list(range(2, NTT))

        def load(tt, b):
            for nb in range(4):
                T.dma("sp", xt[b][:, nb * 512:(nb + 1) * 512], d["XS%d_%d" % (l, nb)][tt * 128:(tt + 1) * 128, :],
                      reads=[R("XS%d_%d" % (l, nb))], writes=[r_x[b]])
        load(tiles[0], 0)
        for n, tt in enumerate(tiles):
            b = n % 2
            if n + 1 < len(tiles):
                load(tiles[n + 1], (n + 1) % 2)
            T.op("act", lambda: nc.scalar.activation(out=junk[:], in_=xt[b][:], func=AF.Square, accum_out=st[:, 0:1]),
                 reads=[r_x[b]], writes=[r_j, r_st])
            T.op("act", lambda: nc.scalar.activation(out=st[:, 1:2], in_=st[:, 0:1], func=AF.Sqrt, scale=1.0 / D, bias=EPS),
                 reads=[r_st], writes=[r_st])
            T.op("dve", lambda: nc.vector.reciprocal(out=st[:, 2:3], in_=st[:, 1:2]), reads=[r_st], writes=[r_st])
            T.op("dve", lambda: nc.vector.scalar_tensor_tensor(out=yo[b][:], in0=xt[b][:], scalar=st[:, 2:3], in1=gb[:],
                                                               op0=ALU.mult, op1=ALU.mult),
                 reads=[r_x[b], r_st, r_g], writes=[r_y[b]])
            T.dma("sp", d["out"][(tt - 2) * 128:(tt - 1) * 128, :], yo[b][:], reads=[r_y[b]], writes=[R("out")])


def make_in_maps(inputs, cores):
    c = const_pack()
    f = lambda a: np.ascontiguousarray(np.asarray(a, dtype=np.float32))
    small = np.zeros((1, 1024), np.float32)
    small[0, 0:8] = f(inputs["a_sink"])[0]
    small[0, 8:136] = f(inputs["b_lam_q1"])[0]
    small[0, 136:264] = f(inputs["b_lam_k1"])[0]
    small[0, 264:392] = f(inputs["b_lam_q2"])[0]
    small[0, 392:520] = f(inputs["b_lam_k2"])[0]
    small[0, 520:776] = f(inputs["b_subln"])[0]
    small2 = np.concatenate([f(inputs["c_q_norm"])[0], f(inputs["c_k_norm"])[0]])[None, :]
    gvec = np.stack([f(inputs["g_mix"])[0], f(inputs["g_mix"])[1], f(inputs["g_ffn"])[0], f(inputs["g_ffn"])[1],
                     f(inputs["g_final"])], axis=0)
    rpbtab = d_tables(f(inputs["d_rpb"])[0])
    shared = dict(
        w_ada=f(inputs["w_ada"]), b_ada=f(inputs["b_ada"]), gvec=np.ascontiguousarray(gvec), w_in=f(inputs["w_in"]),
        w_out=f(inputs["w_out"]), w_router=f(inputs["w_router"]), w_gate=f(inputs["w_gate"]), w_up=f(inputs["w_up"]),
        w_down=f(inputs["w_down"]), small=small, small2=np.ascontiguousarray(small2), rpbtab=rpbtab, **c)
    maps = []
    x = inputs["x"]
    ctx = inputs["ctx"]
    cc = f(inputs["c"])
    c_ctx = f(inputs["c_ctx"])
    for b in cores:
        m = dict(shared)
        m["x"] = f(x[b])
        m["ctx"] = f(ctx[b])
        m["cvec"] = np.ascontiguousarray(np.stack([cc[b], c_ctx], axis=0))
        maps.append(m)
    return maps


def kernel(**inputs):
    nc = build_program()
    maps = make_in_maps(inputs, list(range(8)))
    res = run_bass_kernel_spmd(nc, maps, core_ids=list(range(8)))
    out = np.stack([np.asarray(r["out"], dtype=np.float32) for r in res.results], axis=0)
    return out
```
